# Optimizing a Trainium2 kernel written in Bass

```python
import jax, jax.numpy as jnp
from jax import lax
import numpy as np

D_MODEL = 1024
BATCH = 16
SEQ = 2048
DEPTH = 2

SB_HEAD_DIM = 64
SB_HEADS = D_MODEL // (2 * SB_HEAD_DIM)
SB_WIDTH = SB_HEADS * SB_HEAD_DIM
MLA_NOPE_DIM = 64
MLA_ROPE_DIM = 32
MLA_V_DIM = 64
MLA_HEADS = D_MODEL // (2 * MLA_V_DIM)
MLA_Q_RANK = 256
MLA_KV_RANK = 128
MLA_WIDTH = MLA_HEADS * MLA_V_DIM
MIX_WIDTH = SB_WIDTH + MLA_WIDTH
IN_WIDTH = 3 * SB_WIDTH + MLA_Q_RANK + MLA_KV_RANK + MLA_ROPE_DIM
IN_SPLITS = (SB_WIDTH, 2 * SB_WIDTH, 3 * SB_WIDTH,
             3 * SB_WIDTH + MLA_Q_RANK, 3 * SB_WIDTH + MLA_Q_RANK + MLA_KV_RANK)
ROPE_BASE = 10000.0
Q_BLOCK = 128
N_EXPERTS = 32
N_GROUPS = 8
EXPERTS_PER_GROUP = N_EXPERTS // N_GROUPS
TOP_K = 2
D_EXPERT = 256
DISPATCH_BLOCK = 256
DEEPNORM_ALPHA = (2 * DEPTH) ** 0.25
DEEPNORM_BETA = (8 * DEPTH) ** -0.25
LN_EPS = 1e-5
RMS_EPS = 1e-6

kernel_name = "hybrid_stickbreak_mla_groupmoe_deepnorm_adaln"


def standardize(x):
    xf = x.astype(jnp.float32)
    mu = jnp.mean(xf, axis=-1, keepdims=True)
    var = jnp.mean(jnp.square(xf - mu), axis=-1, keepdims=True)
    return (xf - mu) * lax.rsqrt(var + LN_EPS)


def layer_norm(x, gain, bias):
    return (standardize(x) * gain.astype(jnp.float32) + bias.astype(jnp.float32)).astype(x.dtype)


def modulate(x, shift, scale):
    return (standardize(x) * (1.0 + scale.astype(jnp.float32)) + shift.astype(jnp.float32)).astype(x.dtype)


def rms_norm(x, gain):
    xf = x.astype(jnp.float32)
    y = xf * lax.rsqrt(jnp.mean(jnp.square(xf), axis=-1, keepdims=True) + RMS_EPS)
    return (y * gain.astype(jnp.float32)).astype(x.dtype)


def rope_tables(positions):
    inv_freq = ROPE_BASE ** (-jnp.arange(0, MLA_ROPE_DIM, 2, dtype=jnp.float32) / MLA_ROPE_DIM)
    ang = positions.astype(jnp.float32)[..., None] * inv_freq
    return jnp.cos(ang), jnp.sin(ang)


def apply_rope(t, cos, sin):
    t1, t2 = jnp.split(t.astype(jnp.float32), 2, axis=-1)
    return jnp.concatenate([t1 * cos - t2 * sin, t1 * sin + t2 * cos], axis=-1).astype(t.dtype)


def split_heads(t, n_heads):
    b, s, _ = t.shape
    return t.reshape(b, s, n_heads, -1).transpose(0, 2, 1, 3)


def merge_heads(t):
    b, h, s, d = t.shape
    return t.transpose(0, 2, 1, 3).reshape(b, s, h * d)


def stick_breaking_attention(q, k, v):
    scale = SB_HEAD_DIM ** -0.5
    outs = []
    for blk in range(q.shape[2] // Q_BLOCK):
        q0 = blk * Q_BLOCK
        end = q0 + Q_BLOCK
        z = jnp.einsum('bhqd,bhkd->bhqk', q[:, :, q0:end].astype(jnp.float32),
                       k[:, :, :end].astype(jnp.float32)) * scale
        strict = jnp.arange(end)[None, :] < (q0 + jnp.arange(Q_BLOCK))[:, None]
        log_keep = jnp.where(strict, jax.nn.log_sigmoid(-z), 0.0)
        later = lax.cumsum(log_keep, axis=3, reverse=True) - log_keep
        w = jnp.where(strict, jnp.exp(jax.nn.log_sigmoid(z) + later), 0.0)
        outs.append(jnp.einsum('bhqk,bhkd->bhqd', w.astype(v.dtype), v[:, :, :end]))
    return jnp.concatenate(outs, axis=2)


def mla_attention(q_nope, q_rope, k_nope, k_rope, v):
    scale = (MLA_NOPE_DIM + MLA_ROPE_DIM) ** -0.5
    outs = []
    for blk in range(q_nope.shape[2] // Q_BLOCK):
        q0 = blk * Q_BLOCK
        end = q0 + Q_BLOCK
        s = (jnp.einsum('bhqd,bhkd->bhqk', q_nope[:, :, q0:end].astype(jnp.float32),
                        k_nope[:, :, :end].astype(jnp.float32))
             + jnp.einsum('bhqr,bkr->bhqk', q_rope[:, :, q0:end].astype(jnp.float32),
                          k_rope[:, :end].astype(jnp.float32))) * scale
        causal = jnp.arange(end)[None, :] <= (q0 + jnp.arange(Q_BLOCK))[:, None]
        p = jax.nn.softmax(jnp.where(causal, s, -jnp.inf), axis=-1)
        outs.append(jnp.einsum('bhqk,bhkd->bhqd', p.astype(v.dtype), v[:, :, :end]))
    return jnp.concatenate(outs, axis=2)


def token_mixer(h, cos, sin, w_in, q_norm, kv_norm, w_uq, w_ukv, w_o):
    proj = h @ w_in
    sb_q, sb_k, sb_v, q_lat, kv_lat, k_rope = jnp.split(proj, IN_SPLITS, axis=-1)
    sb_out = stick_breaking_attention(split_heads(sb_q, SB_HEADS), split_heads(sb_k, SB_HEADS),
                                      split_heads(sb_v, SB_HEADS))
    q = split_heads(rms_norm(q_lat, q_norm) @ w_uq, MLA_HEADS)
    kv = split_heads(rms_norm(kv_lat, kv_norm) @ w_ukv, MLA_HEADS)
    q_rope = apply_rope(q[..., MLA_NOPE_DIM:], cos[:, None], sin[:, None])
    k_rope = apply_rope(k_rope, cos, sin)
    mla_out = mla_attention(q[..., :MLA_NOPE_DIM], q_rope, kv[..., :MLA_NOPE_DIM], k_rope,
                            kv[..., MLA_NOPE_DIM:])
    merged = jnp.concatenate([merge_heads(sb_out), merge_heads(mla_out)], axis=-1)
    return merged @ w_o


def moe_ffn(h, router_w, router_bias, w_gate, w_up, w_down):
    b, s, d = h.shape
    x = h.reshape(-1, d)
    n_tok = x.shape[0]
    scores = jax.nn.sigmoid(x.astype(jnp.float32) @ router_w.astype(jnp.float32))
    biased = (scores + router_bias.astype(jnp.float32)).reshape(n_tok, N_GROUPS, EXPERTS_PER_GROUP)
    group_score = lax.top_k(biased, TOP_K)[0].sum(-1)
    g_sel = jnp.argmax(group_score, axis=-1)
    in_group = jnp.take_along_axis(biased, g_sel[:, None, None], axis=1)[:, 0]
    _, local = lax.top_k(in_group, TOP_K)
    expert_idx = g_sel[:, None] * EXPERTS_PER_GROUP + local
    gate = jnp.take_along_axis(scores, expert_idx, axis=-1)
    gate = gate / jnp.sum(gate, axis=-1, keepdims=True)
    m = n_tok * TOP_K
    flat_e = expert_idx.reshape(m)
    flat_tok = jnp.repeat(jnp.arange(n_tok, dtype=jnp.int32), TOP_K)
    flat_gate = gate.reshape(m)
    order = jnp.argsort(flat_e)
    se, stok, sgate = flat_e[order], flat_tok[order], flat_gate[order]
    counts = jnp.bincount(flat_e, length=N_EXPERTS)
    padded = (counts + DISPATCH_BLOCK - 1) // DISPATCH_BLOCK * DISPATCH_BLOCK
    start = jnp.cumsum(counts) - counts
    pend = jnp.cumsum(padded)
    pstart = pend - padded
    dest = pstart[se] + jnp.arange(m, dtype=jnp.int32) - start[se]
    n_blocks = -(-m // DISPATCH_BLOCK) + N_EXPERTS
    buf = jnp.zeros((n_blocks * DISPATCH_BLOCK, d), x.dtype).at[dest].set(x[stok])
    block_e = jnp.minimum(jnp.searchsorted(pend, jnp.arange(n_blocks) * DISPATCH_BLOCK, side='right'),
                          N_EXPERTS - 1)

    def expert_block(args):
        xb, e = args
        return (jax.nn.silu(xb @ w_gate[e]) * (xb @ w_up[e])) @ w_down[e]

    y_buf = lax.map(expert_block, (buf.reshape(n_blocks, DISPATCH_BLOCK, d), block_e))
    y_buf = y_buf.reshape(n_blocks * DISPATCH_BLOCK, d)
    y = jax.ops.segment_sum(y_buf[dest] * sgate[:, None].astype(x.dtype), stok, num_segments=n_tok)
    return y.reshape(b, s, d)


def setup_inputs(seed: int = 0) -> dict:
    key = jax.random.key(seed)
    ks = jax.random.split(key, 20)
    f32 = jnp.float32

    def nrm(k, shape, scale):
        return jax.random.normal(k, shape, f32) * scale

    d_in = D_MODEL ** -0.5
    x = nrm(ks[0], (BATCH, SEQ, D_MODEL), 1.0)
    c = nrm(ks[1], (BATCH, D_MODEL), 1.0)
    offset = jax.random.randint(ks[2], (BATCH, 1), 0, 1024, dtype=jnp.int32)
    positions = offset + jnp.arange(SEQ, dtype=jnp.int32)[None, :]
    ada_w = nrm(ks[3], (DEPTH, D_MODEL, 6 * D_MODEL), 0.1 * d_in)
    ada_b = nrm(ks[4], (DEPTH, 6 * D_MODEL), 0.01)
    in_scale = jnp.concatenate([
        jnp.full((2 * SB_WIDTH,), d_in, f32),
        jnp.full((SB_WIDTH,), d_in * DEEPNORM_BETA, f32),
        jnp.full((MLA_Q_RANK + MLA_KV_RANK + MLA_ROPE_DIM,), d_in, f32)])
    w_in = nrm(ks[5], (DEPTH, D_MODEL, IN_WIDTH), 1.0) * in_scale
    q_norm = 1.0 + nrm(ks[6], (DEPTH, MLA_Q_RANK), 0.01)
    kv_norm = 1.0 + nrm(ks[7], (DEPTH, MLA_KV_RANK), 0.01)
    w_uq = nrm(ks[8], (DEPTH, MLA_Q_RANK, MLA_HEADS * (MLA_NOPE_DIM + MLA_ROPE_DIM)), MLA_Q_RANK ** -0.5)
    ukv_scale = jnp.tile(jnp.concatenate([jnp.ones((MLA_NOPE_DIM,), f32),
                                          jnp.full((MLA_V_DIM,), DEEPNORM_BETA, f32)]),
                         MLA_HEADS) * (MLA_KV_RANK ** -0.5)
    w_ukv = nrm(ks[9], (DEPTH, MLA_KV_RANK, MLA_HEADS * (MLA_NOPE_DIM + MLA_V_DIM)), 1.0) * ukv_scale
    w_o = nrm(ks[10], (DEPTH, MIX_WIDTH, D_MODEL), MIX_WIDTH ** -0.5 * DEEPNORM_BETA)
    ln1_g = 1.0 + nrm(ks[11], (DEPTH, D_MODEL), 0.01)
    ln1_b = nrm(ks[12], (DEPTH, D_MODEL), 0.01)
    router_w = nrm(ks[13], (D_MODEL, N_EXPERTS), d_in)
    router_bias = nrm(ks[14], (N_EXPERTS,), 0.01)
    w_gate = nrm(ks[15], (DEPTH, N_EXPERTS, D_MODEL, D_EXPERT), d_in * DEEPNORM_BETA)
    w_up = nrm(ks[16], (DEPTH, N_EXPERTS, D_MODEL, D_EXPERT), d_in * DEEPNORM_BETA)
    w_down = nrm(ks[17], (DEPTH, N_EXPERTS, D_EXPERT, D_MODEL), D_EXPERT ** -0.5 * DEEPNORM_BETA)
    ln2_g = 1.0 + nrm(ks[18], (DEPTH, D_MODEL), 0.01)
    ln2_b = nrm(ks[19], (DEPTH, D_MODEL), 0.01)
    return {"x": x, "c": c, "positions": positions, "ada_w": ada_w, "ada_b": ada_b,
            "w_in": w_in, "q_norm": q_norm, "kv_norm": kv_norm, "w_uq": w_uq, "w_ukv": w_ukv,
            "w_o": w_o, "ln1_g": ln1_g, "ln1_b": ln1_b, "router_w": router_w,
            "router_bias": router_bias, "w_gate": w_gate, "w_up": w_up, "w_down": w_down,
            "ln2_g": ln2_g, "ln2_b": ln2_b}


def reference(x, c, positions, ada_w, ada_b, w_in, q_norm, kv_norm, w_uq, w_ukv, w_o,
              ln1_g, ln1_b, router_w, router_bias, w_gate, w_up, w_down, ln2_g, ln2_b):
    cos, sin = rope_tables(positions)
    c_act = jax.nn.silu(c)
    for l in range(DEPTH):
        mod = (c_act @ ada_w[l] + ada_b[l])[:, None, :]
        shift1, scale1, gate1, shift2, scale2, gate2 = jnp.split(mod, 6, axis=-1)
        h = modulate(x, shift1, scale1)
        mix = token_mixer(h, cos, sin, w_in[l], q_norm[l], kv_norm[l], w_uq[l], w_ukv[l], w_o[l])
        x = layer_norm(DEEPNORM_ALPHA * x + (1.0 + gate1) * mix, ln1_g[l], ln1_b[l])
        h = modulate(x, shift2, scale2)
        ffn = moe_ffn(h, router_w, router_bias, w_gate[l], w_up[l], w_down[l])
        x = layer_norm(DEEPNORM_ALPHA * x + (1.0 + gate2) * ffn, ln2_g[l], ln2_b[l])
    return x
```

```python
import numpy as np
from contextlib import ExitStack
import concourse.bass as bass
import concourse.mybir as mybir
from concourse.bass_utils import run_bass_kernel_spmd

F32 = mybir.dt.float32
BF16 = mybir.dt.bfloat16
I32 = mybir.dt.int32
AF = mybir.ActivationFunctionType
ALU = mybir.AluOpType
AX = mybir.AxisListType

P = 128
D = 1024
KC = 8
NE = 32
DE = 256
IN_W = 1952
ALPHA = float((2 * 2) ** 0.25)
LN_EPS = 1e-5
RMS_EPS = 1e-6
PI = float(np.pi)
NDMA = 24
DEBUG_MG = False
DBG = set()


class Sched:
    ENG = ("pe", "act", "dve", "pool", "sp")

    def __init__(self, nc, es):
        self.nc = nc
        self.sem = {e: es.enter_context(nc.semaphore("s_" + e)) for e in self.ENG}
        self.dsem = [es.enter_context(nc.semaphore("s_dma%d" % i)) for i in range(NDMA)]
        self.cnt = {e: 0 for e in self.ENG}
        self.dcnt = [0] * NDMA
        self.drr = {"sp": 0, "pool": 0}
        self.known = {e: {} for e in self.ENG}
        self.prog = {e: [] for e in self.ENG}
        self.last_w = {}
        self.readers = {}
        self.out_tokens = []
        self.defer = None

    def _deps(self, eng, r, w):
        deps = {}

        def add(tok):
            if tok is None:
                return
            k, v = tok
            if deps.get(k, 0) < v:
                deps[k] = v

        for k in r:
            add(self.last_w.get(k))
            if isinstance(k, tuple) and k[0] in ("Zb", "WT", "M"):
                for tok in self.readers.get(k, {}).items():
                    if tok[0] != eng:
                        add(tok)
        for k in w:
            add(self.last_w.get(k))
            for tok in self.readers.get(k, {}).items():
                add(tok)
        waits = []
        kn = self.known[eng]
        for k, v in deps.items():
            if k == eng and eng == "pe":
                continue
            if kn.get(k, 0) >= v:
                continue
            kn[k] = v
            waits.append((k, v))
        return waits

    def _commit(self, tok, r, w):
        for k in w:
            self.last_w[k] = tok
            self.readers[k] = {}
        for k in r:
            if k in w:
                continue
            d = self.readers.setdefault(k, {})
            if d.get(tok[0], 0) < tok[1]:
                d[tok[0]] = tok[1]

    def flush(self, lst):
        d, self.defer = self.defer, None
        for item in lst:
            if item[0] == "dma":
                self.dma(*item[1:])
            else:
                self.op(*item)
        self.defer = d

    def op(self, eng, fn, r=(), w=()):
        if self.defer is not None:
            self.defer.append((eng, fn, tuple(r), tuple(w)))
            return
        waits = self._deps(eng, r, w)
        self.cnt[eng] += 1
        tok = (eng, self.cnt[eng])
        self.prog[eng].append((waits, fn, (eng, 1)))
        self._commit(tok, r, w)

    def dma(self, q, fn, r=(), w=(), is_out=False):
        if self.defer is not None:
            self.defer.append(("dma", q, fn, tuple(r), tuple(w), is_out))
            return
        half = NDMA // 2
        i = self.drr[q] + (0 if q == "sp" else half)
        self.drr[q] = (self.drr[q] + 1) % half
        waits = self._deps(q, r, w)
        dk = ("d", i)
        prev = self.dcnt[i]
        if prev > 0 and self.known[q].get(dk, 0) < prev:
            self.known[q][dk] = prev
            waits.append((dk, prev))
        self.dcnt[i] += 16
        tok = (dk, self.dcnt[i])
        self.prog[q].append((waits, fn, (dk, 16)))
        self._commit(tok, r, w)
        if is_out:
            self.out_tokens.append(tok)

    def barrier(self):
        for e in self.ENG:
            waits = []
            for o in self.ENG:
                if o != e and self.cnt[o] > self.known[e].get(o, 0):
                    self.known[e][o] = self.cnt[o]
                    waits.append((o, self.cnt[o]))
            for i in range(NDMA):
                dk = ("d", i)
                if self.dcnt[i] > self.known[e].get(dk, 0):
                    self.known[e][dk] = self.dcnt[i]
                    waits.append((dk, self.dcnt[i]))
            if waits:
                self.prog[e].append((waits, None, None))

    def finish(self):
        best = {}
        for (dk, v) in self.out_tokens:
            if best.get(dk, 0) < v:
                best[dk] = v
        self.prog["sp"].append((list(best.items()), None, None))

    def _semof(self, k):
        if isinstance(k, tuple):
            return self.dsem[k[1]]
        return self.sem[k]

    def replay(self, block):
        sections = {"pe": block.tensor, "act": block.scalar, "dve": block.vector,
                    "pool": block.gpsimd, "sp": block.sync}
        for en in self.ENG:
            prog = self.prog[en]

            def body(e, prog=prog):
                for waits, fn, inc in prog:
                    for (k, v) in waits:
                        e.wait_ge(self._semof(k), v)
                    if fn is not None:
                        ins = fn(e)
                        ins.then_inc(self._semof(inc[0]), inc[1])

            sections[en](body)


def build_nc(S=2048, NB=2, L=2, TQ=512, KCH=1024, NEXP=NE, KSB=512, KML=1024, TMOE=256):
    T = min(TQ, S)
    NT = S // T
    NS = T // P
    TT = S // P
    nc = bass.Bass("TRN2", target_bir_lowering=False)

    def din(name, shape, dt=F32):
        return nc.dram_tensor(name, list(shape), dt, kind="ExternalInput").ap()

    x_d = din("x", [NB, S, D])
    cT_d = din("cT", [P, KC * NB])
    posr_d = din("posr", [NB, 32, S], I32)
    cst_d = din("cst", [P, 4])
    ada_w_d = din("ada_w", [L, D, 6 * D])
    ada_bT_d = din("ada_bT", [P, L * 48])
    w_in_d = din("w_in", [L, D, IN_W])
    w_kr_d = din("w_kr", [L, D, 64])
    qn_d = din("qn", [P, L * 2])
    kvn_d = din("kvn", [P, L])
    w_uq_d = din("w_uq", [L, 256, 1024])
    w_uqs_d = din("w_uqs", [L, 256, 256])
    wk_d = din("wk_p", [L, 128, 1024])
    wv_d = din("wv_p", [L, 128, 512])
    w_o_d = din("w_o", [L, D, D])
    lnp_d = din("lnp", [P, L * 4 * KC])
    rw_d = din("router_w", [D, NE])
    rb_d = din("rbias", [P, NE])
    wg_d = din("w_gate", [L, NE, D, DE])
    wu_d = din("w_up", [L, NE, D, DE])
    wd_d = din("w_down", [L, NE, DE, D])
    out_d = nc.dram_tensor("out", [NB, S, D], F32, kind="ExternalOutput").ap()
    scr_d = nc.dram_tensor("xscr", [NB, P, KC, S], F32, kind="Internal").ap()
    dbg_d = nc.dram_tensor("dbg_mg", [P, KC, S], BF16, kind="ExternalOutput").ap() if DEBUG_MG else None

    with ExitStack() as es:
        sc = Sched(nc, es)

        sb_cache = {}

        def sb(name, shape, dt, stack=es):
            if name not in sb_cache:
                sb_cache[name] = stack.enter_context(nc.sbuf_tensor("sb_" + name, list(shape), dt))
            return sb_cache[name]

        Z = [es.enter_context(nc.psum_tensor("Z%d" % i, [P, 1024], F32)) for i in range(2)]
        WTp = es.enter_context(nc.psum_tensor("WTp", [P, 1024], F32))
        M = [es.enter_context(nc.psum_tensor("M%d" % i, [P, 512], F32)) for i in range(2)]
        WTb = WTp[:, :].bitcast(BF16)

        def MK(i, a=0, b=512):
            return [("M", i)]

        def mm(out, lhsT, rhs, start, stop, r, w):
            sc.op("pe", lambda e: e.matmul(out, lhsT=lhsT, rhs=rhs, start=start, stop=stop), r, w)

        def tr(out, in_, ident, r, w):
            sc.op("pe", lambda e: e.transpose(out, in_, ident), r, w)

        def act(out, in_, func, r, w, bias=None, scale=None, accum_out=None):
            kw = {}
            if bias is not None:
                kw["bias"] = bias
            if scale is not None:
                kw["scale"] = scale
            if accum_out is not None:
                kw["accum_out"] = accum_out
            sc.op("act", lambda e: e.activation(out, in_, func, **kw), r, w)

        def tt(eng, out, in0, in1, op, r, w):
            sc.op(eng, lambda e: e.tensor_tensor(out, in0, in1, op), r, w)

        def ts(eng, out, in0, s1, s2, op0, op1, r, w):
            if s2 is None:
                sc.op(eng, lambda e: e.tensor_scalar(out, in0, s1, None, op0), r, w)
            else:
                sc.op(eng, lambda e: e.tensor_scalar(out, in0, s1, s2, op0, op1), r, w)

        def stt(eng, out, in0, scalar, in1, op0, op1, r, w):
            sc.op(eng, lambda e: e.scalar_tensor_tensor(out, in0, scalar, in1, op0, op1), r, w)

        def cp(eng, out, in_, r, w):
            if eng == "act":
                sc.op("act", lambda e: e.activation(out, in_, AF.Copy), r, w)
            else:
                sc.op(eng, lambda e: e.tensor_copy(out, in_), r, w)

        def dma(q, out, in_, r, w, is_out=False):
            sc.dma(q, lambda e: e.dma_start(out=out, in_=in_), r, w, is_out=is_out)

        ident_f = sb("ident_f", [P, P], F32)
        ident_b = sb("ident_b", [P, P], BF16)
        ones_f = sb("ones_f", [P, P], F32)
        mask_s = sb("mask_s", [P, P], F32)
        mask_sb = sb("mask_sb", [P, P], BF16)
        negm = sb("negm", [P, P], F32)
        maskT = sb("maskT", [P, P], BF16)
        ones_b = sb("ones_b", [P, P], BF16)
        scan1 = sb("scan1", [P, KCH], F32)
        selrows = sb("selrows", [32, NE * P], BF16)
        cst = sb("cst", [P, 4], F32)
        modT = sb("modT", [P, L * NB * 48], F32)
        ada_bT = sb("ada_bT", [P, L * 48], F32)
        lnp = sb("lnp", [P, L * 4 * KC], F32)
        qn = sb("qn", [P, L * 2], F32)
        kvn = sb("kvn", [P, L], F32)
        rw = sb("rw", [P, KC, NE], F32)
        rb = sb("rb", [P, NE], F32)
        rbt = sb("rbt", [P, NS * NE], F32)
        cact = sb("cact", [P, KC * NB], F32)
        hT = sb("hT", [P, KC, S], BF16)
        xt = sb("xt", [P, KC, T], F32)
        tmpA = sb("tmpA", [P, T], F32)
        tmpB = sb("tmpB", [P, T], F32)
        tmpC = sb("tmpC", [P, T], F32)
        tmpD = sb("tmpD", [P, T], F32)
        cb16 = [sb("cb16_%d" % i, [P, T], BF16) for i in range(2)]
        sq16 = [sb("sq16_%d" % i, [P, T], BF16) for i in range(2)]
        cwT = sb("cwT", [32, 2, S], BF16)
        rope = sb("rope", [P, 2, S], BF16)
        xin = sb("xin", [P, D], F32)
        xin2 = sb("xin2", [P, D], F32)
        xins = [(xin, "xin0"), (xin2, "xin1")]
        io_cnt = [0]

        def mod(l, b, j):
            o = (l * NB + b) * 48 + j
            return modT[:, o:o + 1]

        def lnpar(l, which, c):
            o = (l * 4 + which) * KC + c
            return lnp[:, o:o + 1]

        sc.op("pool", lambda e: e.memset(ident_f[:], 1.0), w=["ident_f"])
        sc.op("pool", lambda e: e.affine_select(out=ident_f[:], in_=ident_f[:], pattern=[[-1, P]],
                                                compare_op=ALU.is_equal, fill=0.0, base=0,
                                                channel_multiplier=1), r=["ident_f"], w=["ident_f"])
        cp("pool", ident_b[:], ident_f[:], ["ident_f"], ["ident_b"])
        sc.op("pool", lambda e: e.memset(ones_f[:], 1.0), w=["ones_f"])
        sc.op("pool", lambda e: e.memset(scan1[:], 1.0), w=["scan1"])
        sc.op("pool", lambda e: e.memset(mask_s[:], 1.0), w=["mask_s"])
        sc.op("pool", lambda e: e.affine_select(out=mask_s[:], in_=mask_s[:], pattern=[[-1, P]],
                                                compare_op=ALU.is_gt, fill=0.0, base=0,
                                                channel_multiplier=1), r=["mask_s"], w=["mask_s"])
        cp("pool", mask_sb[:], mask_s[:], ["mask_s"], ["mask_sb"])
        sc.op("pool", lambda e: e.memset(negm[:], 0.0), w=["negm"])
        sc.op("pool", lambda e: e.affine_select(out=negm[:], in_=negm[:], pattern=[[-1, P]],
                                                compare_op=ALU.is_ge, fill=-30000.0, base=0,
                                                channel_multiplier=1), r=["negm"], w=["negm"])
        ts("dve", maskT[:], mask_s[:], -1.0, 1.0, ALU.mult, ALU.add, ["mask_s"], ["maskT"])
        cp("dve", ones_b[:], ones_f[:], ["ones_f"], ["ones_b"])
        sc.op("pool", lambda e: e.memset(selrows[:], 1.0), w=["selrows"])
        sc.op("pool", lambda e: e.affine_select(
            out=selrows[:].rearrange("k (e m) -> k e m", m=P), in_=selrows[:].rearrange("k (e m) -> k e m", m=P),
            pattern=[[-1, NE], [0, P]], compare_op=ALU.is_equal, fill=0.0, base=0,
            channel_multiplier=1), r=["selrows"], w=["selrows"])

        dma("sp", cst[:], cst_d, [], ["cst"])
        dma("sp", ada_bT[:], ada_bT_d, [], ["ada_bT"])
        dma("sp", lnp[:], lnp_d, [], ["lnp"])
        dma("sp", qn[:], qn_d, [], ["qn"])
        dma("sp", kvn[:], kvn_d, [], ["kvn"])
        dma("sp", rw[:], rw_d.rearrange("(c p) n -> p c n", p=P), [], ["rw"])
        dma("sp", rb[:], rb_d, [], ["rb"])
        for s_ in range(NS):
            dma("sp", rbt[:, s_ * NE:(s_ + 1) * NE], rb_d, [], ["rbt"])
        dma("sp", cact[:], cT_d, [], ["cact"])
        act(tmpA[:, 0:KC * NB], cact[:], AF.Exp, ["cact"], ["tmpA"], scale=-1.0)
        ts("dve", tmpA[:, 0:KC * NB], tmpA[:, 0:KC * NB], 1.0, None, ALU.add, None, ["tmpA"], ["tmpA"])
        sc.op("dve", lambda e: e.reciprocal(tmpA[:, 0:KC * NB], tmpA[:, 0:KC * NB]), ["tmpA"], ["tmpA"])
        tt("dve", cact[:], cact[:], tmpA[:, 0:KC * NB], ALU.mult, ["cact", "tmpA"], ["cact"])

        with ExitStack() as es0:
            awst = [sb("awst%d" % i, [P, KC, 512], BF16, es0) for i in range(3)]
            cact16 = sb("cact16", [P, KC * NB], BF16, es0)
            cp("dve", cact16[:], cact[:], ["cact"], ["cact16"])
            gi = 0
            for l in range(L):
                for jg in range(12):
                    bufi = gi % 3
                    gi += 1
                    aw = awst[bufi]
                    dma("pool", aw[:], ada_w_d[l, :, jg * 512:(jg + 1) * 512].rearrange("(c p) n -> p c n", p=P),
                        [], [("awst", bufi)])
                    for jj in range(4):
                        j = jg * 4 + jj
                        for kc in range(KC):
                            mm(M[jj % 2][:, 0:NB], aw[:, kc, jj * P:(jj + 1) * P],
                               cact16[:, kc * NB:(kc + 1) * NB], kc == 0, kc == KC - 1,
                               [("awst", bufi), "cact16"], MK(jj % 2))
                        for b in range(NB):
                            ts("dve", mod(l, b, j), M[jj % 2][:, b:b + 1], ada_bT[:, l * 48 + j:l * 48 + j + 1], None,
                               ALU.add, None, MK(jj % 2) + ["ada_bT"], ["modT"])
                for b in range(NB):
                    for (lo, hi) in ((8, 24), (32, 48)):
                        o = (l * NB + b) * 48
                        ts("dve", modT[:, o + lo:o + hi], modT[:, o + lo:o + hi], 1.0, None, ALU.add, None,
                           ["modT"], ["modT"])
            sc.barrier()

        def stats(src_keys, xt=xt):
            for c in range(KC):
                i = c % 2
                cp("dve", cb16[i][:], xt[:, c, :], src_keys, [("cb16", i)])
                mm(M[0][:, 0:T], ones_b[:], cb16[i][:], c == 0, c == KC - 1, [("cb16", i), "ones_b"], MK(0))
                act(sq16[i][:], xt[:, c, :], AF.Square, src_keys, [("sq16", i)])
                mm(M[1][:, 0:T], ones_b[:], sq16[i][:], c == 0, c == KC - 1, [("sq16", i), "ones_b"], MK(1))
            ts("dve", tmpA[:], M[0][:, 0:T], 1.0 / D, None, ALU.mult, None, MK(0), ["tmpA"])
            tt("dve", tmpC[:], tmpA[:], tmpA[:], ALU.mult, ["tmpA"], ["tmpC"])
            stt("dve", tmpC[:], M[1][:, 0:T], 1.0 / D, tmpC[:], ALU.mult, ALU.subtract, MK(1) + ["tmpC"], ["tmpC"])
            ts("dve", tmpC[:], tmpC[:], LN_EPS, None, ALU.add, None, ["tmpC"], ["tmpC"])
            act(tmpC[:], tmpC[:], AF.Ln, ["tmpC"], ["tmpC"])
            act(tmpB[:], tmpC[:], AF.Exp, ["tmpC"], ["tmpB"], scale=-0.5)

        def normalize(c, out_ap, scale_ap, bias_ap, src_keys, out_keys, second_out=None, xt=xt):
            tbuf, tk = (tmpD, "tmpD") if c % 2 == 0 else (tmpC, "tmpC")
            tt("dve", tbuf[:], xt[:, c, :], tmpA[:], ALU.subtract, src_keys + ["tmpA"], [tk])
            tt("dve", tbuf[:], tbuf[:], tmpB[:], ALU.mult, [tk, "tmpB"], [tk])
            act(out_ap, tbuf[:], AF.Identity, [tk, "modT", "lnp"], out_keys, bias=bias_ap, scale=scale_ap)
            if second_out is not None:
                o2, k2 = second_out
                act(o2, tbuf[:], AF.Identity, [tk, "modT", "lnp"], k2, bias=bias_ap, scale=scale_ap)

        def adaln_to_hT(l, b, t, which, second=None, xt=xt, xk="xt"):
            stats([xk], xt)
            base = 0 if which == 0 else 24
            for c in range(KC):
                so = None
                if second is not None:
                    so = (second[:, c, :], ["h2f"])
                normalize(c, hT[:, c, t * T:(t + 1) * T], mod(l, b, base + 8 + c), mod(l, b, base + c),
                          [xk], [("hT", t)], so, xt)

        def spill(b, t, xt=xt, xk="xt"):
            dma("sp", scr_d[b, :, :, t * T:(t + 1) * T], xt[:], [xk], [("scr", b, t)])

        def reload(b, t, xt=xt, xk="xt"):
            dma("sp", xt[:], scr_d[b, :, :, t * T:(t + 1) * T], [("scr", b, t)], [xk])

        for b in range(NB):
            with ExitStack() as esr:
                posi = sb("posi", [P, S], I32, esr)
                ang = sb("ang", [P, S], F32, esr)
                ang2 = sb("ang2", [P, S], F32, esr)
                kf = sb("kf", [P, S], F32, esr)
                R = slice(0, 32)
                dma("sp", posi[R, :], posr_d[b], [], ["posi"])
                cp("dve", ang[R, :], posi[R, :], ["posi"], ["ang"])
                ts("dve", ang[R, :], ang[R, :], cst[R, 0:1], None, ALU.mult, None, ["ang", "cst"], ["ang"])
                C1 = 6.28125
                C2 = 2.0 * PI - C1
                for which, shift in ((0, 0.5 * PI), (1, 0.0)):
                    if shift != 0.0:
                        ts("dve", ang2[R, :], ang[R, :], shift, None, ALU.add, None, ["ang"], ["ang2"])
                    else:
                        cp("dve", ang2[R, :], ang[R, :], ["ang"], ["ang2"])
                    ts("dve", posi[R, :], ang2[R, :], 1.0 / (2.0 * PI), None, ALU.mult, None, ["ang2"], ["posi"])
                    cp("dve", kf[R, :], posi[R, :], ["posi"], ["kf"])
                    stt("dve", ang2[R, :], kf[R, :], -C1, ang2[R, :], ALU.mult, ALU.add, ["kf", "ang2"], ["ang2"])
                    stt("dve", ang2[R, :], kf[R, :], -C2, ang2[R, :], ALU.mult, ALU.add, ["kf", "ang2"], ["ang2"])
                    ts("dve", kf[R, :], ang2[R, :], PI, -2.0 * PI, ALU.is_gt, ALU.mult, ["ang2"], ["kf"])
                    tt("dve", ang2[R, :], ang2[R, :], kf[R, :], ALU.add, ["ang2", "kf"], ["ang2"])
                    ts("dve", ang2[R, :], ang2[R, :], -3.1415925, 3.1415925, ALU.max, ALU.min, ["ang2"], ["ang2"])
                    act(ang2[R, :], ang2[R, :], AF.Sin, ["ang2"], ["ang2"])
                    if which == 0:
                        cp("dve", rope[R, 0, :], ang2[R, :], ["ang2"], ["rope"])
                    else:
                        ts("dve", rope[R, 1, :], ang2[R, :], cst[R, 1:2], None, ALU.mult, None,
                           ["ang2", "cst"], ["rope"])
                sc.barrier()

            for l in range(L):
                if l == 0:
                    for t in range(NT):
                        for s_ in range(NS):
                            tok0 = t * T + s_ * P
                            xi, xik = xins[io_cnt[0] % 2]
                            io_cnt[0] += 1
                            dma("sp", xi[:], x_d[b, tok0:tok0 + P, :], [], [xik])
                            for half in range(2):
                                Zt = Z[half]
                                for c4 in range(4):
                                    c = half * 4 + c4
                                    tr(Zt[:, c4 * P:(c4 + 1) * P], xi[:, c * P:(c + 1) * P], ident_f[:],
                                       [xik, "ident_f"], [("Zb", 2 * half)])
                                for c4 in range(4):
                                    c = half * 4 + c4
                                    cp("act" if c4 % 2 else "dve", xt[:, c, s_ * P:(s_ + 1) * P],
                                       Zt[:, c4 * P:(c4 + 1) * P], [("Zb", 2 * half)], ["xt"])
                        spill(b, t)
                        adaln_to_hT(l, b, t, 0)
                    sc.barrier()

                with ExitStack() as esBC:
                    mergedT = sb("mergedT", [P, KC, S], BF16, esBC)
                    with ExitStack() as esB:
                        QT = sb("QT", [P, 2, S], BF16, esB)
                        KT = sb("KT", [P, 2, S], BF16, esB)
                        Vp = sb("Vp", [P, TT, P], BF16, esB)
                        lat = sb("lat", [P, 4096], F32, esB)
                        uT = lat[:, 0:S].bitcast(BF16).rearrange("p (c s) -> p c s", c=2)
                        ukvT = lat[:, 2048:2048 + S // 2].bitcast(BF16)
                        krT = lat[:, 3072:3072 + S // 2].bitcast(BF16)
                        wst = [sb("wst%d" % i, [P, KC, 384], BF16, esB) for i in range(2)]
                        wuq = sb("wuq", [P, 2, 1024], BF16, esB)
                        wuqs = sb("wuqs", [P, 2, 256], BF16, esB)
                        wk = sb("wk", [P, 1024], BF16, esB)
                        wv = sb("wv", [P, 512], BF16, esB)
                        kst = sb("kst", [P, 16], F32, esB)
                        nball = lat[:, 0:2048]
                        bball = lat[:, 2048:4096]
                        dbuf = [sb("dbuf%d" % i, [P, P], F32, esB) for i in range(4)]
                        wrall = sb("wrall", [P, 2048], BF16, esB)
                        wtsall = sb("wtsall", [P, 2048], BF16, esB)
                        opair = [sb("opair%d" % i, [P, P], BF16, esB) for i in range(2)]
                        rcs = [sb("rcs%d" % i, [P, 8], F32, esB) for i in range(8)]
                        rrs = [sb("rrs%d" % i, [P, 16], F32, esB) for i in range(4)]
                        state = {"row": 0, "wst": 0, "zc": 0, "oc": 0, "rc": 0}

                        def load_w(col_specs):
                            i = state["wst"] % 2
                            state["wst"] += 1
                            off = 0
                            for (src, ncols) in col_specs:
                                dma("pool", wst[i][:, :, off:off + ncols], src.rearrange("(c p) n -> p c n", p=P),
                                    [], [("wst", i)])
                                off += ncols
                            return wst[i], ("wst", i)

                        def proj_T(dst_fn, w_ap, wkey, ncolsM, scale=None, dst_keys=()):
                            for t in range(NT):
                                bi = t % 4
                                ps = Z[bi // 2][0:ncolsM, (bi % 2) * 512:(bi % 2) * 512 + T]
                                for c in range(KC):
                                    mm(ps, w_ap[:, c, :], hT[:, c, t * T:(t + 1) * T], c == 0, c == KC - 1,
                                       [wkey, ("hT", t)], [("Zb", bi)])
                                dst_fn(t, ps, ("Zb", bi))

                        NSTG = 7

                        def attn_rows(kind, hh, Kdim, pbase, qidx, vcol0, mcol, opi, tasks):
                            sm_scale = (64 + 32) ** -0.5
                            CH = KSB if kind == "sb" else KML
                            units = 1 if kind == "sb" else 2
                            for qb in range(TT):
                                F = (qb + 1) * P
                                q_ap = QT[pbase:pbase + Kdim, qidx, qb * P:(qb + 1) * P]
                                rr = rrs[state["row"] % 4]
                                rrk = ("rrs", state["row"] % 4)
                                state["row"] += 1
                                chunks = []
                                f1 = F
                                while f1 > 0:
                                    f0 = max(0, f1 - CH)
                                    chunks.append((f0, f1))
                                    f1 = f0
                                nch = len(chunks)
                                crec = []
                                if kind == "sb":
                                    osl = state["oc"] % 2
                                    state["oc"] += 1
                                for ci, (f0, f1) in enumerate(chunks):
                                    n = f1 - f0
                                    if kind == "sb":
                                        u = state["zc"] % 4
                                        state["zc"] += 1
                                        ukeys = [u]
                                    else:
                                        u = 2 * (state["zc"] % 2)
                                        state["zc"] += 1
                                        ukeys = [u, u + 1]
                                    c0 = u * 512
                                    zt = Z[u // 2][:, (u % 2) * 512:(u % 2) * 512 + units * 512]
                                    zk = [("Zb", k) for k in ukeys]
                                    nb, nk = nball[:, c0:c0 + units * 512], [("nb", k) for k in ukeys]
                                    bb, bk = bball[:, c0:c0 + units * 512], [("bb", k) for k in ukeys]
                                    db, dk = dbuf[u], [("dbuf", u)]
                                    wr, wrk = wrall[:, c0:c0 + units * 512], [("wr", k) for k in ukeys]
                                    wti = (u // units) % 2
                                    wt, wtk = WTb[:, wti * 1024:(wti + 1) * 1024], [("WT", wti)]
                                    wts, wtsk = wtsall[:, c0:c0 + units * 512], [("wts", k) for k in ukeys]
                                    rci = state["rc"] % 8
                                    state["rc"] += 1
                                    rc, rck = rcs[rci], [("rcs", rci)]
                                    stg = [[] for _ in range(NSTG)]
                                    tasks.append(stg)
                                    sc.defer = stg[0]
                                    for g0 in range(f0, f1, 512):
                                        g1 = min(f1, g0 + 512)
                                        mm(zt[:, g0 - f0:g1 - f0], q_ap, KT[pbase:pbase + Kdim, qidx, g0:g1],
                                           True, True, ["QT", "KT"], [("Zb", u + (g0 - f0) // 512)])
                                    lo_n = n
                                    if kind == "sb":
                                        sc.defer = stg[1]
                                        act(nb[:, 0:n], zt[:, 0:n], AF.Exp, zk, nk, scale=-1.0)
                                        act(nb[:, 0:n], nb[:, 0:n], AF.Ln, nk, nk, bias=1.0)
                                        sc.defer = stg[2]
                                        if ci == 0:
                                            d0 = n - P
                                            lo_n = d0
                                            tt("dve", db[:], zt[:, d0:n], nb[:, d0:n], ALU.add, zk + nk, dk)
                                            tt("pool", db[:], db[:], mask_s[:], ALU.mult, dk + ["mask_s"], dk)
                                            sc.op("dve", lambda e, bb=bb, db=db, d0=d0, n=n: e.tensor_tensor_scan(
                                                out=bb[:, d0:n][:, ::-1], data0=scan1[:, 0:P], data1=db[:, ::-1],
                                                initial=0.0, op0=ALU.mult, op1=ALU.add),
                                                dk + ["scan1"], bk)
                                            cp("dve", rr[:, 0:1], bb[:, d0:d0 + 1], bk, [rrk])
                                            tt("dve", bb[:, d0:n], bb[:, d0:n], zt[:, d0:n], ALU.subtract, bk + zk, bk)
                                        if lo_n > 0:
                                            tt("dve", bb[:, lo_n - 1:lo_n], rr[:, 0:1], nb[:, lo_n - 1:lo_n], ALU.add,
                                               [rrk] + nk, bk)
                                            if lo_n > 1:
                                                sc.op("dve", lambda e, zt=zt, nb=nb, bb=bb, lo_n=lo_n: e.tensor_tensor_scan(
                                                    out=bb[:, 0:lo_n - 1][:, ::-1], data0=zt[:, 1:lo_n][:, ::-1],
                                                    data1=nb[:, 0:lo_n - 1][:, ::-1], initial=bb[:, lo_n - 1:lo_n],
                                                    op0=ALU.add, op1=ALU.add),
                                                    zk + nk + bk, bk)
                                        if ci + 1 < nch:
                                            tt("dve", rr[:, 0:1], bb[:, 0:1], zt[:, 0:1], ALU.add, bk + zk, [rrk])
                                        sc.defer = stg[3]
                                        act(wr[:, 0:n], bb[:, 0:n], AF.Exp, bk, wrk, scale=-1.0)
                                        if ci == 0:
                                            tt("pool", wr[:, n - P:n], wr[:, n - P:n], mask_sb[:], ALU.mult,
                                               wrk + ["mask_sb"], wrk)
                                    else:
                                        sc.defer = stg[1]
                                        sc.op("dve", lambda e, rc=rc, zt=zt, n=n: e.reduce_max(rc[:, 0:1], zt[:, 0:n], AX.X),
                                              zk, rck)
                                        ts("dve", rc[:, 1:2], rc[:, 0:1], -sm_scale, None, ALU.mult, None, rck, rck)
                                        sc.op("dve", lambda e, rc=rc: e.memset(rc[:, 2:4], 0.0), [], rck)
                                        if ci == 0:
                                            d0 = n - P
                                            lo_n = d0
                                            tt("dve", db[:], zt[:, d0:n], negm[:], ALU.add, zk + ["negm"], dk)
                                        sc.defer = stg[2]
                                        if ci == 0:
                                            act(wr[:, d0:n], db[:], AF.Exp, dk + rck, wrk + rck,
                                                bias=rc[:, 1:2], scale=sm_scale, accum_out=rc[:, 2:3])
                                        if lo_n > 0:
                                            act(wr[:, 0:lo_n], zt[:, 0:lo_n], AF.Exp, zk + rck, wrk + rck,
                                                bias=rc[:, 1:2], scale=sm_scale, accum_out=rc[:, 3:4])
                                        tt("dve", rc[:, 4:5], rc[:, 2:3], rc[:, 3:4], ALU.add, rck, rck)
                                        osl = state["oc"] % 2
                                        state["oc"] += 1
                                    sc.defer = stg[4]
                                    nblk = n // P
                                    for jj in range(nblk):
                                        tr(wt[:, jj * P:(jj + 1) * P], wr[:, jj * P:(jj + 1) * P], ident_b[:],
                                           wrk + ["ident_b"], wtk)
                                    sc.defer = stg[5]
                                    cp("dve", wts[:, 0:n], wt[:, 0:n], wtk, wtsk)
                                    sc.defer = stg[6]
                                    ops = M[osl][:, 0:64]
                                    opk = MK(osl)
                                    for jj in range(nblk):
                                        if kind == "sb":
                                            st = (ci == 0 and jj == 0)
                                            sp_ = (ci == nch - 1 and jj == nblk - 1)
                                        else:
                                            st = (jj == 0)
                                            sp_ = (jj == nblk - 1)
                                        mm(ops, wts[:, jj * P:(jj + 1) * P], Vp[:, f0 // P + jj, vcol0:vcol0 + 64], st, sp_,
                                           wtsk + ["Vp"], opk)
                                    crec.append((ops, opk, rc, rck))
                                op_ = opair[opi[0] % 2]
                                ok_ = ("opair", opi[0] % 2)
                                dst = op_[:, 64 * hh:64 * hh + 64]
                                if kind == "sb":
                                    ops, opk, _, _ = crec[-1]
                                    cp("dve", dst, ops, opk, [ok_])
                                elif nch == 1:
                                    ops, opk, rc, rck = crec[0]
                                    sc.op("dve", lambda e, rc=rc: e.reciprocal(rc[:, 5:6], rc[:, 4:5]), rck, rck)
                                    ts("dve", dst, ops, rc[:, 5:6], None, ALU.mult, None, opk + rck, [ok_])
                                else:
                                    assert nch == 2
                                    (o0, ok0, r0, rk0), (o1, ok1, r1, rk1) = crec
                                    tt("dve", rr[:, 2:3], r0[:, 0:1], r1[:, 0:1], ALU.max, rk0 + rk1, [rrk])
                                    ts("dve", rr[:, 3:4], rr[:, 2:3], -sm_scale, None, ALU.mult, None, [rrk], [rrk])
                                    act(rr[:, 4:5], r0[:, 0:1], AF.Exp, rk0 + [rrk], [rrk], bias=rr[:, 3:4], scale=sm_scale)
                                    act(rr[:, 5:6], r1[:, 0:1], AF.Exp, rk1 + [rrk], [rrk], bias=rr[:, 3:4], scale=sm_scale)
                                    tt("dve", rr[:, 6:7], rr[:, 4:5], r0[:, 4:5], ALU.mult, [rrk] + rk0, [rrk])
                                    stt("dve", rr[:, 7:8], rr[:, 5:6], r1[:, 4:5], rr[:, 6:7], ALU.mult, ALU.add,
                                        [rrk] + rk1, [rrk])
                                    sc.op("dve", lambda e, rr=rr: e.reciprocal(rr[:, 8:9], rr[:, 7:8]), [rrk], [rrk])
                                    ts("dve", rr[:, 4:6], rr[:, 4:6], rr[:, 8:9], None, ALU.mult, None, [rrk], [rrk])
                                    ts("dve", dst, o0, rr[:, 4:5], None, ALU.mult, None, ok0 + [rrk], [ok_])
                                    stt("dve", dst, o1, rr[:, 5:6], dst, ALU.mult, ALU.add, ok1 + [rrk, ok_], [ok_])
                                if hh == 1:
                                    finish_pair(qb, op_, ok_, mcol)
                                sc.defer = None
                                yield qb

                        def finish_pair(qb, op_, ok_, mcol):
                            k = state["oc"] % 2
                            state["oc"] += 1
                            tps = M[k][:, 0:64].bitcast(BF16)
                            tr(tps, op_[:], ident_b[:], [ok_, "ident_b"], MK(k))
                            cp("dve", mergedT[:, mcol, qb * P:(qb + 1) * P], tps, MK(k), [("mg", mcol)])

                        def run_pair(kind, Kdim, pbases, qidxs, vcols, mcol):
                            opi = [0]
                            tasks = []
                            g0 = attn_rows(kind, 0, Kdim, pbases[0], qidxs[0], vcols[0], mcol, opi, tasks)
                            g1 = attn_rows(kind, 1, Kdim, pbases[1], qidxs[1], vcols[1], mcol, opi, tasks)
                            for _ in range(TT):
                                next(g0)
                                next(g1)
                                opi[0] += 1
                            nt = len(tasks)
                            for step in range(nt + NSTG - 1):
                                for sg_ in range(NSTG - 1, -1, -1):
                                    k = step - sg_
                                    if 0 <= k < nt:
                                        sc.flush(tasks[k][sg_])

                        for pr in range(4):
                            wt_, wkey = load_w([(w_in_d[l, :, pr * P:(pr + 1) * P], P),
                                                (w_in_d[l, :, 512 + pr * P:512 + (pr + 1) * P], P),
                                                (w_in_d[l, :, 1024 + pr * P:1024 + (pr + 1) * P], P)])

                            def put_q(t, ps, zk):
                                act(QT[:, 0, t * T:(t + 1) * T], ps, AF.Copy, [zk], ["QT"], scale=0.125)

                            def put_k(t, ps, zk):
                                cp("dve", KT[:, 0, t * T:(t + 1) * T], ps, [zk], ["KT"])

                            proj_T(put_q, wt_[:, :, 0:P], wkey, P)
                            proj_T(put_k, wt_[:, :, P:2 * P], wkey, P)
                            for j in range(TT):
                                t = (j * P) // T
                                ps = M[j % 2][:, 0:P]
                                pk = MK(j % 2, 0, P)
                                for c in range(KC):
                                    mm(ps, hT[:, c, j * P:(j + 1) * P], wt_[:, c, 2 * P:3 * P], c == 0, c == KC - 1,
                                       [wkey, ("hT", t)], pk)
                                cp("act" if j % 2 else "dve", Vp[:, j, :], ps, pk, ["Vp"])
                            run_pair("sb", 64, (0, 64), (0, 0), (0, 64), pr)

                        sc.barrier()
                        wt_, wkey = load_w([(w_in_d[l, :, 1536:1792], 256)])
                        wt2_, wkey2 = load_w([(w_in_d[l, :, 1792:1920], 128), (w_kr_d[l, :, 0:64], 64)])
                        dma("pool", wuq[:], w_uq_d[l].rearrange("(c p) n -> p c n", p=P), [], ["wuq"])
                        dma("pool", wuqs[:], w_uqs_d[l].rearrange("(c p) n -> p c n", p=P), [], ["wuqs"])
                        dma("pool", wk[:], wk_d[l], [], ["wk"])
                        dma("pool", wv[:], wv_d[l], [], ["wv"])
                        R = slice(0, 32)
                        for t in range(NT if "nolat" not in DBG else 0):
                            cols = slice(t * T, (t + 1) * T)
                            for c2 in range(2):
                                ps = Z[0][:, c2 * 512:c2 * 512 + T]
                                for c in range(KC):
                                    mm(ps, wt_[:, c, c2 * P:(c2 + 1) * P], hT[:, c, cols], c == 0, c == KC - 1,
                                       [wkey, ("hT", t)], [("Zb", c2)])
                                act(tmpA[:] if c2 == 0 else tmpB[:], ps, AF.Square, [("Zb", c2)],
                                    ["tmpA" if c2 == 0 else "tmpB"])
                            mm(M[0][:, 0:T], ones_f[:], tmpA[:], True, False, ["tmpA", "ones_f"], MK(0))
                            mm(M[0][:, 0:T], ones_f[:], tmpB[:], False, True, ["tmpB", "ones_f"], MK(0))
                            act(tmpC[:], M[0][:, 0:T], AF.Ln, MK(0), ["tmpC"], bias=RMS_EPS, scale=1.0 / 256)
                            act(tmpC[:], tmpC[:], AF.Exp, ["tmpC"], ["tmpC"], scale=-0.5)
                            for c2 in range(2):
                                stt("dve", uT[:, c2, cols], Z[0][:, c2 * 512:c2 * 512 + T], qn[:, l * 2 + c2:l * 2 + c2 + 1],
                                    tmpC[:], ALU.mult, ALU.mult, [("Zb", c2), "qn", "tmpC"], ["uT"])
                            ps = Z[1][:, 0:T]
                            for c in range(KC):
                                mm(ps, wt2_[:, c, 0:P], hT[:, c, cols], c == 0, c == KC - 1, [wkey2, ("hT", t)], [("Zb", 2)])
                            act(tmpA[:], ps, AF.Square, [("Zb", 2)], ["tmpA"])
                            mm(M[1][:, 0:T], ones_f[:], tmpA[:], True, True, ["tmpA", "ones_f"], MK(1))
                            act(tmpD[:], M[1][:, 0:T], AF.Ln, MK(1), ["tmpD"], bias=RMS_EPS, scale=1.0 / 128)
                            act(tmpD[:], tmpD[:], AF.Exp, ["tmpD"], ["tmpD"], scale=-0.5)
                            stt("dve", ukvT[:, cols], ps, kvn[:, l:l + 1], tmpD[:], ALU.mult, ALU.mult,
                                [("Zb", 2), "kvn", "tmpD"], ["ukvT"])
                            psA = Z[1][0:32, 512:512 + T]
                            for c in range(KC):
                                mm(psA, wt2_[:, c, P:P + 32], hT[:, c, cols], c == 0, c == KC - 1, [wkey2, ("hT", t)], [("Zb", 3)])
                            psB = M[0][0:32, 0:T]
                            for c in range(KC):
                                mm(psB, wt2_[:, c, P + 32:P + 64], hT[:, c, cols], c == 0, c == KC - 1, [wkey2, ("hT", t)], MK(0))
                            tt("dve", tmpA[R, :], psA, rope[R, 0, cols], ALU.mult, [("Zb", 3), "rope"], ["tmpA"])
                            tt("dve", tmpB[R, :], psB, rope[R, 1, cols], ALU.mult, MK(0) + ["rope"], ["tmpB"])
                            tt("dve", krT[R, cols], tmpA[R, :], tmpB[R, :], ALU.add, ["tmpA", "tmpB"], ["krT"])

                        sm_scale = (64 + 32) ** -0.5
                        HS = S // 2
                        for pr in range(4 if "nomlaheads" not in DBG else 0):
                            for j in range(TT):
                                ps = M[j % 2][:, 0:P]
                                pk = MK(j % 2, 0, P)
                                mm(ps, ukvT[:, j * P:(j + 1) * P], wv[:, 128 * pr:128 * pr + 128], True, True, ["ukvT", "wv"], pk)
                                cp("act" if j % 2 else "dve", Vp[:, j, :], ps, pk, ["Vp"])
                            for hh in range(2):
                                h = 2 * pr + hh
                                qk_, kk_ = ("QTm", hh), ("KTm", hh)
                                for t in range(NT if "noproj" not in DBG else 0):
                                    cols = slice(t * T, (t + 1) * T)
                                    psA = Z[0][:, 0:T]
                                    psB = Z[0][0:32, 512:512 + T]
                                    for c2 in range(2):
                                        mm(psA, wuq[:, c2, P * h:P * h + P], uT[:, c2, cols], c2 == 0, c2 == 1,
                                           ["wuq", "uT"], [("Zb", 0)])
                                    for c2 in range(2 if "noB" not in DBG else 0):
                                        mm(psB, wuqs[:, c2, 32 * h:32 * h + 32], uT[:, c2, cols], c2 == 0, c2 == 1,
                                           ["wuqs", "uT"], [("Zb", 1)])
                                    if "noQcopy" not in DBG:
                                        cp("act", QT[:, hh, cols], Z[0][:, 0:T], [("Zb", 0)], [qk_])
                                    if "norope" not in DBG:
                                        tt("dve", tmpA[R, :], Z[0][R, 0:T], rope[R, 0, cols], ALU.mult, [("Zb", 0), "rope"], ["tmpA"])
                                    if "noB" not in DBG:
                                        tt("dve", tmpB[R, :], psB, rope[R, 1, cols], ALU.mult, [("Zb", 1), "rope"], ["tmpB"])
                                    if "norope" not in DBG:
                                        tt("dve", QT[R, hh, cols], tmpA[R, :], tmpB[R, :], ALU.add, ["tmpA", "tmpB"], [qk_])
                                    psK = Z[1][:, 0:T]
                                    if "noK" not in DBG:
                                        mm(psK, wk[:, P * h:P * h + P], ukvT[:, cols], True, True, ["wk", "ukvT"], [("Zb", 2)])
                                        cp("act", KT[:, hh, cols], Z[1][:, 0:T], [("Zb", 2)], [kk_])
                                if "nokr" not in DBG:
                                    cp("dve", KT[R, hh, :], krT[R, :], ["krT"], [kk_])
                                sqb = wtsall
                                if "nonrm" not in DBG:
                                    act(sqb[:, 0:S], KT[:, hh, :], AF.Square, [kk_], ["sqb"])
                                for t in range(NT if "nonrm" not in DBG else 0):
                                    mm(M[0][0:64, 0:T], ones_b[:, 0:64], sqb[:, t * T:(t + 1) * T], True, True, ["sqb", "ones_b"], MK(0))
                                    sc.op("dve", lambda e, t=t: e.reduce_max(kst[0:64, t:t + 1], M[0][0:64, 0:T], AX.X),
                                          MK(0), ["kst"])
                                if "nonrm" not in DBG:
                                    sc.op("dve", lambda e: e.reduce_max(kst[0:64, 8:9], kst[0:64, 0:NT], AX.X), ["kst"], ["kst"])
                                    act(kst[0:64, 9:10], kst[0:64, 8:9], AF.Ln, ["kst"], ["kst"])
                                    act(kst[0:64, 9:10], kst[0:64, 9:10], AF.Exp, ["kst"], ["kst"], scale=0.5)
                                    ts("dve", kst[0:64, 10:11], kst[0:64, 9:10], -1.0, None, ALU.mult, None, ["kst"], ["kst"])
                                    act(sqb[:, 0:S], QT[:, hh, :], AF.Square, [qk_], ["sqb"])
                                A1 = slice(32, 33)
                                for t in range(NT if "nonrm" not in DBG else 0):
                                    cols = slice(t * T, (t + 1) * T)
                                    mm(M[1][0:64, 0:T], ones_b[:, 0:64], sqb[:, cols], True, True, ["sqb", "ones_b"], MK(1))
                                    if "noaug" in DBG:
                                        continue
                                    act(tmpC[A1, :], M[1][A1, 0:T], AF.Ln, MK(1), ["tmpC"], bias=1e-20)
                                    act(tmpC[A1, :], tmpC[A1, :], AF.Exp, ["tmpC"], ["tmpC"], scale=0.5)
                                    ts("dve", QT[A1, hh, cols], tmpC[A1, :], kst[A1, 10:11], None, ALU.mult, None,
                                       ["tmpC", "kst"], [qk_])
                                if "noaug" not in DBG:
                                    sc.op("dve", lambda e, hh=hh: e.memset(KT[32:33, hh, :], 1.0), [kk_], [kk_])

                            mt = []
                            cnt = 0
                            for hh in range(2):
                                qk_, kk_ = ("QTm", hh), ("KTm", hh)
                                rows = slice(0, 64) if hh == 0 else slice(64, 128)
                                Mv = P
                                for half in range(2 if "noattn" not in DBG else 0):
                                    qlo, qhi = half * HS, (half + 1) * HS
                                    nj = qhi // P
                                    for j in range(nj):
                                        stg = [[] for _ in range(4)]
                                        mt.append(stg)
                                        zi = cnt % 2
                                        cnt += 1
                                        c0 = max(qlo, j * P)
                                        F = qhi - c0
                                        diag = (j * P >= qlo)
                                        zt = Z[zi]
                                        pT, pk_ = wrall[:, zi * 1024:zi * 1024 + F], ("pT", zi)
                                        sc.defer = stg[0]
                                        for g0 in range(0, F, 512):
                                            g1 = min(F, g0 + 512)
                                            mm(zt[:, g0:g1], KT[:, hh, j * P:(j + 1) * P], QT[:, hh, c0 + g0:c0 + g1],
                                               True, True, [qk_, kk_], [("Zb", 2 * zi + g0 // 512)])
                                        sc.defer = stg[1]
                                        act(pT, zt[:, 0:F], AF.Exp, [("Zb", 2 * zi + k) for k in range((F + 511) // 512)],
                                            [pk_], scale=sm_scale)
                                        sc.defer = stg[2]
                                        if diag:
                                            tt("dve", pT[:, 0:P], pT[:, 0:P], maskT[:], ALU.mult, [pk_, "maskT"], [pk_])
                                        sc.defer = stg[3]
                                        a0 = c0 - qlo
                                        for ab in range((HS + 511) // 512):
                                            lo = max(a0, ab * 512)
                                            hi = min(HS, (ab + 1) * 512)
                                            if lo >= hi:
                                                continue
                                            jl = min(nj, (qlo + hi) // P) - 1
                                            st, sp_ = (j == 0), (j == jl)
                                            mm(WTp[0:Mv, lo:hi], Vp[:, j, 0:Mv], pT[:, lo - a0:hi - a0], st, sp_,
                                               [pk_, "Vp"], [("WT", ab)])
                                            mm(M[ab][0:Mv, lo - ab * 512:hi - ab * 512], ones_b[:, 0:Mv], pT[:, lo - a0:hi - a0],
                                               st, sp_, [pk_, "ones_b"], MK(ab))
                                        if j == nj - 1:
                                            for ab in range((HS + 511) // 512):
                                                wdt = min(512, HS - ab * 512)
                                                tb, tk = (tmpA, "tmpA") if ab == 0 else (tmpB, "tmpB")
                                                if "rcp2" in DBG:
                                                    cp("dve", tb[rows, 0:wdt], M[ab][rows, 0:wdt], MK(ab), [tk])
                                                    sc.op("dve", lambda e, tb=tb, wdt=wdt, rows=rows: e.reciprocal(
                                                        tb[rows, 0:wdt], tb[rows, 0:wdt]), [tk], [tk])
                                                else:
                                                    sc.op("dve", lambda e, tb=tb, ab=ab, wdt=wdt, rows=rows: e.reciprocal(
                                                        tb[rows, 0:wdt], M[ab][rows, 0:wdt]), MK(ab), [tk])
                                                tt("dve", mergedT[rows, 4 + pr, qlo + ab * 512:qlo + ab * 512 + wdt],
                                                   WTp[rows, ab * 512:ab * 512 + wdt], tb[rows, 0:wdt], ALU.mult,
                                                   [("WT", ab), tk], [("mg", 4 + pr)])
                            sc.defer = None
                            nt_ = len(mt)
                            for step in range(nt_ + 3):
                                for sg_ in range(3, -1, -1):
                                    k = step - sg_
                                    if 0 <= k < nt_:
                                        sc.flush(mt[k][sg_])
                        sc.barrier()

                    if DEBUG_MG and b == 0 and l == 0:
                        dma("sp", dbg_d, mergedT[:], [("mg", c) for c in range(KC)], ["dbg"], is_out=True)
                    with ExitStack() as esC:
                        wo = sb("wo", [P, KC, D], BF16, esC)
                        h2f = sb("h2f", [P, KC, T], F32, esC)
                        rt = sb("rt", [P, 4 * NS * NE + NS * 64 + 2 * NS + 8], F32, esC)
                        for half in range(2):
                            dma("pool", wo[:, :, half * 512:(half + 1) * 512],
                                w_o_d[l, :, half * 512:(half + 1) * 512].rearrange("(c p) n -> p c n", p=P), [], ["wo"])
                        xt2 = sb("xt2", [P, KC, T], F32, esC)
                        xbufs = [(xt, "xt"), (xt2, "xt2")]
                        ctasks = []
                        for t in range(NT):
                            cols = slice(t * T, (t + 1) * T)
                            xb, xk = xbufs[t % 2]
                            stg = [[] for _ in range(4)]
                            ctasks.append(stg)
                            sc.defer = stg[0]
                            reload(b, t, xb, xk)
                            for dc in range(KC):
                                bi = dc % 4
                                ps = Z[bi // 2][:, (bi % 2) * 512:(bi % 2) * 512 + T]
                                zk = ("Zb", bi)
                                for c in range(KC):
                                    mm(ps, wo[:, c, dc * P:(dc + 1) * P], mergedT[:, c, cols], c == 0, c == KC - 1,
                                       ["wo", ("mg", c)], [zk])
                                act(xb[:, dc, :], xb[:, dc, :], AF.Copy, [xk], [xk], scale=ALPHA)
                                stt("dve", xb[:, dc, :], ps, mod(l, b, 16 + dc), xb[:, dc, :], ALU.mult, ALU.add,
                                    [zk, "modT", xk], [xk])
                            sc.defer = stg[1]
                            stats([xk], xb)
                            for c in range(KC):
                                normalize(c, xb[:, c, :], lnpar(l, 0, c), lnpar(l, 1, c), [xk], [xk], None, xb)
                            spill(b, t, xb, xk)
                            sc.defer = stg[2]
                            adaln_to_hT(l, b, t, 1, second=h2f, xt=xb, xk=xk)
                            sc.defer = stg[3]
                            W = NS * NE
                            lg = M[0][:, 0:W]
                            for s_ in range(NS):
                                for c in range(KC):
                                    mm(lg[:, s_ * NE:(s_ + 1) * NE], h2f[:, c, s_ * P:(s_ + 1) * P], rw[:, c, :],
                                       c == 0, c == KC - 1, ["h2f", "rw"], MK(0))
                            scv = rt[:, 0:W]
                            bs = rt[:, W:2 * W]
                            act(scv, lg, AF.Exp, MK(0), ["rt"], scale=-1.0)
                            ts("dve", scv, scv, 1.0, None, ALU.add, None, ["rt"], ["rt"])
                            sc.op("dve", lambda e, scv=scv: e.reciprocal(scv, scv), ["rt"], ["rt"])
                            tt("dve", bs, scv, rbt[:], ALU.add, ["rt", "rbt"], ["rt"])
                            b4 = bs.rearrange("p (s g f) -> p s g f", s=NS, f=4)
                            G8 = lambda i: rt[:, 2 * W + NS * 8 * i:2 * W + NS * 8 * (i + 1)].rearrange("p (s g) -> p s g", s=NS)
                            hi1, lo1, hi2, lo2, top1, sec, gs, gm = [G8(i) for i in range(8)]
                            tt("dve", hi1, b4[:, :, :, 0], b4[:, :, :, 1], ALU.max, ["rt"], ["rt"])
                            tt("dve", lo1, b4[:, :, :, 0], b4[:, :, :, 1], ALU.min, ["rt"], ["rt"])
                            tt("dve", hi2, b4[:, :, :, 2], b4[:, :, :, 3], ALU.max, ["rt"], ["rt"])
                            tt("dve", lo2, b4[:, :, :, 2], b4[:, :, :, 3], ALU.min, ["rt"], ["rt"])
                            tt("dve", top1, hi1, hi2, ALU.max, ["rt"], ["rt"])
                            tt("dve", hi1, hi1, hi2, ALU.min, ["rt"], ["rt"])
                            tt("dve", lo1, lo1, lo2, ALU.max, ["rt"], ["rt"])
                            tt("dve", sec, hi1, lo1, ALU.max, ["rt"], ["rt"])
                            tt("dve", gs, top1, sec, ALU.add, ["rt"], ["rt"])
                            o2 = 2 * W + NS * 64
                            gmx = rt[:, o2:o2 + NS]
                            gsum = rt[:, o2 + NS:o2 + 2 * NS]
                            sc.op("dve", lambda e, gmx=gmx, gs=gs: e.reduce_max(gmx, gs, AX.X), ["rt"], ["rt"])
                            for s_ in range(NS):
                                ts("dve", gm[:, s_, :], gs[:, s_, :], gmx[:, s_:s_ + 1], None, ALU.is_ge, None, ["rt"], ["rt"])
                            sel = rt[:, o2 + 2 * NS:o2 + 2 * NS + W]
                            s4 = sel.rearrange("p (s g f) -> p s g f", s=NS, f=4)
                            for i in range(4):
                                tt("dve", s4[:, :, :, i], b4[:, :, :, i], sec, ALU.is_ge, ["rt"], ["rt"])
                                tt("dve", s4[:, :, :, i], s4[:, :, :, i], gm, ALU.mult, ["rt"], ["rt"])
                            tt("dve", sel, sel, scv, ALU.mult, ["rt"], ["rt"])
                            sc.op("dve", lambda e, gsum=gsum, sel=sel: e.reduce_sum(
                                gsum, sel.rearrange("p (s e) -> p s e", s=NS), AX.X), ["rt"], ["rt"])
                            sc.op("dve", lambda e, gsum=gsum: e.reciprocal(gsum, gsum), ["rt"], ["rt"])
                            cwv = rt[:, o2 + 2 * NS + W:o2 + 2 * NS + 2 * W]
                            for s_ in range(NS):
                                ts("dve", cwv[:, s_ * NE:(s_ + 1) * NE], sel[:, s_ * NE:(s_ + 1) * NE], gsum[:, s_:s_ + 1],
                                   None, ALU.mult, None, ["rt"], ["rt"])
                            for s_ in range(NS):
                                tr(M[1][0:NE, s_ * P:(s_ + 1) * P], cwv[:, s_ * NE:(s_ + 1) * NE], ident_f[:],
                                   ["rt", "ident_f"], MK(1))
                            cp("dve", cwT[:, 0, cols], M[1][0:NE, 0:T], MK(1), ["cwT"])
                            tt("dve", cwT[:, 1, cols], M[1][0:NE, 0:T], cwT[:, 0, cols], ALU.subtract,
                               MK(1) + ["cwT"], ["cwT"])
                        sc.defer = None
                        for step in range(NT + 3):
                            for sg_ in range(3, -1, -1):
                                k = step - sg_
                                if 0 <= k < NT:
                                    sc.flush(ctasks[k][sg_])
                        sc.barrier()

                with ExitStack() as esD:
                    TM = min(TMOE, T)
                    NTM = S // TM
                    yacc = sb("yacc", [P, KC, S], F32, esD)
                    wall = sb("wall", [P, 6 * KC * DE], BF16, esD)
                    WSZ = KC * DE
                    wgs = [wall[:, i * WSZ:(i + 1) * WSZ].rearrange("p (c n) -> p c n", c=KC) for i in range(2)]
                    wus = [wall[:, (2 + i) * WSZ:(3 + i) * WSZ].rearrange("p (c n) -> p c n", c=KC) for i in range(2)]
                    wds = [wall[:, (4 + i) * WSZ:(5 + i) * WSZ].rearrange("p (j n) -> p j n", j=2) for i in range(2)]
                    xt2e = wall[:, 0:2 * KC * T].bitcast(F32).rearrange("p (c t) -> p c t", c=KC)
                    sgs = [sb("sg%d" % i, [P, 2 * TM], F32, esD) for i in range(2)]
                    actTs = [sb("actT%d" % i, [P, 2 * TM], BF16, esD) for i in range(2)]

                    def load_gu(ex):
                        i = ex % 2
                        dma("pool", wgs[i], wg_d[l, ex].rearrange("(c p) n -> p c n", p=P), [], [("wg", i)])
                        dma("pool", wus[i], wu_d[l, ex].rearrange("(c p) n -> p c n", p=P), [], [("wu", i)])

                    def load_d(ex):
                        i = ex % 2
                        dma("pool", wds[i], wd_d[l, ex].rearrange("(c p) n -> p c n", p=P), [], [("wd", i)])

                    load_gu(0)
                    load_d(0)
                    mtasks = []
                    cnt = 0
                    for ex in range(NEXP):
                        i = ex % 2
                        for t in range(NTM):
                            stg = [[] for _ in range(4)]
                            mtasks.append(stg)
                            par = cnt % 2
                            cnt += 1
                            cols = slice(t * TM, (t + 1) * TM)
                            t5 = (t * TM) // T
                            sc.defer = stg[0]
                            if t == 0 and ex + 1 < NEXP:
                                load_gu(ex + 1)
                                if ex == 0:
                                    load_d(1)
                            Gb, gk = Z[par][:, 0:2 * TM], ("Zb", 2 * par)
                            Ub, uk = Z[par][:, 512:512 + 2 * TM], ("Zb", 2 * par + 1)
                            CW, ck = WTp[:, par * 512:par * 512 + TM], ("WT", par)
                            for j in range(2):
                                for c in range(KC):
                                    mm(Gb[:, j * TM:(j + 1) * TM], wgs[i][:, c, j * P:(j + 1) * P], hT[:, c, cols],
                                       c == 0, c == KC - 1, [("wg", i), ("hT", t5)], [gk])
                            for j in range(2):
                                for c in range(KC):
                                    mm(Ub[:, j * TM:(j + 1) * TM], wus[i][:, c, j * P:(j + 1) * P], hT[:, c, cols],
                                       c == 0, c == KC - 1, [("wu", i), ("hT", t5)], [uk])
                            mm(CW, selrows[:, ex * P:(ex + 1) * P], cwT[:, 0, cols], True, False, ["selrows", "cwT"], [ck])
                            mm(CW, selrows[:, ex * P:(ex + 1) * P], cwT[:, 1, cols], False, True, ["selrows", "cwT"], [ck])
                            sg, sk = sgs[par], ("sg", par)
                            aT, ak = actTs[par], ("actT", par)
                            sc.defer = stg[1]
                            act(sg[:], Gb, AF.Silu, [gk], [sk])
                            sc.defer = stg[2]
                            tt("dve", sg[:], sg[:], Ub, ALU.mult, [sk, uk], [sk])
                            for j in range(2):
                                tt("dve", aT[:, j * TM:(j + 1) * TM], sg[:, j * TM:(j + 1) * TM], CW, ALU.mult,
                                   [sk, ck], [ak])
                            sc.defer = stg[3]
                            for dp in range(KC // 2):
                                Yb = M[dp % 2][:, 0:2 * TM]
                                yk = MK(dp % 2)
                                for dd in range(2):
                                    dc = 2 * dp + dd
                                    for j in range(2):
                                        mm(Yb[:, dd * TM:(dd + 1) * TM], wds[i][:, j, dc * P:(dc + 1) * P],
                                           aT[:, j * TM:(j + 1) * TM], j == 0, j == 1, [("wd", i), ak], yk)
                                yv = yacc[:, 2 * dp:2 * dp + 2, cols]
                                Yv = Yb.rearrange("p (a t) -> p a t", a=2)
                                if ex == 0:
                                    cp("act" if dp % 2 else "dve", yv, Yv, yk, [("ya", t5)])
                                else:
                                    tt("dve", yv, yv, Yv, ALU.add, yk + [("ya", t5)], [("ya", t5)])
                            if t == NTM - 1 and ex + 2 < NEXP:
                                load_d(ex + 2)
                    sc.defer = None
                    nt = len(mtasks)
                    for step in range(nt + 3):
                        for sg_ in range(3, -1, -1):
                            k = step - sg_
                            if 0 <= k < nt:
                                sc.flush(mtasks[k][sg_])
                    sc.barrier()

                    xbufs = [(xt, "xt"), (xt2e, "xt2e")]
                    etasks = []
                    for t in range(NT):
                        cols = slice(t * T, (t + 1) * T)
                        xb, xk = xbufs[t % 2]
                        stg = [[] for _ in range(3)]
                        etasks.append(stg)
                        sc.defer = stg[0]
                        reload(b, t, xb, xk)
                        for dc in range(KC):
                            act(xb[:, dc, :], xb[:, dc, :], AF.Copy, [xk], [xk], scale=ALPHA)
                            stt("dve", xb[:, dc, :], yacc[:, dc, cols], mod(l, b, 40 + dc), xb[:, dc, :], ALU.mult, ALU.add,
                                [("ya", t), "modT", xk], [xk])
                        sc.defer = stg[1]
                        stats([xk], xb)
                        for c in range(KC):
                            normalize(c, xb[:, c, :], lnpar(l, 2, c), lnpar(l, 3, c), [xk], [xk], None, xb)
                        sc.defer = stg[2]
                        if l + 1 < L:
                            spill(b, t, xb, xk)
                            adaln_to_hT(l + 1, b, t, 0, xt=xb, xk=xk)
                        else:
                            for s_ in range(NS):
                                tok0 = t * T + s_ * P
                                xi, xik = xins[io_cnt[0] % 2]
                                io_cnt[0] += 1
                                for half in range(2):
                                    Zt = Z[half]
                                    for c4 in range(4):
                                        c = half * 4 + c4
                                        tr(Zt[:, c4 * P:(c4 + 1) * P], xb[:, c, s_ * P:(s_ + 1) * P], ident_f[:],
                                           [xk, "ident_f"], [("Zb", 2 * half)])
                                    cp("act" if half else "dve", xi[:, half * 512:(half + 1) * 512], Zt[:, 0:512],
                                       [("Zb", 2 * half)], [xik])
                                dma("sp", out_d[b, tok0:tok0 + P, :], xi[:], [xik], [("out", b, tok0)], is_out=True)
                    sc.defer = None
                    for step in range(NT + 2):
                        for sg_ in range(2, -1, -1):
                            k = step - sg_
                            if 0 <= k < NT:
                                sc.flush(etasks[k][sg_])
                    sc.barrier()

        sc.finish()
        block = es.enter_context(nc.Block())
        sc.replay(block)
    return nc


def make_in_maps(inputs, n_cores, NB, L=2):
    f = np.float32
    x = np.ascontiguousarray(inputs["x"], dtype=f)
    c = np.asarray(inputs["c"], dtype=f)
    pos = np.asarray(inputs["positions"]).astype(np.int32)
    w_in = np.ascontiguousarray(inputs["w_in"], dtype=f)
    S = x.shape[1]
    inv = (np.float32(10000.0) ** (-np.arange(0, 32, 2, dtype=np.float32) / np.float32(32))).astype(f)
    cst = np.zeros((P, 4), f)
    cst[:, 1] = 1.0
    for i in range(32):
        cst[i, 0] = inv[i % 16]
        cst[i, 1] = -1.0 if i < 16 else 1.0
    w_kr = np.ascontiguousarray(np.concatenate(
        [w_in[:, :, 1920:1952], w_in[:, :, 1936:1952], w_in[:, :, 1920:1936]], axis=2))
    w_uq0 = np.asarray(inputs["w_uq"], dtype=f)
    wq4 = w_uq0.reshape(L, 256, 8, 96)
    zq = np.zeros((L, 256, 8, 32), f)
    w_uq = np.ascontiguousarray(np.concatenate([wq4[..., 64:96], zq, wq4[..., 0:64]], axis=-1).reshape(L, 256, 1024))
    w_uqs = np.ascontiguousarray(np.concatenate([wq4[..., 80:96], wq4[..., 64:80]], axis=-1).reshape(L, 256, 256))
    wkv4 = np.asarray(inputs["w_ukv"], dtype=f).reshape(L, 128, 8, 128)
    zk = np.zeros((L, 128, 8, 64), f)
    wk_p = np.ascontiguousarray(np.concatenate([zk, wkv4[..., 0:64]], axis=-1).reshape(L, 128, 1024))
    wv_p = np.ascontiguousarray(wkv4[..., 64:128].reshape(L, 128, 512))
    ada_bT = np.ascontiguousarray(np.asarray(inputs["ada_b"], dtype=f).reshape(L, 48, P).transpose(2, 0, 1).reshape(P, L * 48))
    lnp = np.stack([np.asarray(inputs[k], dtype=f) for k in ("ln1_g", "ln1_b", "ln2_g", "ln2_b")], axis=1)
    lnp = np.ascontiguousarray(lnp.reshape(L, 4, KC, P).transpose(3, 0, 1, 2).reshape(P, L * 4 * KC))
    qn = np.ascontiguousarray(np.asarray(inputs["q_norm"], dtype=f).reshape(L, 2, P).transpose(2, 0, 1).reshape(P, L * 2))
    kvn = np.ascontiguousarray(np.asarray(inputs["kv_norm"], dtype=f).reshape(L, P).T)
    rbias = np.ascontiguousarray(np.broadcast_to(np.asarray(inputs["router_bias"], dtype=f)[None, :], (P, NE)))
    shared = {
        "cst": cst, "ada_w": np.ascontiguousarray(inputs["ada_w"], dtype=f), "ada_bT": ada_bT,
        "w_in": w_in, "w_kr": w_kr, "qn": qn, "kvn": kvn, "w_uq": w_uq, "w_uqs": w_uqs,
        "wk_p": wk_p, "wv_p": wv_p, "w_o": np.ascontiguousarray(inputs["w_o"], dtype=f),
        "lnp": lnp, "router_w": np.ascontiguousarray(inputs["router_w"], dtype=f), "rbias": rbias,
        "w_gate": np.ascontiguousarray(inputs["w_gate"], dtype=f), "w_up": np.ascontiguousarray(inputs["w_up"], dtype=f),
        "w_down": np.ascontiguousarray(inputs["w_down"], dtype=f),
    }
    maps = []
    for ci in range(n_cores):
        sl = slice(ci * NB, (ci + 1) * NB)
        cc = c[sl]
        cT = np.ascontiguousarray(cc.reshape(NB, KC, P).transpose(2, 1, 0).reshape(P, KC * NB))
        posr = np.ascontiguousarray(np.broadcast_to(pos[sl][:, None, :], (NB, 32, S)))
        m = dict(shared)
        m.update({"x": np.ascontiguousarray(x[sl]), "cT": cT, "posr": posr})
        maps.append(m)
    return maps


_NC_CACHE = {}


def kernel(**inputs):
    n_cores = 8
    B, S, _ = inputs["x"].shape
    NB = B // n_cores
    key = (S, NB)
    if key not in _NC_CACHE:
        _NC_CACHE[key] = build_nc(S=S, NB=NB, L=2)
    nc = _NC_CACHE[key]
    maps = make_in_maps(inputs, n_cores, NB)
    res = run_bass_kernel_spmd(nc, maps, core_ids=list(range(n_cores)))
    out = np.concatenate([np.asarray(r["out"]) for r in res.results], axis=0)
    return out.astype(np.float32)
```

```python
import numpy as np
from contextlib import ExitStack
import concourse.bass as bass
import concourse.mybir as mybir
from concourse.bass_utils import run_bass_kernel_spmd

F32 = mybir.dt.float32
BF16 = mybir.dt.bfloat16
I32 = mybir.dt.int32
AF = mybir.ActivationFunctionType
ALU = mybir.AluOpType
AX = mybir.AxisListType

P = 128
D = 1024
KC = 8
NE = 32
DE = 256
IN_W = 1952
ALPHA = float((2 * 2) ** 0.25)
LN_EPS = 1e-5
RMS_EPS = 1e-6
PI = float(np.pi)
NDMA = 24
DEBUG_MG = False
DBG = set()


class Sched:
    ENG = ("pe", "act", "dve", "pool", "sp")

    def __init__(self, nc, es):
        self.nc = nc
        self.sem = {e: es.enter_context(nc.semaphore("s_" + e)) for e in self.ENG}
        self.dsem = [es.enter_context(nc.semaphore("s_dma%d" % i)) for i in range(NDMA)]
        self.cnt = {e: 0 for e in self.ENG}
        self.dcnt = [0] * NDMA
        self.drr = {"sp": 0, "pool": 0}
        self.known = {e: {} for e in self.ENG}
        self.prog = {e: [] for e in self.ENG}
        self.last_w = {}
        self.readers = {}
        self.out_tokens = []
        self.defer = None

    def _deps(self, eng, r, w):
        deps = {}

        def add(tok):
            if tok is None:
                return
            k, v = tok
            if deps.get(k, 0) < v:
                deps[k] = v

        for k in r:
            add(self.last_w.get(k))
            if isinstance(k, tuple) and k[0] in ("Zb", "WT", "M"):
                for tok in self.readers.get(k, {}).items():
                    if tok[0] != eng:
                        add(tok)
        for k in w:
            add(self.last_w.get(k))
            for tok in self.readers.get(k, {}).items():
                add(tok)
        waits = []
        kn = self.known[eng]
        for k, v in deps.items():
            if k == eng and eng == "pe":
                continue
            if kn.get(k, 0) >= v:
                continue
            kn[k] = v
            waits.append((k, v))
        return waits

    def _commit(self, tok, r, w):
        for k in w:
            self.last_w[k] = tok
            self.readers[k] = {}
        for k in r:
            if k in w:
                continue
            d = self.readers.setdefault(k, {})
            if d.get(tok[0], 0) < tok[1]:
                d[tok[0]] = tok[1]

    def flush(self, lst):
        d, self.defer = self.defer, None
        for item in lst:
            if item[0] == "dma":
                self.dma(*item[1:])
            else:
                self.op(*item)
        self.defer = d

    def op(self, eng, fn, r=(), w=()):
        if self.defer is not None:
            self.defer.append((eng, fn, tuple(r), tuple(w)))
            return
        waits = self._deps(eng, r, w)
        self.cnt[eng] += 1
        tok = (eng, self.cnt[eng])
        self.prog[eng].append((waits, fn, (eng, 1)))
        self._commit(tok, r, w)

    def dma(self, q, fn, r=(), w=(), is_out=False):
        if self.defer is not None:
            self.defer.append(("dma", q, fn, tuple(r), tuple(w), is_out))
            return
        half = NDMA // 2
        i = self.drr[q] + (0 if q == "sp" else half)
        self.drr[q] = (self.drr[q] + 1) % half
        waits = self._deps(q, r, w)
        dk = ("d", i)
        prev = self.dcnt[i]
        if prev > 0 and self.known[q].get(dk, 0) < prev:
            self.known[q][dk] = prev
            waits.append((dk, prev))
        self.dcnt[i] += 16
        tok = (dk, self.dcnt[i])
        self.prog[q].append((waits, fn, (dk, 16)))
        self._commit(tok, r, w)
        if is_out:
            self.out_tokens.append(tok)

    def barrier(self):
        for e in self.ENG:
            waits = []
            for o in self.ENG:
                if o != e and self.cnt[o] > self.known[e].get(o, 0):
                    self.known[e][o] = self.cnt[o]
                    waits.append((o, self.cnt[o]))
            for i in range(NDMA):
                dk = ("d", i)
                if self.dcnt[i] > self.known[e].get(dk, 0):
                    self.known[e][dk] = self.dcnt[i]
                    waits.append((dk, self.dcnt[i]))
            if waits:
                self.prog[e].append((waits, None, None))

    def finish(self):
        best = {}
        for (dk, v) in self.out_tokens:
            if best.get(dk, 0) < v:
                best[dk] = v
        self.prog["sp"].append((list(best.items()), None, None))

    def _semof(self, k):
        if isinstance(k, tuple):
            return self.dsem[k[1]]
        return self.sem[k]

    def replay(self, block):
        sections = {"pe": block.tensor, "act": block.scalar, "dve": block.vector,
                    "pool": block.gpsimd, "sp": block.sync}
        for en in self.ENG:
            prog = self.prog[en]

            def body(e, prog=prog):
                for waits, fn, inc in prog:
                    for (k, v) in waits:
                        e.wait_ge(self._semof(k), v)
                    if fn is not None:
                        ins = fn(e)
                        ins.then_inc(self._semof(inc[0]), inc[1])

            sections[en](body)


def build_nc(S=2048, NB=2, L=2, TQ=512, KCH=1024, NEXP=NE, KSB=512, KML=1024, TMOE=256):
    T = min(TQ, S)
    NT = S // T
    NS = T // P
    TT = S // P
    nc = bass.Bass("TRN2", target_bir_lowering=False)

    def din(name, shape, dt=F32):
        return nc.dram_tensor(name, list(shape), dt, kind="ExternalInput").ap()

    x_d = din("x", [NB, S, D])
    cT_d = din("cT", [P, KC * NB])
    posr_d = din("posr", [NB, 32, S], I32)
    cst_d = din("cst", [P, 4])
    ada_w_d = din("ada_w", [L, D, 6 * D])
    ada_bT_d = din("ada_bT", [P, L * 48])
    w_in_d = din("w_in", [L, D, IN_W])
    w_kr_d = din("w_kr", [L, D, 64])
    qn_d = din("qn", [P, L * 2])
    kvn_d = din("kvn", [P, L])
    w_uq_d = din("w_uq", [L, 256, 1024])
    w_uqs_d = din("w_uqs", [L, 256, 256])
    wk_d = din("wk_p", [L, 128, 1024])
    wv_d = din("wv_p", [L, 128, 512])
    w_o_d = din("w_o", [L, D, D])
    lnp_d = din("lnp", [P, L * 4 * KC])
    rw_d = din("router_w", [D, NE])
    rb_d = din("rbias", [P, NE])
    wg_d = din("w_gate", [L, NE, D, DE])
    wu_d = din("w_up", [L, NE, D, DE])
    wd_d = din("w_down", [L, NE, DE, D])
    out_d = nc.dram_tensor("out", [NB, S, D], F32, kind="ExternalOutput").ap()
    scr_d = nc.dram_tensor("xscr", [NB, P, KC, S], F32, kind="Internal").ap()
    dbg_d = nc.dram_tensor("dbg_mg", [P, KC, S], BF16, kind="ExternalOutput").ap() if DEBUG_MG else None

    with ExitStack() as es:
        sc = Sched(nc, es)

        sb_cache = {}

        def sb(name, shape, dt, stack=es):
            if name not in sb_cache:
                sb_cache[name] = stack.enter_context(nc.sbuf_tensor("sb_" + name, list(shape), dt))
            return sb_cache[name]

        Z = [es.enter_context(nc.psum_tensor("Z%d" % i, [P, 1024], F32)) for i in range(2)]
        WTp = es.enter_context(nc.psum_tensor("WTp", [P, 1024], F32))
        M = [es.enter_context(nc.psum_tensor("M%d" % i, [P, 512], F32)) for i in range(2)]
        WTb = WTp[:, :].bitcast(BF16)

        def MK(i, a=0, b=512):
            return [("M", i)]

        def mm(out, lhsT, rhs, start, stop, r, w):
            sc.op("pe", lambda e: e.matmul(out, lhsT=lhsT, rhs=rhs, start=start, stop=stop), r, w)

        def tr(out, in_, ident, r, w):
            sc.op("pe", lambda e: e.transpose(out, in_, ident), r, w)

        def act(out, in_, func, r, w, bias=None, scale=None, accum_out=None):
            kw = {}
            if bias is not None:
                kw["bias"] = bias
            if scale is not None:
                kw["scale"] = scale
            if accum_out is not None:
                kw["accum_out"] = accum_out
            sc.op("act", lambda e: e.activation(out, in_, func, **kw), r, w)

        def tt(eng, out, in0, in1, op, r, w):
            sc.op(eng, lambda e: e.tensor_tensor(out, in0, in1, op), r, w)

        def ts(eng, out, in0, s1, s2, op0, op1, r, w):
            if s2 is None:
                sc.op(eng, lambda e: e.tensor_scalar(out, in0, s1, None, op0), r, w)
            else:
                sc.op(eng, lambda e: e.tensor_scalar(out, in0, s1, s2, op0, op1), r, w)

        def stt(eng, out, in0, scalar, in1, op0, op1, r, w):
            sc.op(eng, lambda e: e.scalar_tensor_tensor(out, in0, scalar, in1, op0, op1), r, w)

        def cp(eng, out, in_, r, w):
            if eng == "act":
                sc.op("act", lambda e: e.activation(out, in_, AF.Copy), r, w)
            else:
                sc.op(eng, lambda e: e.tensor_copy(out, in_), r, w)

        def dma(q, out, in_, r, w, is_out=False):
            sc.dma(q, lambda e: e.dma_start(out=out, in_=in_), r, w, is_out=is_out)

        ident_f = sb("ident_f", [P, P], F32)
        ident_b = sb("ident_b", [P, P], BF16)
        ones_f = sb("ones_f", [P, P], F32)
        mask_s = sb("mask_s", [P, P], F32)
        mask_sb = sb("mask_sb", [P, P], BF16)
        negm = sb("negm", [P, P], F32)
        maskT = sb("maskT", [P, P], BF16)
        ones_b = sb("ones_b", [P, P], BF16)
        scan1 = sb("scan1", [P, KCH], F32)
        selrows = sb("selrows", [32, NE * P], BF16)
        cst = sb("cst", [P, 4], F32)
        modT = sb("modT", [P, L * NB * 48], F32)
        ada_bT = sb("ada_bT", [P, L * 48], F32)
        lnp = sb("lnp", [P, L * 4 * KC], F32)
        qn = sb("qn", [P, L * 2], F32)
        kvn = sb("kvn", [P, L], F32)
        rw = sb("rw", [P, KC, NE], F32)
        rb = sb("rb", [P, NE], F32)
        rbt = sb("rbt", [P, NS * NE], F32)
        cact = sb("cact", [P, KC * NB], F32)
        hT = sb("hT", [P, KC, S], BF16)
        xt = sb("xt", [P, KC, T], F32)
        tmpA = sb("tmpA", [P, T], F32)
        tmpB = sb("tmpB", [P, T], F32)
        tmpC = sb("tmpC", [P, T], F32)
        tmpD = sb("tmpD", [P, T], F32)
        cb16 = [sb("cb16_%d" % i, [P, T], BF16) for i in range(2)]
        sq16 = [sb("sq16_%d" % i, [P, T], BF16) for i in range(2)]
        cwT = sb("cwT", [32, 2, S], BF16)
        rope = sb("rope", [P, 2, S], BF16)
        xin = sb("xin", [P, D], F32)
        xin2 = sb("xin2", [P, D], F32)
        xins = [(xin, "xin0"), (xin2, "xin1")]
        io_cnt = [0]

        def mod(l, b, j):
            o = (l * NB + b) * 48 + j
            return modT[:, o:o + 1]

        def lnpar(l, which, c):
            o = (l * 4 + which) * KC + c
            return lnp[:, o:o + 1]

        sc.op("pool", lambda e: e.memset(ident_f[:], 1.0), w=["ident_f"])
        sc.op("pool", lambda e: e.affine_select(out=ident_f[:], in_=ident_f[:], pattern=[[-1, P]],
                                                compare_op=ALU.is_equal, fill=0.0, base=0,
                                                channel_multiplier=1), r=["ident_f"], w=["ident_f"])
        cp("pool", ident_b[:], ident_f[:], ["ident_f"], ["ident_b"])
        sc.op("pool", lambda e: e.memset(ones_f[:], 1.0), w=["ones_f"])
        sc.op("pool", lambda e: e.memset(scan1[:], 1.0), w=["scan1"])
        sc.op("pool", lambda e: e.memset(mask_s[:], 1.0), w=["mask_s"])
        sc.op("pool", lambda e: e.affine_select(out=mask_s[:], in_=mask_s[:], pattern=[[-1, P]],
                                                compare_op=ALU.is_gt, fill=0.0, base=0,
                                                channel_multiplier=1), r=["mask_s"], w=["mask_s"])
        cp("pool", mask_sb[:], mask_s[:], ["mask_s"], ["mask_sb"])
        sc.op("pool", lambda e: e.memset(negm[:], 0.0), w=["negm"])
        sc.op("pool", lambda e: e.affine_select(out=negm[:], in_=negm[:], pattern=[[-1, P]],
                                                compare_op=ALU.is_ge, fill=-30000.0, base=0,
                                                channel_multiplier=1), r=["negm"], w=["negm"])
        ts("dve", maskT[:], mask_s[:], -1.0, 1.0, ALU.mult, ALU.add, ["mask_s"], ["maskT"])
        cp("dve", ones_b[:], ones_f[:], ["ones_f"], ["ones_b"])
        sc.op("pool", lambda e: e.memset(selrows[:], 1.0), w=["selrows"])
        sc.op("pool", lambda e: e.affine_select(
            out=selrows[:].rearrange("k (e m) -> k e m", m=P), in_=selrows[:].rearrange("k (e m) -> k e m", m=P),
            pattern=[[-1, NE], [0, P]], compare_op=ALU.is_equal, fill=0.0, base=0,
            channel_multiplier=1), r=["selrows"], w=["selrows"])

        dma("sp", cst[:], cst_d, [], ["cst"])
        dma("sp", ada_bT[:], ada_bT_d, [], ["ada_bT"])
        dma("sp", lnp[:], lnp_d, [], ["lnp"])
        dma("sp", qn[:], qn_d, [], ["qn"])
        dma("sp", kvn[:], kvn_d, [], ["kvn"])
        dma("sp", rw[:], rw_d.rearrange("(c p) n -> p c n", p=P), [], ["rw"])
        dma("sp", rb[:], rb_d, [], ["rb"])
        for s_ in range(NS):
            dma("sp", rbt[:, s_ * NE:(s_ + 1) * NE], rb_d, [], ["rbt"])
        dma("sp", cact[:], cT_d, [], ["cact"])
        act(tmpA[:, 0:KC * NB], cact[:], AF.Exp, ["cact"], ["tmpA"], scale=-1.0)
        ts("dve", tmpA[:, 0:KC * NB], tmpA[:, 0:KC * NB], 1.0, None, ALU.add, None, ["tmpA"], ["tmpA"])
        sc.op("dve", lambda e: e.reciprocal(tmpA[:, 0:KC * NB], tmpA[:, 0:KC * NB]), ["tmpA"], ["tmpA"])
        tt("dve", cact[:], cact[:], tmpA[:, 0:KC * NB], ALU.mult, ["cact", "tmpA"], ["cact"])

        with ExitStack() as es0:
            awst = [sb("awst%d" % i, [P, KC, 512], BF16, es0) for i in range(3)]
            cact16 = sb("cact16", [P, KC * NB], BF16, es0)
            cp("dve", cact16[:], cact[:], ["cact"], ["cact16"])
            gi = 0
            for l in range(L):
                for jg in range(12):
                    bufi = gi % 3
                    gi += 1
                    aw = awst[bufi]
                    dma("pool", aw[:], ada_w_d[l, :, jg * 512:(jg + 1) * 512].rearrange("(c p) n -> p c n", p=P),
                        [], [("awst", bufi)])
                    for jj in range(4):
                        j = jg * 4 + jj
                        for kc in range(KC):
                            mm(M[jj % 2][:, 0:NB], aw[:, kc, jj * P:(jj + 1) * P],
                               cact16[:, kc * NB:(kc + 1) * NB], kc == 0, kc == KC - 1,
                               [("awst", bufi), "cact16"], MK(jj % 2))
                        for b in range(NB):
                            ts("dve", mod(l, b, j), M[jj % 2][:, b:b + 1], ada_bT[:, l * 48 + j:l * 48 + j + 1], None,
                               ALU.add, None, MK(jj % 2) + ["ada_bT"], ["modT"])
                for b in range(NB):
                    for (lo, hi) in ((8, 24), (32, 48)):
                        o = (l * NB + b) * 48
                        ts("dve", modT[:, o + lo:o + hi], modT[:, o + lo:o + hi], 1.0, None, ALU.add, None,
                           ["modT"], ["modT"])
            sc.barrier()

        def stats(src_keys, xt=xt):
            for c in range(KC):
                i = c % 2
                cp("dve", cb16[i][:], xt[:, c, :], src_keys, [("cb16", i)])
                mm(M[0][:, 0:T], ones_b[:], cb16[i][:], c == 0, c == KC - 1, [("cb16", i), "ones_b"], MK(0))
                act(sq16[i][:], xt[:, c, :], AF.Square, src_keys, [("sq16", i)])
                mm(M[1][:, 0:T], ones_b[:], sq16[i][:], c == 0, c == KC - 1, [("sq16", i), "ones_b"], MK(1))
            ts("dve", tmpA[:], M[0][:, 0:T], 1.0 / D, None, ALU.mult, None, MK(0), ["tmpA"])
            tt("dve", tmpC[:], tmpA[:], tmpA[:], ALU.mult, ["tmpA"], ["tmpC"])
            stt("dve", tmpC[:], M[1][:, 0:T], 1.0 / D, tmpC[:], ALU.mult, ALU.subtract, MK(1) + ["tmpC"], ["tmpC"])
            ts("dve", tmpC[:], tmpC[:], LN_EPS, None, ALU.add, None, ["tmpC"], ["tmpC"])
            act(tmpC[:], tmpC[:], AF.Ln, ["tmpC"], ["tmpC"])
            act(tmpB[:], tmpC[:], AF.Exp, ["tmpC"], ["tmpB"], scale=-0.5)

        def normalize(c, out_ap, scale_ap, bias_ap, src_keys, out_keys, second_out=None, xt=xt):
            tbuf, tk = (tmpD, "tmpD") if c % 2 == 0 else (tmpC, "tmpC")
            tt("dve", tbuf[:], xt[:, c, :], tmpA[:], ALU.subtract, src_keys + ["tmpA"], [tk])
            tt("dve", tbuf[:], tbuf[:], tmpB[:], ALU.mult, [tk, "tmpB"], [tk])
            act(out_ap, tbuf[:], AF.Identity, [tk, "modT", "lnp"], out_keys, bias=bias_ap, scale=scale_ap)
            if second_out is not None:
                o2, k2 = second_out
                act(o2, tbuf[:], AF.Identity, [tk, "modT", "lnp"], k2, bias=bias_ap, scale=scale_ap)

        def adaln_to_hT(l, b, t, which, second=None, xt=xt, xk="xt"):
            stats([xk], xt)
            base = 0 if which == 0 else 24
            for c in range(KC):
                so = None
                if second is not None:
                    so = (second[:, c, :], ["h2f"])
                normalize(c, hT[:, c, t * T:(t + 1) * T], mod(l, b, base + 8 + c), mod(l, b, base + c),
                          [xk], [("hT", t)], so, xt)

        def spill(b, t, xt=xt, xk="xt"):
            dma("sp", scr_d[b, :, :, t * T:(t + 1) * T], xt[:], [xk], [("scr", b, t)])

        def reload(b, t, xt=xt, xk="xt"):
            dma("sp", xt[:], scr_d[b, :, :, t * T:(t + 1) * T], [("scr", b, t)], [xk])

        for b in range(NB):
            with ExitStack() as esr:
                posi = sb("posi", [P, S], I32, esr)
                ang = sb("ang", [P, S], F32, esr)
                ang2 = sb("ang2", [P, S], F32, esr)
                kf = sb("kf", [P, S], F32, esr)
                R = slice(0, 32)
                dma("sp", posi[R, :], posr_d[b], [], ["posi"])
                cp("dve", ang[R, :], posi[R, :], ["posi"], ["ang"])
                ts("dve", ang[R, :], ang[R, :], cst[R, 0:1], None, ALU.mult, None, ["ang", "cst"], ["ang"])
                C1 = 6.28125
                C2 = 2.0 * PI - C1
                for which, shift in ((0, 0.5 * PI), (1, 0.0)):
                    if shift != 0.0:
                        ts("dve", ang2[R, :], ang[R, :], shift, None, ALU.add, None, ["ang"], ["ang2"])
                    else:
                        cp("dve", ang2[R, :], ang[R, :], ["ang"], ["ang2"])
                    ts("dve", posi[R, :], ang2[R, :], 1.0 / (2.0 * PI), None, ALU.mult, None, ["ang2"], ["posi"])
                    cp("dve", kf[R, :], posi[R, :], ["posi"], ["kf"])
                    stt("dve", ang2[R, :], kf[R, :], -C1, ang2[R, :], ALU.mult, ALU.add, ["kf", "ang2"], ["ang2"])
                    stt("dve", ang2[R, :], kf[R, :], -C2, ang2[R, :], ALU.mult, ALU.add, ["kf", "ang2"], ["ang2"])
                    ts("dve", kf[R, :], ang2[R, :], PI, -2.0 * PI, ALU.is_gt, ALU.mult, ["ang2"], ["kf"])
                    tt("dve", ang2[R, :], ang2[R, :], kf[R, :], ALU.add, ["ang2", "kf"], ["ang2"])
                    ts("dve", ang2[R, :], ang2[R, :], -3.1415925, 3.1415925, ALU.max, ALU.min, ["ang2"], ["ang2"])
                    act(ang2[R, :], ang2[R, :], AF.Sin, ["ang2"], ["ang2"])
                    if which == 0:
                        cp("dve", rope[R, 0, :], ang2[R, :], ["ang2"], ["rope"])
                    else:
                        ts("dve", rope[R, 1, :], ang2[R, :], cst[R, 1:2], None, ALU.mult, None,
                           ["ang2", "cst"], ["rope"])
                sc.barrier()

            for l in range(L):
                if l == 0:
                    for t in range(NT):
                        for s_ in range(NS):
                            tok0 = t * T + s_ * P
                            xi, xik = xins[io_cnt[0] % 2]
                            io_cnt[0] += 1
                            dma("sp", xi[:], x_d[b, tok0:tok0 + P, :], [], [xik])
                            for half in range(2):
                                Zt = Z[half]
                                for c4 in range(4):
                                    c = half * 4 + c4
                                    tr(Zt[:, c4 * P:(c4 + 1) * P], xi[:, c * P:(c + 1) * P], ident_f[:],
                                       [xik, "ident_f"], [("Zb", 2 * half)])
                                for c4 in range(4):
                                    c = half * 4 + c4
                                    cp("act" if c4 % 2 else "dve", xt[:, c, s_ * P:(s_ + 1) * P],
                                       Zt[:, c4 * P:(c4 + 1) * P], [("Zb", 2 * half)], ["xt"])
                        spill(b, t)
                        adaln_to_hT(l, b, t, 0)
                    sc.barrier()

                with ExitStack() as esBC:
                    mergedT = sb("mergedT", [P, KC, S], BF16, esBC)
                    with ExitStack() as esB:
                        QT = sb("QT", [P, 2, S], BF16, esB)
                        KT = sb("KT", [P, 2, S], BF16, esB)
                        Vp = sb("Vp", [P, TT, P], BF16, esB)
                        lat = sb("lat", [P, 4096], F32, esB)
                        uT = lat[:, 0:S].bitcast(BF16).rearrange("p (c s) -> p c s", c=2)
                        ukvT = lat[:, 2048:2048 + S // 2].bitcast(BF16)
                        krT = lat[:, 3072:3072 + S // 2].bitcast(BF16)
                        wst = [sb("wst%d" % i, [P, KC, 384], BF16, esB) for i in range(2)]
                        wuq = sb("wuq", [P, 2, 1024], BF16, esB)
                        wuqs = sb("wuqs", [P, 2, 256], BF16, esB)
                        wk = sb("wk", [P, 1024], BF16, esB)
                        wv = sb("wv", [P, 512], BF16, esB)
                        kst = sb("kst", [P, 16], F32, esB)
                        nball = lat[:, 0:2048]
                        bball = lat[:, 2048:4096]
                        dbuf = [sb("dbuf%d" % i, [P, P], F32, esB) for i in range(4)]
                        wrall = sb("wrall", [P, 2048], BF16, esB)
                        wtsall = sb("wtsall", [P, 2048], BF16, esB)
                        opair = [sb("opair%d" % i, [P, P], BF16, esB) for i in range(2)]
                        rcs = [sb("rcs%d" % i, [P, 8], F32, esB) for i in range(8)]
                        rrs = [sb("rrs%d" % i, [P, 16], F32, esB) for i in range(4)]
                        state = {"row": 0, "wst": 0, "zc": 0, "oc": 0, "rc": 0}

                        def load_w(col_specs):
                            i = state["wst"] % 2
                            state["wst"] += 1
                            off = 0
                            for (src, ncols) in col_specs:
                                dma("pool", wst[i][:, :, off:off + ncols], src.rearrange("(c p) n -> p c n", p=P),
                                    [], [("wst", i)])
                                off += ncols
                            return wst[i], ("wst", i)

                        def proj_T(dst_fn, w_ap, wkey, ncolsM, scale=None, dst_keys=()):
                            for t in range(NT):
                                bi = t % 4
                                ps = Z[bi // 2][0:ncolsM, (bi % 2) * 512:(bi % 2) * 512 + T]
                                for c in range(KC):
                                    mm(ps, w_ap[:, c, :], hT[:, c, t * T:(t + 1) * T], c == 0, c == KC - 1,
                                       [wkey, ("hT", t)], [("Zb", bi)])
                                dst_fn(t, ps, ("Zb", bi))

                        NSTG = 7

                        def attn_rows(kind, hh, Kdim, pbase, qidx, vcol0, mcol, opi, tasks):
                            sm_scale = (64 + 32) ** -0.5
                            CH = KSB if kind == "sb" else KML
                            units = 1 if kind == "sb" else 2
                            for qb in range(TT):
                                F = (qb + 1) * P
                                q_ap = QT[pbase:pbase + Kdim, qidx, qb * P:(qb + 1) * P]
                                rr = rrs[state["row"] % 4]
                                rrk = ("rrs", state["row"] % 4)
                                state["row"] += 1
                                chunks = []
                                f1 = F
                                while f1 > 0:
                                    f0 = max(0, f1 - CH)
                                    chunks.append((f0, f1))
                                    f1 = f0
                                nch = len(chunks)
                                crec = []
                                if kind == "sb":
                                    osl = state["oc"] % 2
                                    state["oc"] += 1
                                for ci, (f0, f1) in enumerate(chunks):
                                    n = f1 - f0
                                    if kind == "sb":
                                        u = state["zc"] % 4
                                        state["zc"] += 1
                                        ukeys = [u]
                                    else:
                                        u = 2 * (state["zc"] % 2)
                                        state["zc"] += 1
                                        ukeys = [u, u + 1]
                                    c0 = u * 512
                                    zt = Z[u // 2][:, (u % 2) * 512:(u % 2) * 512 + units * 512]
                                    zk = [("Zb", k) for k in ukeys]
                                    nb, nk = nball[:, c0:c0 + units * 512], [("nb", k) for k in ukeys]
                                    bb, bk = bball[:, c0:c0 + units * 512], [("bb", k) for k in ukeys]
                                    db, dk = dbuf[u], [("dbuf", u)]
                                    wr, wrk = wrall[:, c0:c0 + units * 512], [("wr", k) for k in ukeys]
                                    wti = (u // units) % 2
                                    wt, wtk = WTb[:, wti * 1024:(wti + 1) * 1024], [("WT", wti)]
                                    wts, wtsk = wtsall[:, c0:c0 + units * 512], [("wts", k) for k in ukeys]
                                    rci = state["rc"] % 8
                                    state["rc"] += 1
                                    rc, rck = rcs[rci], [("rcs", rci)]
                                    stg = [[] for _ in range(NSTG)]
                                    tasks.append(stg)
                                    sc.defer = stg[0]
                                    for g0 in range(f0, f1, 512):
                                        g1 = min(f1, g0 + 512)
                                        mm(zt[:, g0 - f0:g1 - f0], q_ap, KT[pbase:pbase + Kdim, qidx, g0:g1],
                                           True, True, ["QT", "KT"], [("Zb", u + (g0 - f0) // 512)])
                                    lo_n = n
                                    if kind == "sb":
                                        sc.defer = stg[1]
                                        act(nb[:, 0:n], zt[:, 0:n], AF.Exp, zk, nk, scale=-1.0)
                                        act(nb[:, 0:n], nb[:, 0:n], AF.Ln, nk, nk, bias=1.0)
                                        sc.defer = stg[2]
                                        if ci == 0:
                                            d0 = n - P
                                            lo_n = d0
                                            tt("dve", db[:], zt[:, d0:n], nb[:, d0:n], ALU.add, zk + nk, dk)
                                            tt("dve", db[:], db[:], mask_s[:], ALU.mult, dk + ["mask_s"], dk)
                                            sc.op("dve", lambda e, bb=bb, db=db, d0=d0, n=n: e.tensor_tensor_scan(
                                                out=bb[:, d0:n][:, ::-1], data0=scan1[:, 0:P], data1=db[:, ::-1],
                                                initial=0.0, op0=ALU.mult, op1=ALU.add),
                                                dk + ["scan1"], bk)
                                            cp("dve", rr[:, 0:1], bb[:, d0:d0 + 1], bk, [rrk])
                                            tt("dve", bb[:, d0:n], bb[:, d0:n], zt[:, d0:n], ALU.subtract, bk + zk, bk)
                                        if lo_n > 0:
                                            tt("dve", bb[:, lo_n - 1:lo_n], rr[:, 0:1], nb[:, lo_n - 1:lo_n], ALU.add,
                                               [rrk] + nk, bk)
                                            if lo_n > 1:
                                                sc.op("dve", lambda e, zt=zt, nb=nb, bb=bb, lo_n=lo_n: e.tensor_tensor_scan(
                                                    out=bb[:, 0:lo_n - 1][:, ::-1], data0=zt[:, 1:lo_n][:, ::-1],
                                                    data1=nb[:, 0:lo_n - 1][:, ::-1], initial=bb[:, lo_n - 1:lo_n],
                                                    op0=ALU.add, op1=ALU.add),
                                                    zk + nk + bk, bk)
                                        if ci + 1 < nch:
                                            tt("dve", rr[:, 0:1], bb[:, 0:1], zt[:, 0:1], ALU.add, bk + zk, [rrk])
                                        sc.defer = stg[3]
                                        act(wr[:, 0:n], bb[:, 0:n], AF.Exp, bk, wrk, scale=-1.0)
                                        if ci == 0:
                                            tt("pool", wr[:, n - P:n], wr[:, n - P:n], mask_sb[:], ALU.mult,
                                               wrk + ["mask_sb"], wrk)
                                    else:
                                        sc.defer = stg[1]
                                        sc.op("dve", lambda e, rc=rc, zt=zt, n=n: e.reduce_max(rc[:, 0:1], zt[:, 0:n], AX.X),
                                              zk, rck)
                                        ts("dve", rc[:, 1:2], rc[:, 0:1], -sm_scale, None, ALU.mult, None, rck, rck)
                                        sc.op("dve", lambda e, rc=rc: e.memset(rc[:, 2:4], 0.0), [], rck)
                                        if ci == 0:
                                            d0 = n - P
                                            lo_n = d0
                                            tt("dve", db[:], zt[:, d0:n], negm[:], ALU.add, zk + ["negm"], dk)
                                        sc.defer = stg[2]
                                        if ci == 0:
                                            act(wr[:, d0:n], db[:], AF.Exp, dk + rck, wrk + rck,
                                                bias=rc[:, 1:2], scale=sm_scale, accum_out=rc[:, 2:3])
                                        if lo_n > 0:
                                            act(wr[:, 0:lo_n], zt[:, 0:lo_n], AF.Exp, zk + rck, wrk + rck,
                                                bias=rc[:, 1:2], scale=sm_scale, accum_out=rc[:, 3:4])
                                        tt("dve", rc[:, 4:5], rc[:, 2:3], rc[:, 3:4], ALU.add, rck, rck)
                                        osl = state["oc"] % 2
                                        state["oc"] += 1
                                    sc.defer = stg[4]
                                    nblk = n // P
                                    for jj in range(nblk):
                                        tr(wt[:, jj * P:(jj + 1) * P], wr[:, jj * P:(jj + 1) * P], ident_b[:],
                                           wrk + ["ident_b"], wtk)
                                    sc.defer = stg[5]
                                    cp("dve", wts[:, 0:n], wt[:, 0:n], wtk, wtsk)
                                    sc.defer = stg[6]
                                    ops = M[osl][:, 0:64]
                                    opk = MK(osl)
                                    for jj in range(nblk):
                                        if kind == "sb":
                                            st = (ci == 0 and jj == 0)
                                            sp_ = (ci == nch - 1 and jj == nblk - 1)
                                        else:
                                            st = (jj == 0)
                                            sp_ = (jj == nblk - 1)
                                        mm(ops, wts[:, jj * P:(jj + 1) * P], Vp[:, f0 // P + jj, vcol0:vcol0 + 64], st, sp_,
                                           wtsk + ["Vp"], opk)
                                    crec.append((ops, opk, rc, rck))
                                op_ = opair[opi[0] % 2]
                                ok_ = ("opair", opi[0] % 2)
                                dst = op_[:, 64 * hh:64 * hh + 64]
                                if kind == "sb":
                                    ops, opk, _, _ = crec[-1]
                                    cp("dve", dst, ops, opk, [ok_])
                                elif nch == 1:
                                    ops, opk, rc, rck = crec[0]
                                    sc.op("dve", lambda e, rc=rc: e.reciprocal(rc[:, 5:6], rc[:, 4:5]), rck, rck)
                                    ts("dve", dst, ops, rc[:, 5:6], None, ALU.mult, None, opk + rck, [ok_])
                                else:
                                    assert nch == 2
                                    (o0, ok0, r0, rk0), (o1, ok1, r1, rk1) = crec
                                    tt("dve", rr[:, 2:3], r0[:, 0:1], r1[:, 0:1], ALU.max, rk0 + rk1, [rrk])
                                    ts("dve", rr[:, 3:4], rr[:, 2:3], -sm_scale, None, ALU.mult, None, [rrk], [rrk])
                                    act(rr[:, 4:5], r0[:, 0:1], AF.Exp, rk0 + [rrk], [rrk], bias=rr[:, 3:4], scale=sm_scale)
                                    act(rr[:, 5:6], r1[:, 0:1], AF.Exp, rk1 + [rrk], [rrk], bias=rr[:, 3:4], scale=sm_scale)
                                    tt("dve", rr[:, 6:7], rr[:, 4:5], r0[:, 4:5], ALU.mult, [rrk] + rk0, [rrk])
                                    stt("dve", rr[:, 7:8], rr[:, 5:6], r1[:, 4:5], rr[:, 6:7], ALU.mult, ALU.add,
                                        [rrk] + rk1, [rrk])
                                    sc.op("dve", lambda e, rr=rr: e.reciprocal(rr[:, 8:9], rr[:, 7:8]), [rrk], [rrk])
                                    ts("dve", rr[:, 4:6], rr[:, 4:6], rr[:, 8:9], None, ALU.mult, None, [rrk], [rrk])
                                    ts("dve", dst, o0, rr[:, 4:5], None, ALU.mult, None, ok0 + [rrk], [ok_])
                                    stt("dve", dst, o1, rr[:, 5:6], dst, ALU.mult, ALU.add, ok1 + [rrk, ok_], [ok_])
                                if hh == 1:
                                    finish_pair(qb, op_, ok_, mcol)
                                sc.defer = None
                                yield qb

                        def finish_pair(qb, op_, ok_, mcol):
                            k = state["oc"] % 2
                            state["oc"] += 1
                            tps = M[k][:, 0:64].bitcast(BF16)
                            tr(tps, op_[:], ident_b[:], [ok_, "ident_b"], MK(k))
                            cp("dve", mergedT[:, mcol, qb * P:(qb + 1) * P], tps, MK(k), [("mg", mcol)])

                        def run_pair(kind, Kdim, pbases, qidxs, vcols, mcol):
                            opi = [0]
                            tasks = []
                            g0 = attn_rows(kind, 0, Kdim, pbases[0], qidxs[0], vcols[0], mcol, opi, tasks)
                            g1 = attn_rows(kind, 1, Kdim, pbases[1], qidxs[1], vcols[1], mcol, opi, tasks)
                            for _ in range(TT):
                                next(g0)
                                next(g1)
                                opi[0] += 1
                            nt = len(tasks)
                            for step in range(nt + NSTG - 1):
                                for sg_ in range(NSTG - 1, -1, -1):
                                    k = step - sg_
                                    if 0 <= k < nt:
                                        sc.flush(tasks[k][sg_])

                        for pr in range(4):
                            wt_, wkey = load_w([(w_in_d[l, :, pr * P:(pr + 1) * P], P),
                                                (w_in_d[l, :, 512 + pr * P:512 + (pr + 1) * P], P),
                                                (w_in_d[l, :, 1024 + pr * P:1024 + (pr + 1) * P], P)])

                            def put_q(t, ps, zk):
                                act(QT[:, 0, t * T:(t + 1) * T], ps, AF.Copy, [zk], ["QT"], scale=0.125)

                            def put_k(t, ps, zk):
                                cp("dve", KT[:, 0, t * T:(t + 1) * T], ps, [zk], ["KT"])

                            proj_T(put_q, wt_[:, :, 0:P], wkey, P)
                            proj_T(put_k, wt_[:, :, P:2 * P], wkey, P)
                            for j in range(TT):
                                t = (j * P) // T
                                ps = M[j % 2][:, 0:P]
                                pk = MK(j % 2, 0, P)
                                for c in range(KC):
                                    mm(ps, hT[:, c, j * P:(j + 1) * P], wt_[:, c, 2 * P:3 * P], c == 0, c == KC - 1,
                                       [wkey, ("hT", t)], pk)
                                cp("act" if j % 2 else "dve", Vp[:, j, :], ps, pk, ["Vp"])
                            run_pair("sb", 64, (0, 64), (0, 0), (0, 64), pr)

                        sc.barrier()
                        wt_, wkey = load_w([(w_in_d[l, :, 1536:1792], 256)])
                        wt2_, wkey2 = load_w([(w_in_d[l, :, 1792:1920], 128), (w_kr_d[l, :, 0:64], 64)])
                        dma("pool", wuq[:], w_uq_d[l].rearrange("(c p) n -> p c n", p=P), [], ["wuq"])
                        dma("pool", wuqs[:], w_uqs_d[l].rearrange("(c p) n -> p c n", p=P), [], ["wuqs"])
                        dma("pool", wk[:], wk_d[l], [], ["wk"])
                        dma("pool", wv[:], wv_d[l], [], ["wv"])
                        R = slice(0, 32)
                        for t in range(NT if "nolat" not in DBG else 0):
                            cols = slice(t * T, (t + 1) * T)
                            for c2 in range(2):
                                ps = Z[0][:, c2 * 512:c2 * 512 + T]
                                for c in range(KC):
                                    mm(ps, wt_[:, c, c2 * P:(c2 + 1) * P], hT[:, c, cols], c == 0, c == KC - 1,
                                       [wkey, ("hT", t)], [("Zb", c2)])
                                act(tmpA[:] if c2 == 0 else tmpB[:], ps, AF.Square, [("Zb", c2)],
                                    ["tmpA" if c2 == 0 else "tmpB"])
                            mm(M[0][:, 0:T], ones_f[:], tmpA[:], True, False, ["tmpA", "ones_f"], MK(0))
                            mm(M[0][:, 0:T], ones_f[:], tmpB[:], False, True, ["tmpB", "ones_f"], MK(0))
                            act(tmpC[:], M[0][:, 0:T], AF.Ln, MK(0), ["tmpC"], bias=RMS_EPS, scale=1.0 / 256)
                            act(tmpC[:], tmpC[:], AF.Exp, ["tmpC"], ["tmpC"], scale=-0.5)
                            for c2 in range(2):
                                stt("dve", uT[:, c2, cols], Z[0][:, c2 * 512:c2 * 512 + T], qn[:, l * 2 + c2:l * 2 + c2 + 1],
                                    tmpC[:], ALU.mult, ALU.mult, [("Zb", c2), "qn", "tmpC"], ["uT"])
                            ps = Z[1][:, 0:T]
                            for c in range(KC):
                                mm(ps, wt2_[:, c, 0:P], hT[:, c, cols], c == 0, c == KC - 1, [wkey2, ("hT", t)], [("Zb", 2)])
                            act(tmpA[:], ps, AF.Square, [("Zb", 2)], ["tmpA"])
                            mm(M[1][:, 0:T], ones_f[:], tmpA[:], True, True, ["tmpA", "ones_f"], MK(1))
                            act(tmpD[:], M[1][:, 0:T], AF.Ln, MK(1), ["tmpD"], bias=RMS_EPS, scale=1.0 / 128)
                            act(tmpD[:], tmpD[:], AF.Exp, ["tmpD"], ["tmpD"], scale=-0.5)
                            stt("dve", ukvT[:, cols], ps, kvn[:, l:l + 1], tmpD[:], ALU.mult, ALU.mult,
                                [("Zb", 2), "kvn", "tmpD"], ["ukvT"])
                            psA = Z[1][0:32, 512:512 + T]
                            for c in range(KC):
                                mm(psA, wt2_[:, c, P:P + 32], hT[:, c, cols], c == 0, c == KC - 1, [wkey2, ("hT", t)], [("Zb", 3)])
                            psB = M[0][0:32, 0:T]
                            for c in range(KC):
                                mm(psB, wt2_[:, c, P + 32:P + 64], hT[:, c, cols], c == 0, c == KC - 1, [wkey2, ("hT", t)], MK(0))
                            tt("dve", tmpA[R, :], psA, rope[R, 0, cols], ALU.mult, [("Zb", 3), "rope"], ["tmpA"])
                            tt("dve", tmpB[R, :], psB, rope[R, 1, cols], ALU.mult, MK(0) + ["rope"], ["tmpB"])
                            tt("dve", krT[R, cols], tmpA[R, :], tmpB[R, :], ALU.add, ["tmpA", "tmpB"], ["krT"])

                        sm_scale = (64 + 32) ** -0.5
                        HS = S // 2
                        for pr in range(4 if "nomlaheads" not in DBG else 0):
                            for j in range(TT):
                                ps = M[j % 2][:, 0:P]
                                pk = MK(j % 2, 0, P)
                                mm(ps, ukvT[:, j * P:(j + 1) * P], wv[:, 128 * pr:128 * pr + 128], True, True, ["ukvT", "wv"], pk)
                                cp("act" if j % 2 else "dve", Vp[:, j, :], ps, pk, ["Vp"])
                            for hh in range(2):
                                h = 2 * pr + hh
                                qk_, kk_ = ("QTm", hh), ("KTm", hh)
                                for t in range(NT if "noproj" not in DBG else 0):
                                    cols = slice(t * T, (t + 1) * T)
                                    psA = Z[0][:, 0:T]
                                    psB = Z[0][0:32, 512:512 + T]
                                    for c2 in range(2):
                                        mm(psA, wuq[:, c2, P * h:P * h + P], uT[:, c2, cols], c2 == 0, c2 == 1,
                                           ["wuq", "uT"], [("Zb", 0)])
                                    for c2 in range(2 if "noB" not in DBG else 0):
                                        mm(psB, wuqs[:, c2, 32 * h:32 * h + 32], uT[:, c2, cols], c2 == 0, c2 == 1,
                                           ["wuqs", "uT"], [("Zb", 1)])
                                    if "noQcopy" not in DBG:
                                        cp("act", QT[:, hh, cols], Z[0][:, 0:T], [("Zb", 0)], [qk_])
                                    if "norope" not in DBG:
                                        tt("dve", tmpA[R, :], Z[0][R, 0:T], rope[R, 0, cols], ALU.mult, [("Zb", 0), "rope"], ["tmpA"])
                                    if "noB" not in DBG:
                                        tt("dve", tmpB[R, :], psB, rope[R, 1, cols], ALU.mult, [("Zb", 1), "rope"], ["tmpB"])
                                    if "norope" not in DBG:
                                        tt("dve", QT[R, hh, cols], tmpA[R, :], tmpB[R, :], ALU.add, ["tmpA", "tmpB"], [qk_])
                                    psK = Z[1][:, 0:T]
                                    if "noK" not in DBG:
                                        mm(psK, wk[:, P * h:P * h + P], ukvT[:, cols], True, True, ["wk", "ukvT"], [("Zb", 2)])
                                        cp("act", KT[:, hh, cols], Z[1][:, 0:T], [("Zb", 2)], [kk_])
                                if "nokr" not in DBG:
                                    cp("dve", KT[R, hh, :], krT[R, :], ["krT"], [kk_])
                                sqb = wtsall
                                if "nonrm" not in DBG:
                                    act(sqb[:, 0:S], KT[:, hh, :], AF.Square, [kk_], ["sqb"])
                                for t in range(NT if "nonrm" not in DBG else 0):
                                    mm(M[0][0:64, 0:T], ones_b[:, 0:64], sqb[:, t * T:(t + 1) * T], True, True, ["sqb", "ones_b"], MK(0))
                                    sc.op("dve", lambda e, t=t: e.reduce_max(kst[0:64, t:t + 1], M[0][0:64, 0:T], AX.X),
                                          MK(0), ["kst"])
                                if "nonrm" not in DBG:
                                    sc.op("dve", lambda e: e.reduce_max(kst[0:64, 8:9], kst[0:64, 0:NT], AX.X), ["kst"], ["kst"])
                                    act(kst[0:64, 9:10], kst[0:64, 8:9], AF.Ln, ["kst"], ["kst"])
                                    act(kst[0:64, 9:10], kst[0:64, 9:10], AF.Exp, ["kst"], ["kst"], scale=0.5)
                                    ts("dve", kst[0:64, 10:11], kst[0:64, 9:10], -1.0, None, ALU.mult, None, ["kst"], ["kst"])
                                    act(sqb[:, 0:S], QT[:, hh, :], AF.Square, [qk_], ["sqb"])
                                A1 = slice(32, 33)
                                for t in range(NT if "nonrm" not in DBG else 0):
                                    cols = slice(t * T, (t + 1) * T)
                                    mm(M[1][0:64, 0:T], ones_b[:, 0:64], sqb[:, cols], True, True, ["sqb", "ones_b"], MK(1))
                                    if "noaug" in DBG:
                                        continue
                                    act(tmpC[A1, :], M[1][A1, 0:T], AF.Ln, MK(1), ["tmpC"], bias=1e-20)
                                    act(tmpC[A1, :], tmpC[A1, :], AF.Exp, ["tmpC"], ["tmpC"], scale=0.5)
                                    ts("dve", QT[A1, hh, cols], tmpC[A1, :], kst[A1, 10:11], None, ALU.mult, None,
                                       ["tmpC", "kst"], [qk_])
                                if "noaug" not in DBG:
                                    sc.op("dve", lambda e, hh=hh: e.memset(KT[32:33, hh, :], 1.0), [kk_], [kk_])

                            mt = []
                            cnt = 0
                            for hh in range(2):
                                qk_, kk_ = ("QTm", hh), ("KTm", hh)
                                rows = slice(0, 64) if hh == 0 else slice(64, 128)
                                Mv = P
                                for half in range(2 if "noattn" not in DBG else 0):
                                    qlo, qhi = half * HS, (half + 1) * HS
                                    nj = qhi // P
                                    for j in range(nj):
                                        stg = [[] for _ in range(4)]
                                        mt.append(stg)
                                        zi = cnt % 2
                                        cnt += 1
                                        c0 = max(qlo, j * P)
                                        F = qhi - c0
                                        diag = (j * P >= qlo)
                                        zt = Z[zi]
                                        pT, pk_ = wrall[:, zi * 1024:zi * 1024 + F], ("pT", zi)
                                        sc.defer = stg[0]
                                        for g0 in range(0, F, 512):
                                            g1 = min(F, g0 + 512)
                                            mm(zt[:, g0:g1], KT[:, hh, j * P:(j + 1) * P], QT[:, hh, c0 + g0:c0 + g1],
                                               True, True, [qk_, kk_], [("Zb", 2 * zi + g0 // 512)])
                                        sc.defer = stg[1]
                                        act(pT, zt[:, 0:F], AF.Exp, [("Zb", 2 * zi + k) for k in range((F + 511) // 512)],
                                            [pk_], scale=sm_scale)
                                        sc.defer = stg[2]
                                        if diag:
                                            tt("dve", pT[:, 0:P], pT[:, 0:P], maskT[:], ALU.mult, [pk_, "maskT"], [pk_])
                                        sc.defer = stg[3]
                                        a0 = c0 - qlo
                                        for ab in range((HS + 511) // 512):
                                            lo = max(a0, ab * 512)
                                            hi = min(HS, (ab + 1) * 512)
                                            if lo >= hi:
                                                continue
                                            jl = min(nj, (qlo + hi) // P) - 1
                                            st, sp_ = (j == 0), (j == jl)
                                            mm(WTp[0:Mv, lo:hi], Vp[:, j, 0:Mv], pT[:, lo - a0:hi - a0], st, sp_,
                                               [pk_, "Vp"], [("WT", ab)])
                                            mm(M[ab][0:Mv, lo - ab * 512:hi - ab * 512], ones_b[:, 0:Mv], pT[:, lo - a0:hi - a0],
                                               st, sp_, [pk_, "ones_b"], MK(ab))
                                        if j == nj - 1:
                                            for ab in range((HS + 511) // 512):
                                                wdt = min(512, HS - ab * 512)
                                                tb, tk = (tmpA, "tmpA") if ab == 0 else (tmpB, "tmpB")
                                                if "rcp2" in DBG:
                                                    cp("dve", tb[rows, 0:wdt], M[ab][rows, 0:wdt], MK(ab), [tk])
                                                    sc.op("dve", lambda e, tb=tb, wdt=wdt, rows=rows: e.reciprocal(
                                                        tb[rows, 0:wdt], tb[rows, 0:wdt]), [tk], [tk])
                                                else:
                                                    sc.op("dve", lambda e, tb=tb, ab=ab, wdt=wdt, rows=rows: e.reciprocal(
                                                        tb[rows, 0:wdt], M[ab][rows, 0:wdt]), MK(ab), [tk])
                                                tt("dve", mergedT[rows, 4 + pr, qlo + ab * 512:qlo + ab * 512 + wdt],
                                                   WTp[rows, ab * 512:ab * 512 + wdt], tb[rows, 0:wdt], ALU.mult,
                                                   [("WT", ab), tk], [("mg", 4 + pr)])
                            sc.defer = None
                            nt_ = len(mt)
                            for step in range(nt_ + 3):
                                for sg_ in range(3, -1, -1):
                                    k = step - sg_
                                    if 0 <= k < nt_:
                                        sc.flush(mt[k][sg_])
                        sc.barrier()

                    if DEBUG_MG and b == 0 and l == 0:
                        dma("sp", dbg_d, mergedT[:], [("mg", c) for c in range(KC)], ["dbg"], is_out=True)
                    with ExitStack() as esC:
                        wo = sb("wo", [P, KC, D], BF16, esC)
                        h2f = sb("h2f", [P, KC, T], F32, esC)
                        rt = sb("rt", [P, 4 * NS * NE + NS * 64 + 2 * NS + 8], F32, esC)
                        for half in range(2):
                            dma("pool", wo[:, :, half * 512:(half + 1) * 512],
                                w_o_d[l, :, half * 512:(half + 1) * 512].rearrange("(c p) n -> p c n", p=P), [], ["wo"])
                        xt2 = sb("xt2", [P, KC, T], F32, esC)
                        xbufs = [(xt, "xt"), (xt2, "xt2")]
                        ctasks = []
                        for t in range(NT):
                            cols = slice(t * T, (t + 1) * T)
                            xb, xk = xbufs[t % 2]
                            stg = [[] for _ in range(4)]
                            ctasks.append(stg)
                            sc.defer = stg[0]
                            reload(b, t, xb, xk)
                            for dc in range(KC):
                                bi = dc % 4
                                ps = Z[bi // 2][:, (bi % 2) * 512:(bi % 2) * 512 + T]
                                zk = ("Zb", bi)
                                for c in range(KC):
                                    mm(ps, wo[:, c, dc * P:(dc + 1) * P], mergedT[:, c, cols], c == 0, c == KC - 1,
                                       ["wo", ("mg", c)], [zk])
                                act(xb[:, dc, :], xb[:, dc, :], AF.Copy, [xk], [xk], scale=ALPHA)
                                stt("dve", xb[:, dc, :], ps, mod(l, b, 16 + dc), xb[:, dc, :], ALU.mult, ALU.add,
                                    [zk, "modT", xk], [xk])
                            sc.defer = stg[1]
                            stats([xk], xb)
                            for c in range(KC):
                                normalize(c, xb[:, c, :], lnpar(l, 0, c), lnpar(l, 1, c), [xk], [xk], None, xb)
                            spill(b, t, xb, xk)
                            sc.defer = stg[2]
                            adaln_to_hT(l, b, t, 1, second=h2f, xt=xb, xk=xk)
                            sc.defer = stg[3]
                            W = NS * NE
                            lg = M[0][:, 0:W]
                            for s_ in range(NS):
                                for c in range(KC):
                                    mm(lg[:, s_ * NE:(s_ + 1) * NE], h2f[:, c, s_ * P:(s_ + 1) * P], rw[:, c, :],
                                       c == 0, c == KC - 1, ["h2f", "rw"], MK(0))
                            scv = rt[:, 0:W]
                            bs = rt[:, W:2 * W]
                            act(scv, lg, AF.Exp, MK(0), ["rt"], scale=-1.0)
                            ts("dve", scv, scv, 1.0, None, ALU.add, None, ["rt"], ["rt"])
                            sc.op("dve", lambda e, scv=scv: e.reciprocal(scv, scv), ["rt"], ["rt"])
                            tt("dve", bs, scv, rbt[:], ALU.add, ["rt", "rbt"], ["rt"])
                            b4 = bs.rearrange("p (s g f) -> p s g f", s=NS, f=4)
                            G8 = lambda i: rt[:, 2 * W + NS * 8 * i:2 * W + NS * 8 * (i + 1)].rearrange("p (s g) -> p s g", s=NS)
                            hi1, lo1, hi2, lo2, top1, sec, gs, gm = [G8(i) for i in range(8)]
                            tt("dve", hi1, b4[:, :, :, 0], b4[:, :, :, 1], ALU.max, ["rt"], ["rt"])
                            tt("dve", lo1, b4[:, :, :, 0], b4[:, :, :, 1], ALU.min, ["rt"], ["rt"])
                            tt("dve", hi2, b4[:, :, :, 2], b4[:, :, :, 3], ALU.max, ["rt"], ["rt"])
                            tt("dve", lo2, b4[:, :, :, 2], b4[:, :, :, 3], ALU.min, ["rt"], ["rt"])
                            tt("dve", top1, hi1, hi2, ALU.max, ["rt"], ["rt"])
                            tt("dve", hi1, hi1, hi2, ALU.min, ["rt"], ["rt"])
                            tt("dve", lo1, lo1, lo2, ALU.max, ["rt"], ["rt"])
                            tt("dve", sec, hi1, lo1, ALU.max, ["rt"], ["rt"])
                            tt("dve", gs, top1, sec, ALU.add, ["rt"], ["rt"])
                            o2 = 2 * W + NS * 64
                            gmx = rt[:, o2:o2 + NS]
                            gsum = rt[:, o2 + NS:o2 + 2 * NS]
                            sc.op("dve", lambda e, gmx=gmx, gs=gs: e.reduce_max(gmx, gs, AX.X), ["rt"], ["rt"])
                            for s_ in range(NS):
                                ts("dve", gm[:, s_, :], gs[:, s_, :], gmx[:, s_:s_ + 1], None, ALU.is_ge, None, ["rt"], ["rt"])
                            sel = rt[:, o2 + 2 * NS:o2 + 2 * NS + W]
                            s4 = sel.rearrange("p (s g f) -> p s g f", s=NS, f=4)
                            for i in range(4):
                                tt("dve", s4[:, :, :, i], b4[:, :, :, i], sec, ALU.is_ge, ["rt"], ["rt"])
                                tt("dve", s4[:, :, :, i], s4[:, :, :, i], gm, ALU.mult, ["rt"], ["rt"])
                            tt("dve", sel, sel, scv, ALU.mult, ["rt"], ["rt"])
                            sc.op("dve", lambda e, gsum=gsum, sel=sel: e.reduce_sum(
                                gsum, sel.rearrange("p (s e) -> p s e", s=NS), AX.X), ["rt"], ["rt"])
                            sc.op("dve", lambda e, gsum=gsum: e.reciprocal(gsum, gsum), ["rt"], ["rt"])
                            cwv = rt[:, o2 + 2 * NS + W:o2 + 2 * NS + 2 * W]
                            for s_ in range(NS):
                                ts("dve", cwv[:, s_ * NE:(s_ + 1) * NE], sel[:, s_ * NE:(s_ + 1) * NE], gsum[:, s_:s_ + 1],
                                   None, ALU.mult, None, ["rt"], ["rt"])
                            for s_ in range(NS):
                                tr(M[1][0:NE, s_ * P:(s_ + 1) * P], cwv[:, s_ * NE:(s_ + 1) * NE], ident_f[:],
                                   ["rt", "ident_f"], MK(1))
                            cp("dve", cwT[:, 0, cols], M[1][0:NE, 0:T], MK(1), ["cwT"])
                            tt("dve", cwT[:, 1, cols], M[1][0:NE, 0:T], cwT[:, 0, cols], ALU.subtract,
                               MK(1) + ["cwT"], ["cwT"])
                        sc.defer = None
                        for step in range(NT + 3):
                            for sg_ in range(3, -1, -1):
                                k = step - sg_
                                if 0 <= k < NT:
                                    sc.flush(ctasks[k][sg_])
                        sc.barrier()

                with ExitStack() as esD:
                    TM = min(TMOE, T)
                    NTM = S // TM
                    yacc = sb("yacc", [P, KC, S], F32, esD)
                    wall = sb("wall", [P, 6 * KC * DE], BF16, esD)
                    WSZ = KC * DE
                    wgs = [wall[:, i * WSZ:(i + 1) * WSZ].rearrange("p (c n) -> p c n", c=KC) for i in range(2)]
                    wus = [wall[:, (2 + i) * WSZ:(3 + i) * WSZ].rearrange("p (c n) -> p c n", c=KC) for i in range(2)]
                    wds = [wall[:, (4 + i) * WSZ:(5 + i) * WSZ].rearrange("p (j n) -> p j n", j=2) for i in range(2)]
                    xt2e = wall[:, 0:2 * KC * T].bitcast(F32).rearrange("p (c t) -> p c t", c=KC)
                    sgs = [sb("sg%d" % i, [P, 2 * TM], F32, esD) for i in range(2)]
                    actTs = [sb("actT%d" % i, [P, 2 * TM], BF16, esD) for i in range(2)]

                    def load_gu(ex):
                        i = ex % 2
                        dma("pool", wgs[i], wg_d[l, ex].rearrange("(c p) n -> p c n", p=P), [], [("wg", i)])
                        dma("pool", wus[i], wu_d[l, ex].rearrange("(c p) n -> p c n", p=P), [], [("wu", i)])

                    def load_d(ex):
                        i = ex % 2
                        dma("pool", wds[i], wd_d[l, ex].rearrange("(c p) n -> p c n", p=P), [], [("wd", i)])

                    load_gu(0)
                    load_d(0)
                    mtasks = []
                    cnt = 0
                    for ex in range(NEXP):
                        i = ex % 2
                        for t in range(NTM):
                            stg = [[] for _ in range(4)]
                            mtasks.append(stg)
                            par = cnt % 2
                            cnt += 1
                            cols = slice(t * TM, (t + 1) * TM)
                            t5 = (t * TM) // T
                            sc.defer = stg[0]
                            if t == 0 and ex + 1 < NEXP:
                                load_gu(ex + 1)
                                if ex == 0:
                                    load_d(1)
                            Gb, gk = Z[par][:, 0:2 * TM], ("Zb", 2 * par)
                            Ub, uk = Z[par][:, 512:512 + 2 * TM], ("Zb", 2 * par + 1)
                            CW, ck = WTp[:, par * 512:par * 512 + TM], ("WT", par)
                            for j in range(2):
                                for c in range(KC):
                                    mm(Gb[:, j * TM:(j + 1) * TM], wgs[i][:, c, j * P:(j + 1) * P], hT[:, c, cols],
                                       c == 0, c == KC - 1, [("wg", i), ("hT", t5)], [gk])
                            for j in range(2):
                                for c in range(KC):
                                    mm(Ub[:, j * TM:(j + 1) * TM], wus[i][:, c, j * P:(j + 1) * P], hT[:, c, cols],
                                       c == 0, c == KC - 1, [("wu", i), ("hT", t5)], [uk])
                            mm(CW, selrows[:, ex * P:(ex + 1) * P], cwT[:, 0, cols], True, False, ["selrows", "cwT"], [ck])
                            mm(CW, selrows[:, ex * P:(ex + 1) * P], cwT[:, 1, cols], False, True, ["selrows", "cwT"], [ck])
                            sg, sk = sgs[par], ("sg", par)
                            aT, ak = actTs[par], ("actT", par)
                            sc.defer = stg[1]
                            act(sg[:], Gb, AF.Silu, [gk], [sk])
                            sc.defer = stg[2]
                            tt("dve", sg[:], sg[:], Ub, ALU.mult, [sk, uk], [sk])
                            for j in range(2):
                                tt("dve", aT[:, j * TM:(j + 1) * TM], sg[:, j * TM:(j + 1) * TM], CW, ALU.mult,
                                   [sk, ck], [ak])
                            sc.defer = stg[3]
                            for dp in range(KC // 2):
                                Yb = M[dp % 2][:, 0:2 * TM]
                                yk = MK(dp % 2)
                                for dd in range(2):
                                    dc = 2 * dp + dd
                                    for j in range(2):
                                        mm(Yb[:, dd * TM:(dd + 1) * TM], wds[i][:, j, dc * P:(dc + 1) * P],
                                           aT[:, j * TM:(j + 1) * TM], j == 0, j == 1, [("wd", i), ak], yk)
                                yv = yacc[:, 2 * dp:2 * dp + 2, cols]
                                Yv = Yb.rearrange("p (a t) -> p a t", a=2)
                                if ex == 0:
                                    cp("act" if dp % 2 else "dve", yv, Yv, yk, [("ya", t5)])
                                else:
                                    tt("dve", yv, yv, Yv, ALU.add, yk + [("ya", t5)], [("ya", t5)])
                            if t == NTM - 1 and ex + 2 < NEXP:
                                load_d(ex + 2)
                    sc.defer = None
                    nt = len(mtasks)
                    for step in range(nt + 3):
                        for sg_ in range(3, -1, -1):
                            k = step - sg_
                            if 0 <= k < nt:
                                sc.flush(mtasks[k][sg_])
                    sc.barrier()

                    xbufs = [(xt, "xt"), (xt2e, "xt2e")]
                    etasks = []
                    for t in range(NT):
                        cols = slice(t * T, (t + 1) * T)
                        xb, xk = xbufs[t % 2]
                        stg = [[] for _ in range(3)]
                        etasks.append(stg)
                        sc.defer = stg[0]
                        reload(b, t, xb, xk)
                        for dc in range(KC):
                            act(xb[:, dc, :], xb[:, dc, :], AF.Copy, [xk], [xk], scale=ALPHA)
                            stt("dve", xb[:, dc, :], yacc[:, dc, cols], mod(l, b, 40 + dc), xb[:, dc, :], ALU.mult, ALU.add,
                                [("ya", t), "modT", xk], [xk])
                        sc.defer = stg[1]
                        stats([xk], xb)
                        for c in range(KC):
                            normalize(c, xb[:, c, :], lnpar(l, 2, c), lnpar(l, 3, c), [xk], [xk], None, xb)
                        sc.defer = stg[2]
                        if l + 1 < L:
                            spill(b, t, xb, xk)
                            adaln_to_hT(l + 1, b, t, 0, xt=xb, xk=xk)
                        else:
                            for s_ in range(NS):
                                tok0 = t * T + s_ * P
                                xi, xik = xins[io_cnt[0] % 2]
                                io_cnt[0] += 1
                                for half in range(2):
                                    Zt = Z[half]
                                    for c4 in range(4):
                                        c = half * 4 + c4
                                        tr(Zt[:, c4 * P:(c4 + 1) * P], xb[:, c, s_ * P:(s_ + 1) * P], ident_f[:],
                                           [xk, "ident_f"], [("Zb", 2 * half)])
                                    cp("act" if half else "dve", xi[:, half * 512:(half + 1) * 512], Zt[:, 0:512],
                                       [("Zb", 2 * half)], [xik])
                                dma("sp", out_d[b, tok0:tok0 + P, :], xi[:], [xik], [("out", b, tok0)], is_out=True)
                    sc.defer = None
                    for step in range(NT + 2):
                        for sg_ in range(2, -1, -1):
                            k = step - sg_
                            if 0 <= k < NT:
                                sc.flush(etasks[k][sg_])
                    sc.barrier()

        sc.finish()
        block = es.enter_context(nc.Block())
        sc.replay(block)
    return nc


def make_in_maps(inputs, n_cores, NB, L=2):
    f = np.float32
    x = np.ascontiguousarray(inputs["x"], dtype=f)
    c = np.asarray(inputs["c"], dtype=f)
    pos = np.asarray(inputs["positions"]).astype(np.int32)
    w_in = np.ascontiguousarray(inputs["w_in"], dtype=f)
    S = x.shape[1]
    inv = (np.float32(10000.0) ** (-np.arange(0, 32, 2, dtype=np.float32) / np.float32(32))).astype(f)
    cst = np.zeros((P, 4), f)
    cst[:, 1] = 1.0
    for i in range(32):
        cst[i, 0] = inv[i % 16]
        cst[i, 1] = -1.0 if i < 16 else 1.0
    w_kr = np.ascontiguousarray(np.concatenate(
        [w_in[:, :, 1920:1952], w_in[:, :, 1936:1952], w_in[:, :, 1920:1936]], axis=2))
    w_uq0 = np.asarray(inputs["w_uq"], dtype=f)
    wq4 = w_uq0.reshape(L, 256, 8, 96)
    zq = np.zeros((L, 256, 8, 32), f)
    w_uq = np.ascontiguousarray(np.concatenate([wq4[..., 64:96], zq, wq4[..., 0:64]], axis=-1).reshape(L, 256, 1024))
    w_uqs = np.ascontiguousarray(np.concatenate([wq4[..., 80:96], wq4[..., 64:80]], axis=-1).reshape(L, 256, 256))
    wkv4 = np.asarray(inputs["w_ukv"], dtype=f).reshape(L, 128, 8, 128)
    zk = np.zeros((L, 128, 8, 64), f)
    wk_p = np.ascontiguousarray(np.concatenate([zk, wkv4[..., 0:64]], axis=-1).reshape(L, 128, 1024))
    wv_p = np.ascontiguousarray(wkv4[..., 64:128].reshape(L, 128, 512))
    ada_bT = np.ascontiguousarray(np.asarray(inputs["ada_b"], dtype=f).reshape(L, 48, P).transpose(2, 0, 1).reshape(P, L * 48))
    lnp = np.stack([np.asarray(inputs[k], dtype=f) for k in ("ln1_g", "ln1_b", "ln2_g", "ln2_b")], axis=1)
    lnp = np.ascontiguousarray(lnp.reshape(L, 4, KC, P).transpose(3, 0, 1, 2).reshape(P, L * 4 * KC))
    qn = np.ascontiguousarray(np.asarray(inputs["q_norm"], dtype=f).reshape(L, 2, P).transpose(2, 0, 1).reshape(P, L * 2))
    kvn = np.ascontiguousarray(np.asarray(inputs["kv_norm"], dtype=f).reshape(L, P).T)
    rbias = np.ascontiguousarray(np.broadcast_to(np.asarray(inputs["router_bias"], dtype=f)[None, :], (P, NE)))
    shared = {
        "cst": cst, "ada_w": np.ascontiguousarray(inputs["ada_w"], dtype=f), "ada_bT": ada_bT,
        "w_in": w_in, "w_kr": w_kr, "qn": qn, "kvn": kvn, "w_uq": w_uq, "w_uqs": w_uqs,
        "wk_p": wk_p, "wv_p": wv_p, "w_o": np.ascontiguousarray(inputs["w_o"], dtype=f),
        "lnp": lnp, "router_w": np.ascontiguousarray(inputs["router_w"], dtype=f), "rbias": rbias,
        "w_gate": np.ascontiguousarray(inputs["w_gate"], dtype=f), "w_up": np.ascontiguousarray(inputs["w_up"], dtype=f),
        "w_down": np.ascontiguousarray(inputs["w_down"], dtype=f),
    }
    maps = []
    for ci in range(n_cores):
        sl = slice(ci * NB, (ci + 1) * NB)
        cc = c[sl]
        cT = np.ascontiguousarray(cc.reshape(NB, KC, P).transpose(2, 1, 0).reshape(P, KC * NB))
        posr = np.ascontiguousarray(np.broadcast_to(pos[sl][:, None, :], (NB, 32, S)))
        m = dict(shared)
        m.update({"x": np.ascontiguousarray(x[sl]), "cT": cT, "posr": posr})
        maps.append(m)
    return maps


_NC_CACHE = {}


def kernel(**inputs):
    n_cores = 8
    B, S, _ = inputs["x"].shape
    NB = B // n_cores
    key = (S, NB)
    if key not in _NC_CACHE:
        _NC_CACHE[key] = build_nc(S=S, NB=NB, L=2)
    nc = _NC_CACHE[key]
    maps = make_in_maps(inputs, n_cores, NB)
    res = run_bass_kernel_spmd(nc, maps, core_ids=list(range(n_cores)))
    out = np.concatenate([np.asarray(r["out"]) for r in res.results], axis=0)
    return out.astype(np.float32)
```

```python
import numpy as np
from contextlib import ExitStack
import concourse.bass as bass
import concourse.mybir as mybir
from concourse.bass_utils import run_bass_kernel_spmd

F32 = mybir.dt.float32
BF16 = mybir.dt.bfloat16
I32 = mybir.dt.int32
AF = mybir.ActivationFunctionType
ALU = mybir.AluOpType
AX = mybir.AxisListType

P = 128
D = 1024
KC = 8
NE = 32
DE = 256
IN_W = 1952
ALPHA = float((2 * 2) ** 0.25)
LN_EPS = 1e-5
RMS_EPS = 1e-6
PI = float(np.pi)
NDMA = 24
DEBUG_MG = False
DBG = set()


class Sched:
    ENG = ("pe", "act", "dve", "pool", "sp")

    def __init__(self, nc, es):
        self.nc = nc
        self.sem = {e: es.enter_context(nc.semaphore("s_" + e)) for e in self.ENG}
        self.dsem = [es.enter_context(nc.semaphore("s_dma%d" % i)) for i in range(NDMA)]
        self.cnt = {e: 0 for e in self.ENG}
        self.dcnt = [0] * NDMA
        self.drr = {"sp": 0, "pool": 0}
        self.known = {e: {} for e in self.ENG}
        self.prog = {e: [] for e in self.ENG}
        self.last_w = {}
        self.readers = {}
        self.out_tokens = []
        self.defer = None

    def _deps(self, eng, r, w):
        deps = {}

        def add(tok):
            if tok is None:
                return
            k, v = tok
            if deps.get(k, 0) < v:
                deps[k] = v

        for k in r:
            add(self.last_w.get(k))
            if isinstance(k, tuple) and k[0] in ("Zb", "WT", "M"):
                for tok in self.readers.get(k, {}).items():
                    if tok[0] != eng:
                        add(tok)
        for k in w:
            add(self.last_w.get(k))
            for tok in self.readers.get(k, {}).items():
                add(tok)
        waits = []
        kn = self.known[eng]
        for k, v in deps.items():
            if k == eng and eng == "pe":
                continue
            if kn.get(k, 0) >= v:
                continue
            kn[k] = v
            waits.append((k, v))
        return waits

    def _commit(self, tok, r, w):
        for k in w:
            self.last_w[k] = tok
            self.readers[k] = {}
        for k in r:
            if k in w:
                continue
            d = self.readers.setdefault(k, {})
            if d.get(tok[0], 0) < tok[1]:
                d[tok[0]] = tok[1]

    def flush(self, lst):
        d, self.defer = self.defer, None
        for item in lst:
            if item[0] == "dma":
                self.dma(*item[1:])
            else:
                self.op(*item)
        self.defer = d

    def op(self, eng, fn, r=(), w=()):
        if self.defer is not None:
            self.defer.append((eng, fn, tuple(r), tuple(w)))
            return
        waits = self._deps(eng, r, w)
        self.cnt[eng] += 1
        tok = (eng, self.cnt[eng])
        self.prog[eng].append((waits, fn, (eng, 1)))
        self._commit(tok, r, w)

    def dma(self, q, fn, r=(), w=(), is_out=False):
        if self.defer is not None:
            self.defer.append(("dma", q, fn, tuple(r), tuple(w), is_out))
            return
        half = NDMA // 2
        i = self.drr[q] + (0 if q == "sp" else half)
        self.drr[q] = (self.drr[q] + 1) % half
        waits = self._deps(q, r, w)
        dk = ("d", i)
        prev = self.dcnt[i]
        if prev > 0 and self.known[q].get(dk, 0) < prev:
            self.known[q][dk] = prev
            waits.append((dk, prev))
        self.dcnt[i] += 16
        tok = (dk, self.dcnt[i])
        self.prog[q].append((waits, fn, (dk, 16)))
        self._commit(tok, r, w)
        if is_out:
            self.out_tokens.append(tok)

    def barrier(self):
        for e in self.ENG:
            waits = []
            for o in self.ENG:
                if o != e and self.cnt[o] > self.known[e].get(o, 0):
                    self.known[e][o] = self.cnt[o]
                    waits.append((o, self.cnt[o]))
            for i in range(NDMA):
                dk = ("d", i)
                if self.dcnt[i] > self.known[e].get(dk, 0):
                    self.known[e][dk] = self.dcnt[i]
                    waits.append((dk, self.dcnt[i]))
            if waits:
                self.prog[e].append((waits, None, None))

    def finish(self):
        best = {}
        for (dk, v) in self.out_tokens:
            if best.get(dk, 0) < v:
                best[dk] = v
        self.prog["sp"].append((list(best.items()), None, None))

    def _semof(self, k):
        if isinstance(k, tuple):
            return self.dsem[k[1]]
        return self.sem[k]

    def replay(self, block):
        sections = {"pe": block.tensor, "act": block.scalar, "dve": block.vector,
                    "pool": block.gpsimd, "sp": block.sync}
        for en in self.ENG:
            prog = self.prog[en]

            def body(e, prog=prog):
                for waits, fn, inc in prog:
                    for (k, v) in waits:
                        e.wait_ge(self._semof(k), v)
                    if fn is not None:
                        ins = fn(e)
                        ins.then_inc(self._semof(inc[0]), inc[1])

            sections[en](body)


def build_nc(S=2048, NB=2, L=2, TQ=512, KCH=1024, NEXP=NE, KSB=512, KML=1024, TMOE=256):
    T = min(TQ, S)
    NT = S // T
    NS = T // P
    TT = S // P
    nc = bass.Bass("TRN2", target_bir_lowering=False)

    def din(name, shape, dt=F32):
        return nc.dram_tensor(name, list(shape), dt, kind="ExternalInput").ap()

    x_d = din("x", [NB, S, D])
    cT_d = din("cT", [P, KC * NB])
    posr_d = din("posr", [NB, 32, S], I32)
    cst_d = din("cst", [P, 4])
    ada_w_d = din("ada_w", [L, D, 6 * D])
    ada_bT_d = din("ada_bT", [P, L * 48])
    w_in_d = din("w_in", [L, D, IN_W])
    w_kr_d = din("w_kr", [L, D, 64])
    qn_d = din("qn", [P, L * 2])
    kvn_d = din("kvn", [P, L])
    w_uq_d = din("w_uq", [L, 256, 1024])
    w_uqs_d = din("w_uqs", [L, 256, 256])
    wk_d = din("wk_p", [L, 128, 1024])
    wv_d = din("wv_p", [L, 128, 512])
    w_o_d = din("w_o", [L, D, D])
    lnp_d = din("lnp", [P, L * 4 * KC])
    rw_d = din("router_w", [D, NE])
    rb_d = din("rbias", [P, NE])
    wg_d = din("w_gate", [L, NE, D, DE])
    wu_d = din("w_up", [L, NE, D, DE])
    wd_d = din("w_down", [L, NE, DE, D])
    out_d = nc.dram_tensor("out", [NB, S, D], F32, kind="ExternalOutput").ap()
    scr_d = nc.dram_tensor("xscr", [NB, P, KC, S], F32, kind="Internal").ap()
    dbg_d = nc.dram_tensor("dbg_mg", [P, KC, S], BF16, kind="ExternalOutput").ap() if DEBUG_MG else None

    with ExitStack() as es:
        sc = Sched(nc, es)

        sb_cache = {}

        def sb(name, shape, dt, stack=es):
            if name not in sb_cache:
                sb_cache[name] = stack.enter_context(nc.sbuf_tensor("sb_" + name, list(shape), dt))
            return sb_cache[name]

        Z = [es.enter_context(nc.psum_tensor("Z%d" % i, [P, 1024], F32)) for i in range(2)]
        WTp = es.enter_context(nc.psum_tensor("WTp", [P, 1024], F32))
        M = [es.enter_context(nc.psum_tensor("M%d" % i, [P, 512], F32)) for i in range(2)]
        WTb = WTp[:, :].bitcast(BF16)

        def MK(i, a=0, b=512):
            return [("M", i)]

        def mm(out, lhsT, rhs, start, stop, r, w):
            sc.op("pe", lambda e: e.matmul(out, lhsT=lhsT, rhs=rhs, start=start, stop=stop), r, w)

        def tr(out, in_, ident, r, w):
            sc.op("pe", lambda e: e.transpose(out, in_, ident), r, w)

        def act(out, in_, func, r, w, bias=None, scale=None, accum_out=None):
            kw = {}
            if bias is not None:
                kw["bias"] = bias
            if scale is not None:
                kw["scale"] = scale
            if accum_out is not None:
                kw["accum_out"] = accum_out
            sc.op("act", lambda e: e.activation(out, in_, func, **kw), r, w)

        def tt(eng, out, in0, in1, op, r, w):
            sc.op(eng, lambda e: e.tensor_tensor(out, in0, in1, op), r, w)

        def ts(eng, out, in0, s1, s2, op0, op1, r, w):
            if s2 is None:
                sc.op(eng, lambda e: e.tensor_scalar(out, in0, s1, None, op0), r, w)
            else:
                sc.op(eng, lambda e: e.tensor_scalar(out, in0, s1, s2, op0, op1), r, w)

        def stt(eng, out, in0, scalar, in1, op0, op1, r, w):
            sc.op(eng, lambda e: e.scalar_tensor_tensor(out, in0, scalar, in1, op0, op1), r, w)

        def cp(eng, out, in_, r, w):
            if eng == "act":
                sc.op("act", lambda e: e.activation(out, in_, AF.Copy), r, w)
            else:
                sc.op(eng, lambda e: e.tensor_copy(out, in_), r, w)

        def dma(q, out, in_, r, w, is_out=False):
            sc.dma(q, lambda e: e.dma_start(out=out, in_=in_), r, w, is_out=is_out)

        ident_f = sb("ident_f", [P, P], F32)
        ident_b = sb("ident_b", [P, P], BF16)
        ones_f = sb("ones_f", [P, P], F32)
        mask_s = sb("mask_s", [P, P], F32)
        mask_sb = sb("mask_sb", [P, P], BF16)
        negm = sb("negm", [P, P], F32)
        maskT = sb("maskT", [P, P], BF16)
        bigS = sb("bigS", [P, P], F32)
        ones_b = sb("ones_b", [P, P], BF16)
        scan1 = sb("scan1", [P, KCH], F32)
        selrows = sb("selrows", [32, NE * P], BF16)
        cst = sb("cst", [P, 4], F32)
        modT = sb("modT", [P, L * NB * 48], F32)
        ada_bT = sb("ada_bT", [P, L * 48], F32)
        lnp = sb("lnp", [P, L * 4 * KC], F32)
        qn = sb("qn", [P, L * 2], F32)
        kvn = sb("kvn", [P, L], F32)
        rw = sb("rw", [P, KC, NE], F32)
        rb = sb("rb", [P, NE], F32)
        rbt = sb("rbt", [P, NS * NE], F32)
        cact = sb("cact", [P, KC * NB], F32)
        hT = sb("hT", [P, KC, S], BF16)
        xt = sb("xt", [P, KC, T], F32)
        tmpA = sb("tmpA", [P, T], F32)
        tmpB = sb("tmpB", [P, T], F32)
        tmpC = sb("tmpC", [P, T], F32)
        tmpD = sb("tmpD", [P, T], F32)
        cb16 = [sb("cb16_%d" % i, [P, T], BF16) for i in range(2)]
        sq16 = [sb("sq16_%d" % i, [P, T], BF16) for i in range(2)]
        cwT = sb("cwT", [32, 2, S], BF16)
        rope = sb("rope", [P, 2, S], BF16)
        xin = sb("xin", [P, D], F32)
        xin2 = sb("xin2", [P, D], F32)
        xins = [(xin, "xin0"), (xin2, "xin1")]
        io_cnt = [0]

        def mod(l, b, j):
            o = (l * NB + b) * 48 + j
            return modT[:, o:o + 1]

        def lnpar(l, which, c):
            o = (l * 4 + which) * KC + c
            return lnp[:, o:o + 1]

        sc.op("pool", lambda e: e.memset(ident_f[:], 1.0), w=["ident_f"])
        sc.op("pool", lambda e: e.affine_select(out=ident_f[:], in_=ident_f[:], pattern=[[-1, P]],
                                                compare_op=ALU.is_equal, fill=0.0, base=0,
                                                channel_multiplier=1), r=["ident_f"], w=["ident_f"])
        cp("pool", ident_b[:], ident_f[:], ["ident_f"], ["ident_b"])
        sc.op("pool", lambda e: e.memset(ones_f[:], 1.0), w=["ones_f"])
        sc.op("pool", lambda e: e.memset(scan1[:], 1.0), w=["scan1"])
        sc.op("pool", lambda e: e.memset(mask_s[:], 1.0), w=["mask_s"])
        sc.op("pool", lambda e: e.affine_select(out=mask_s[:], in_=mask_s[:], pattern=[[-1, P]],
                                                compare_op=ALU.is_gt, fill=0.0, base=0,
                                                channel_multiplier=1), r=["mask_s"], w=["mask_s"])
        cp("pool", mask_sb[:], mask_s[:], ["mask_s"], ["mask_sb"])
        sc.op("pool", lambda e: e.memset(negm[:], 0.0), w=["negm"])
        sc.op("pool", lambda e: e.affine_select(out=negm[:], in_=negm[:], pattern=[[-1, P]],
                                                compare_op=ALU.is_ge, fill=-30000.0, base=0,
                                                channel_multiplier=1), r=["negm"], w=["negm"])
        ts("dve", maskT[:], mask_s[:], -1.0, 1.0, ALU.mult, ALU.add, ["mask_s"], ["maskT"])
        cp("dve", ones_b[:], ones_f[:], ["ones_f"], ["ones_b"])
        ts("dve", bigS[:], mask_s[:], -30000.0, 30000.0, ALU.mult, ALU.add, ["mask_s"], ["bigS"])
        sc.op("pool", lambda e: e.memset(selrows[:], 1.0), w=["selrows"])
        sc.op("pool", lambda e: e.affine_select(
            out=selrows[:].rearrange("k (e m) -> k e m", m=P), in_=selrows[:].rearrange("k (e m) -> k e m", m=P),
            pattern=[[-1, NE], [0, P]], compare_op=ALU.is_equal, fill=0.0, base=0,
            channel_multiplier=1), r=["selrows"], w=["selrows"])

        dma("sp", cst[:], cst_d, [], ["cst"])
        dma("sp", ada_bT[:], ada_bT_d, [], ["ada_bT"])
        dma("sp", lnp[:], lnp_d, [], ["lnp"])
        dma("sp", qn[:], qn_d, [], ["qn"])
        dma("sp", kvn[:], kvn_d, [], ["kvn"])
        dma("sp", rw[:], rw_d.rearrange("(c p) n -> p c n", p=P), [], ["rw"])
        dma("sp", rb[:], rb_d, [], ["rb"])
        for s_ in range(NS):
            dma("sp", rbt[:, s_ * NE:(s_ + 1) * NE], rb_d, [], ["rbt"])
        dma("sp", cact[:], cT_d, [], ["cact"])
        act(tmpA[:, 0:KC * NB], cact[:], AF.Exp, ["cact"], ["tmpA"], scale=-1.0)
        ts("dve", tmpA[:, 0:KC * NB], tmpA[:, 0:KC * NB], 1.0, None, ALU.add, None, ["tmpA"], ["tmpA"])
        sc.op("dve", lambda e: e.reciprocal(tmpA[:, 0:KC * NB], tmpA[:, 0:KC * NB]), ["tmpA"], ["tmpA"])
        tt("dve", cact[:], cact[:], tmpA[:, 0:KC * NB], ALU.mult, ["cact", "tmpA"], ["cact"])

        with ExitStack() as es0:
            awst = [sb("awst%d" % i, [P, KC, 512], BF16, es0) for i in range(3)]
            cact16 = sb("cact16", [P, KC * NB], BF16, es0)
            cp("dve", cact16[:], cact[:], ["cact"], ["cact16"])
            gi = 0
            for l in range(L):
                for jg in range(12):
                    bufi = gi % 3
                    gi += 1
                    aw = awst[bufi]
                    dma("pool", aw[:], ada_w_d[l, :, jg * 512:(jg + 1) * 512].rearrange("(c p) n -> p c n", p=P),
                        [], [("awst", bufi)])
                    for jj in range(4):
                        j = jg * 4 + jj
                        for kc in range(KC):
                            mm(M[jj % 2][:, 0:NB], aw[:, kc, jj * P:(jj + 1) * P],
                               cact16[:, kc * NB:(kc + 1) * NB], kc == 0, kc == KC - 1,
                               [("awst", bufi), "cact16"], MK(jj % 2))
                        for b in range(NB):
                            ts("dve", mod(l, b, j), M[jj % 2][:, b:b + 1], ada_bT[:, l * 48 + j:l * 48 + j + 1], None,
                               ALU.add, None, MK(jj % 2) + ["ada_bT"], ["modT"])
                for b in range(NB):
                    for (lo, hi) in ((8, 24), (32, 48)):
                        o = (l * NB + b) * 48
                        ts("dve", modT[:, o + lo:o + hi], modT[:, o + lo:o + hi], 1.0, None, ALU.add, None,
                           ["modT"], ["modT"])
            sc.barrier()

        def stats(src_keys, xt=xt):
            for c in range(KC):
                i = c % 2
                cp("dve", cb16[i][:], xt[:, c, :], src_keys, [("cb16", i)])
                mm(M[0][:, 0:T], ones_b[:], cb16[i][:], c == 0, c == KC - 1, [("cb16", i), "ones_b"], MK(0))
                act(sq16[i][:], xt[:, c, :], AF.Square, src_keys, [("sq16", i)])
                mm(M[1][:, 0:T], ones_b[:], sq16[i][:], c == 0, c == KC - 1, [("sq16", i), "ones_b"], MK(1))
            ts("dve", tmpA[:], M[0][:, 0:T], 1.0 / D, None, ALU.mult, None, MK(0), ["tmpA"])
            tt("dve", tmpC[:], tmpA[:], tmpA[:], ALU.mult, ["tmpA"], ["tmpC"])
            stt("dve", tmpC[:], M[1][:, 0:T], 1.0 / D, tmpC[:], ALU.mult, ALU.subtract, MK(1) + ["tmpC"], ["tmpC"])
            ts("dve", tmpC[:], tmpC[:], LN_EPS, None, ALU.add, None, ["tmpC"], ["tmpC"])
            act(tmpC[:], tmpC[:], AF.Ln, ["tmpC"], ["tmpC"])
            act(tmpB[:], tmpC[:], AF.Exp, ["tmpC"], ["tmpB"], scale=-0.5)

        def normalize(c, out_ap, scale_ap, bias_ap, src_keys, out_keys, second_out=None, xt=xt):
            tbuf, tk = (tmpD, "tmpD") if c % 2 == 0 else (tmpC, "tmpC")
            tt("dve", tbuf[:], xt[:, c, :], tmpA[:], ALU.subtract, src_keys + ["tmpA"], [tk])
            tt("dve", tbuf[:], tbuf[:], tmpB[:], ALU.mult, [tk, "tmpB"], [tk])
            act(out_ap, tbuf[:], AF.Identity, [tk, "modT", "lnp"], out_keys, bias=bias_ap, scale=scale_ap)
            if second_out is not None:
                o2, k2 = second_out
                act(o2, tbuf[:], AF.Identity, [tk, "modT", "lnp"], k2, bias=bias_ap, scale=scale_ap)

        def adaln_to_hT(l, b, t, which, second=None, xt=xt, xk="xt"):
            stats([xk], xt)
            base = 0 if which == 0 else 24
            for c in range(KC):
                so = None
                if second is not None:
                    so = (second[:, c, :], ["h2f"])
                normalize(c, hT[:, c, t * T:(t + 1) * T], mod(l, b, base + 8 + c), mod(l, b, base + c),
                          [xk], [("hT", t)], so, xt)

        def spill(b, t, xt=xt, xk="xt"):
            dma("sp", scr_d[b, :, :, t * T:(t + 1) * T], xt[:], [xk], [("scr", b, t)])

        def reload(b, t, xt=xt, xk="xt"):
            dma("sp", xt[:], scr_d[b, :, :, t * T:(t + 1) * T], [("scr", b, t)], [xk])

        for b in range(NB):
            with ExitStack() as esr:
                posi = sb("posi", [P, S], I32, esr)
                ang = sb("ang", [P, S], F32, esr)
                ang2 = sb("ang2", [P, S], F32, esr)
                kf = sb("kf", [P, S], F32, esr)
                R = slice(0, 32)
                dma("sp", posi[R, :], posr_d[b], [], ["posi"])
                cp("dve", ang[R, :], posi[R, :], ["posi"], ["ang"])
                ts("dve", ang[R, :], ang[R, :], cst[R, 0:1], None, ALU.mult, None, ["ang", "cst"], ["ang"])
                C1 = 6.28125
                C2 = 2.0 * PI - C1
                for which, shift in ((0, 0.5 * PI), (1, 0.0)):
                    if shift != 0.0:
                        ts("dve", ang2[R, :], ang[R, :], shift, None, ALU.add, None, ["ang"], ["ang2"])
                    else:
                        cp("dve", ang2[R, :], ang[R, :], ["ang"], ["ang2"])
                    ts("dve", posi[R, :], ang2[R, :], 1.0 / (2.0 * PI), None, ALU.mult, None, ["ang2"], ["posi"])
                    cp("dve", kf[R, :], posi[R, :], ["posi"], ["kf"])
                    stt("dve", ang2[R, :], kf[R, :], -C1, ang2[R, :], ALU.mult, ALU.add, ["kf", "ang2"], ["ang2"])
                    stt("dve", ang2[R, :], kf[R, :], -C2, ang2[R, :], ALU.mult, ALU.add, ["kf", "ang2"], ["ang2"])
                    ts("dve", kf[R, :], ang2[R, :], PI, -2.0 * PI, ALU.is_gt, ALU.mult, ["ang2"], ["kf"])
                    tt("dve", ang2[R, :], ang2[R, :], kf[R, :], ALU.add, ["ang2", "kf"], ["ang2"])
                    ts("dve", ang2[R, :], ang2[R, :], -3.1415925, 3.1415925, ALU.max, ALU.min, ["ang2"], ["ang2"])
                    act(ang2[R, :], ang2[R, :], AF.Sin, ["ang2"], ["ang2"])
                    if which == 0:
                        cp("dve", rope[R, 0, :], ang2[R, :], ["ang2"], ["rope"])
                    else:
                        ts("dve", rope[R, 1, :], ang2[R, :], cst[R, 1:2], None, ALU.mult, None,
                           ["ang2", "cst"], ["rope"])
                sc.barrier()

            for l in range(L):
                if l == 0:
                    for t in range(NT):
                        for s_ in range(NS):
                            tok0 = t * T + s_ * P
                            xi, xik = xins[io_cnt[0] % 2]
                            io_cnt[0] += 1
                            dma("sp", xi[:], x_d[b, tok0:tok0 + P, :], [], [xik])
                            for half in range(2):
                                Zt = Z[half]
                                for c4 in range(4):
                                    c = half * 4 + c4
                                    tr(Zt[:, c4 * P:(c4 + 1) * P], xi[:, c * P:(c + 1) * P], ident_f[:],
                                       [xik, "ident_f"], [("Zb", 2 * half)])
                                for c4 in range(4):
                                    c = half * 4 + c4
                                    cp("act" if c4 % 2 else "dve", xt[:, c, s_ * P:(s_ + 1) * P],
                                       Zt[:, c4 * P:(c4 + 1) * P], [("Zb", 2 * half)], ["xt"])
                        spill(b, t)
                        adaln_to_hT(l, b, t, 0)
                    sc.barrier()

                with ExitStack() as esBC:
                    mergedT = sb("mergedT", [P, KC, S], BF16, esBC)
                    with ExitStack() as esB:
                        QT = sb("QT", [P, 2, S], BF16, esB)
                        KT = sb("KT", [P, 2, S], BF16, esB)
                        Vp = sb("Vp", [P, TT, P], BF16, esB)
                        lat = sb("lat", [P, 4096], F32, esB)
                        uT = lat[:, 0:S].bitcast(BF16).rearrange("p (c s) -> p c s", c=2)
                        ukvT = lat[:, 2048:2048 + S // 2].bitcast(BF16)
                        krT = lat[:, 3072:3072 + S // 2].bitcast(BF16)
                        wst = [sb("wst%d" % i, [P, KC, 384], BF16, esB) for i in range(2)]
                        wuq = sb("wuq", [P, 2, 1024], BF16, esB)
                        wuqs = sb("wuqs", [P, 2, 256], BF16, esB)
                        wk = sb("wk", [P, 1024], BF16, esB)
                        wv = sb("wv", [P, 512], BF16, esB)
                        kst = sb("kst", [P, 16], F32, esB)
                        nball = lat[:, 0:2048]
                        bball = lat[:, 2048:4096]
                        dbuf = [sb("dbuf%d" % i, [P, P], F32, esB) for i in range(4)]
                        wrall = sb("wrall", [P, 2048], BF16, esB)
                        wtsall = sb("wtsall", [P, 2048], BF16, esB)
                        opair = [sb("opair%d" % i, [P, P], BF16, esB) for i in range(2)]
                        rcs = [sb("rcs%d" % i, [P, 8], F32, esB) for i in range(8)]
                        rrs = [sb("rrs%d" % i, [P, 16], F32, esB) for i in range(4)]
                        state = {"row": 0, "wst": 0, "zc": 0, "oc": 0, "rc": 0}

                        def load_w(col_specs):
                            i = state["wst"] % 2
                            state["wst"] += 1
                            off = 0
                            for (src, ncols) in col_specs:
                                dma("pool", wst[i][:, :, off:off + ncols], src.rearrange("(c p) n -> p c n", p=P),
                                    [], [("wst", i)])
                                off += ncols
                            return wst[i], ("wst", i)

                        def proj_T(dst_fn, w_ap, wkey, ncolsM, scale=None, dst_keys=()):
                            for t in range(NT):
                                bi = t % 4
                                ps = Z[bi // 2][0:ncolsM, (bi % 2) * 512:(bi % 2) * 512 + T]
                                for c in range(KC):
                                    mm(ps, w_ap[:, c, :], hT[:, c, t * T:(t + 1) * T], c == 0, c == KC - 1,
                                       [wkey, ("hT", t)], [("Zb", bi)])
                                dst_fn(t, ps, ("Zb", bi))

                        NSTG = 7

                        def attn_rows(kind, hh, Kdim, pbase, qidx, vcol0, mcol, opi, tasks):
                            sm_scale = (64 + 32) ** -0.5
                            CH = KSB if kind == "sb" else KML
                            units = 1 if kind == "sb" else 2
                            for qb in range(TT):
                                F = (qb + 1) * P
                                q_ap = QT[pbase:pbase + Kdim, qidx, qb * P:(qb + 1) * P]
                                rr = rrs[state["row"] % 4]
                                rrk = ("rrs", state["row"] % 4)
                                state["row"] += 1
                                chunks = []
                                f1 = F
                                while f1 > 0:
                                    f0 = max(0, f1 - CH)
                                    chunks.append((f0, f1))
                                    f1 = f0
                                nch = len(chunks)
                                crec = []
                                if kind == "sb":
                                    osl = state["oc"] % 2
                                    state["oc"] += 1
                                for ci, (f0, f1) in enumerate(chunks):
                                    n = f1 - f0
                                    if kind == "sb":
                                        u = state["zc"] % 4
                                        state["zc"] += 1
                                        ukeys = [u]
                                    else:
                                        u = 2 * (state["zc"] % 2)
                                        state["zc"] += 1
                                        ukeys = [u, u + 1]
                                    c0 = u * 512
                                    zt = Z[u // 2][:, (u % 2) * 512:(u % 2) * 512 + units * 512]
                                    zk = [("Zb", k) for k in ukeys]
                                    nb, nk = nball[:, c0:c0 + units * 512], [("nb", k) for k in ukeys]
                                    bb, bk = bball[:, c0:c0 + units * 512], [("bb", k) for k in ukeys]
                                    db, dk = dbuf[u], [("dbuf", u)]
                                    wr, wrk = wrall[:, c0:c0 + units * 512], [("wr", k) for k in ukeys]
                                    wti = (u // units) % 2
                                    wt, wtk = WTb[:, wti * 1024:(wti + 1) * 1024], [("WT", wti)]
                                    wts, wtsk = wtsall[:, c0:c0 + units * 512], [("wts", k) for k in ukeys]
                                    rci = state["rc"] % 8
                                    state["rc"] += 1
                                    rc, rck = rcs[rci], [("rcs", rci)]
                                    stg = [[] for _ in range(NSTG)]
                                    tasks.append(stg)
                                    sc.defer = stg[0]
                                    for g0 in range(f0, f1, 512):
                                        g1 = min(f1, g0 + 512)
                                        mm(zt[:, g0 - f0:g1 - f0], q_ap, KT[pbase:pbase + Kdim, qidx, g0:g1],
                                           True, True, ["QT", "KT"], [("Zb", u + (g0 - f0) // 512)])
                                    lo_n = n
                                    if kind == "sb":
                                        sc.defer = stg[1]
                                        act(nb[:, 0:n], zt[:, 0:n], AF.Exp, zk, nk, scale=-1.0)
                                        act(nb[:, 0:n], nb[:, 0:n], AF.Ln, nk, nk, bias=1.0)
                                        sc.defer = stg[2]
                                        if ci == 0:
                                            d0 = n - P
                                            lo_n = d0
                                            tt("dve", db[:], zt[:, d0:n], nb[:, d0:n], ALU.add, zk + nk, dk)
                                            tt("dve", db[:], db[:], mask_s[:], ALU.mult, dk + ["mask_s"], dk)
                                            sc.op("dve", lambda e, bb=bb, db=db, d0=d0, n=n: e.tensor_tensor_scan(
                                                out=bb[:, d0:n][:, ::-1], data0=scan1[:, 0:P], data1=db[:, ::-1],
                                                initial=0.0, op0=ALU.mult, op1=ALU.add),
                                                dk + ["scan1"], bk)
                                            cp("dve", rr[:, 0:1], bb[:, d0:d0 + 1], bk, [rrk])
                                            tt("dve", bb[:, d0:n], bb[:, d0:n], zt[:, d0:n], ALU.subtract, bk + zk, bk)
                                            tt("dve", bb[:, d0:n], bb[:, d0:n], bigS[:], ALU.add, bk + ["bigS"], bk)
                                        if lo_n > 0:
                                            tt("dve", bb[:, lo_n - 1:lo_n], rr[:, 0:1], nb[:, lo_n - 1:lo_n], ALU.add,
                                               [rrk] + nk, bk)
                                            if lo_n > 1:
                                                sc.op("dve", lambda e, zt=zt, nb=nb, bb=bb, lo_n=lo_n: e.tensor_tensor_scan(
                                                    out=bb[:, 0:lo_n - 1][:, ::-1], data0=zt[:, 1:lo_n][:, ::-1],
                                                    data1=nb[:, 0:lo_n - 1][:, ::-1], initial=bb[:, lo_n - 1:lo_n],
                                                    op0=ALU.add, op1=ALU.add),
                                                    zk + nk + bk, bk)
                                        if ci + 1 < nch and lo_n > 0:
                                            tt("dve", rr[:, 0:1], bb[:, 0:1], zt[:, 0:1], ALU.add, bk + zk, [rrk])
                                        sc.defer = stg[3]
                                        act(wr[:, 0:n], bb[:, 0:n], AF.Exp, bk, wrk, scale=-1.0)
                                    else:
                                        sc.defer = stg[1]
                                        sc.op("dve", lambda e, rc=rc, zt=zt, n=n: e.reduce_max(rc[:, 0:1], zt[:, 0:n], AX.X),
                                              zk, rck)
                                        ts("dve", rc[:, 1:2], rc[:, 0:1], -sm_scale, None, ALU.mult, None, rck, rck)
                                        sc.op("dve", lambda e, rc=rc: e.memset(rc[:, 2:4], 0.0), [], rck)
                                        if ci == 0:
                                            d0 = n - P
                                            lo_n = d0
                                            tt("dve", db[:], zt[:, d0:n], negm[:], ALU.add, zk + ["negm"], dk)
                                        sc.defer = stg[2]
                                        if ci == 0:
                                            act(wr[:, d0:n], db[:], AF.Exp, dk + rck, wrk + rck,
                                                bias=rc[:, 1:2], scale=sm_scale, accum_out=rc[:, 2:3])
                                        if lo_n > 0:
                                            act(wr[:, 0:lo_n], zt[:, 0:lo_n], AF.Exp, zk + rck, wrk + rck,
                                                bias=rc[:, 1:2], scale=sm_scale, accum_out=rc[:, 3:4])
                                        tt("dve", rc[:, 4:5], rc[:, 2:3], rc[:, 3:4], ALU.add, rck, rck)
                                        osl = state["oc"] % 2
                                        state["oc"] += 1
                                    sc.defer = stg[4]
                                    nblk = n // P
                                    for jj in range(nblk):
                                        tr(wt[:, jj * P:(jj + 1) * P], wr[:, jj * P:(jj + 1) * P], ident_b[:],
                                           wrk + ["ident_b"], wtk)
                                    sc.defer = stg[5]
                                    cp("dve", wts[:, 0:n], wt[:, 0:n], wtk, wtsk)
                                    sc.defer = stg[6]
                                    ops = M[osl][:, 0:64]
                                    opk = MK(osl)
                                    for jj in range(nblk):
                                        if kind == "sb":
                                            st = (ci == 0 and jj == 0)
                                            sp_ = (ci == nch - 1 and jj == nblk - 1)
                                        else:
                                            st = (jj == 0)
                                            sp_ = (jj == nblk - 1)
                                        mm(ops, wts[:, jj * P:(jj + 1) * P], Vp[:, f0 // P + jj, vcol0:vcol0 + 64], st, sp_,
                                           wtsk + ["Vp"], opk)
                                    crec.append((ops, opk, rc, rck))
                                op_ = opair[opi[0] % 2]
                                ok_ = ("opair", opi[0] % 2)
                                dst = op_[:, 64 * hh:64 * hh + 64]
                                if kind == "sb":
                                    ops, opk, _, _ = crec[-1]
                                    cp("dve", dst, ops, opk, [ok_])
                                elif nch == 1:
                                    ops, opk, rc, rck = crec[0]
                                    sc.op("dve", lambda e, rc=rc: e.reciprocal(rc[:, 5:6], rc[:, 4:5]), rck, rck)
                                    ts("dve", dst, ops, rc[:, 5:6], None, ALU.mult, None, opk + rck, [ok_])
                                else:
                                    assert nch == 2
                                    (o0, ok0, r0, rk0), (o1, ok1, r1, rk1) = crec
                                    tt("dve", rr[:, 2:3], r0[:, 0:1], r1[:, 0:1], ALU.max, rk0 + rk1, [rrk])
                                    ts("dve", rr[:, 3:4], rr[:, 2:3], -sm_scale, None, ALU.mult, None, [rrk], [rrk])
                                    act(rr[:, 4:5], r0[:, 0:1], AF.Exp, rk0 + [rrk], [rrk], bias=rr[:, 3:4], scale=sm_scale)
                                    act(rr[:, 5:6], r1[:, 0:1], AF.Exp, rk1 + [rrk], [rrk], bias=rr[:, 3:4], scale=sm_scale)
                                    tt("dve", rr[:, 6:7], rr[:, 4:5], r0[:, 4:5], ALU.mult, [rrk] + rk0, [rrk])
                                    stt("dve", rr[:, 7:8], rr[:, 5:6], r1[:, 4:5], rr[:, 6:7], ALU.mult, ALU.add,
                                        [rrk] + rk1, [rrk])
                                    sc.op("dve", lambda e, rr=rr: e.reciprocal(rr[:, 8:9], rr[:, 7:8]), [rrk], [rrk])
                                    ts("dve", rr[:, 4:6], rr[:, 4:6], rr[:, 8:9], None, ALU.mult, None, [rrk], [rrk])
                                    ts("dve", dst, o0, rr[:, 4:5], None, ALU.mult, None, ok0 + [rrk], [ok_])
                                    stt("dve", dst, o1, rr[:, 5:6], dst, ALU.mult, ALU.add, ok1 + [rrk, ok_], [ok_])
                                if hh == 1:
                                    finish_pair(qb, op_, ok_, mcol)
                                sc.defer = None
                                yield qb

                        def finish_pair(qb, op_, ok_, mcol):
                            k = state["oc"] % 2
                            state["oc"] += 1
                            tps = M[k][:, 0:64].bitcast(BF16)
                            tr(tps, op_[:], ident_b[:], [ok_, "ident_b"], MK(k))
                            cp("dve", mergedT[:, mcol, qb * P:(qb + 1) * P], tps, MK(k), [("mg", mcol)])

                        def run_pair(kind, Kdim, pbases, qidxs, vcols, mcol):
                            opi = [0]
                            tasks = []
                            g0 = attn_rows(kind, 0, Kdim, pbases[0], qidxs[0], vcols[0], mcol, opi, tasks)
                            g1 = attn_rows(kind, 1, Kdim, pbases[1], qidxs[1], vcols[1], mcol, opi, tasks)
                            for _ in range(TT):
                                next(g0)
                                next(g1)
                                opi[0] += 1
                            nt = len(tasks)
                            for step in range(nt + NSTG - 1):
                                for sg_ in range(NSTG - 1, -1, -1):
                                    k = step - sg_
                                    if 0 <= k < nt:
                                        sc.flush(tasks[k][sg_])

                        for pr in range(4):
                            wt_, wkey = load_w([(w_in_d[l, :, pr * P:(pr + 1) * P], P),
                                                (w_in_d[l, :, 512 + pr * P:512 + (pr + 1) * P], P),
                                                (w_in_d[l, :, 1024 + pr * P:1024 + (pr + 1) * P], P)])

                            def put_q(t, ps, zk):
                                act(QT[:, 0, t * T:(t + 1) * T], ps, AF.Copy, [zk], ["QT"], scale=0.125)

                            def put_k(t, ps, zk):
                                cp("dve", KT[:, 0, t * T:(t + 1) * T], ps, [zk], ["KT"])

                            proj_T(put_q, wt_[:, :, 0:P], wkey, P)
                            proj_T(put_k, wt_[:, :, P:2 * P], wkey, P)
                            for j in range(TT):
                                t = (j * P) // T
                                ps = M[j % 2][:, 0:P]
                                pk = MK(j % 2, 0, P)
                                for c in range(KC):
                                    mm(ps, hT[:, c, j * P:(j + 1) * P], wt_[:, c, 2 * P:3 * P], c == 0, c == KC - 1,
                                       [wkey, ("hT", t)], pk)
                                cp("act" if j % 2 else "dve", Vp[:, j, :], ps, pk, ["Vp"])
                            run_pair("sb", 64, (0, 64), (0, 0), (0, 64), pr)

                        sc.barrier()
                        wt_, wkey = load_w([(w_in_d[l, :, 1536:1792], 256)])
                        wt2_, wkey2 = load_w([(w_in_d[l, :, 1792:1920], 128), (w_kr_d[l, :, 0:64], 64)])
                        dma("pool", wuq[:], w_uq_d[l].rearrange("(c p) n -> p c n", p=P), [], ["wuq"])
                        dma("pool", wuqs[:], w_uqs_d[l].rearrange("(c p) n -> p c n", p=P), [], ["wuqs"])
                        dma("pool", wk[:], wk_d[l], [], ["wk"])
                        dma("pool", wv[:], wv_d[l], [], ["wv"])
                        R = slice(0, 32)
                        for t in range(NT if "nolat" not in DBG else 0):
                            cols = slice(t * T, (t + 1) * T)
                            for c2 in range(2):
                                ps = Z[0][:, c2 * 512:c2 * 512 + T]
                                for c in range(KC):
                                    mm(ps, wt_[:, c, c2 * P:(c2 + 1) * P], hT[:, c, cols], c == 0, c == KC - 1,
                                       [wkey, ("hT", t)], [("Zb", c2)])
                                act(tmpA[:] if c2 == 0 else tmpB[:], ps, AF.Square, [("Zb", c2)],
                                    ["tmpA" if c2 == 0 else "tmpB"])
                            mm(M[0][:, 0:T], ones_f[:], tmpA[:], True, False, ["tmpA", "ones_f"], MK(0))
                            mm(M[0][:, 0:T], ones_f[:], tmpB[:], False, True, ["tmpB", "ones_f"], MK(0))
                            act(tmpC[:], M[0][:, 0:T], AF.Ln, MK(0), ["tmpC"], bias=RMS_EPS, scale=1.0 / 256)
                            act(tmpC[:], tmpC[:], AF.Exp, ["tmpC"], ["tmpC"], scale=-0.5)
                            for c2 in range(2):
                                stt("dve", uT[:, c2, cols], Z[0][:, c2 * 512:c2 * 512 + T], qn[:, l * 2 + c2:l * 2 + c2 + 1],
                                    tmpC[:], ALU.mult, ALU.mult, [("Zb", c2), "qn", "tmpC"], ["uT"])
                            ps = Z[1][:, 0:T]
                            for c in range(KC):
                                mm(ps, wt2_[:, c, 0:P], hT[:, c, cols], c == 0, c == KC - 1, [wkey2, ("hT", t)], [("Zb", 2)])
                            act(tmpA[:], ps, AF.Square, [("Zb", 2)], ["tmpA"])
                            mm(M[1][:, 0:T], ones_f[:], tmpA[:], True, True, ["tmpA", "ones_f"], MK(1))
                            act(tmpD[:], M[1][:, 0:T], AF.Ln, MK(1), ["tmpD"], bias=RMS_EPS, scale=1.0 / 128)
                            act(tmpD[:], tmpD[:], AF.Exp, ["tmpD"], ["tmpD"], scale=-0.5)
                            stt("dve", ukvT[:, cols], ps, kvn[:, l:l + 1], tmpD[:], ALU.mult, ALU.mult,
                                [("Zb", 2), "kvn", "tmpD"], ["ukvT"])
                            psA = Z[1][0:32, 512:512 + T]
                            for c in range(KC):
                                mm(psA, wt2_[:, c, P:P + 32], hT[:, c, cols], c == 0, c == KC - 1, [wkey2, ("hT", t)], [("Zb", 3)])
                            psB = M[0][0:32, 0:T]
                            for c in range(KC):
                                mm(psB, wt2_[:, c, P + 32:P + 64], hT[:, c, cols], c == 0, c == KC - 1, [wkey2, ("hT", t)], MK(0))
                            tt("dve", tmpA[R, :], psA, rope[R, 0, cols], ALU.mult, [("Zb", 3), "rope"], ["tmpA"])
                            tt("dve", tmpB[R, :], psB, rope[R, 1, cols], ALU.mult, MK(0) + ["rope"], ["tmpB"])
                            tt("dve", krT[R, cols], tmpA[R, :], tmpB[R, :], ALU.add, ["tmpA", "tmpB"], ["krT"])

                        sm_scale = (64 + 32) ** -0.5
                        HS = S // 2
                        for pr in range(4 if "nomlaheads" not in DBG else 0):
                            for j in range(TT):
                                ps = M[j % 2][:, 0:P]
                                pk = MK(j % 2, 0, P)
                                mm(ps, ukvT[:, j * P:(j + 1) * P], wv[:, 128 * pr:128 * pr + 128], True, True, ["ukvT", "wv"], pk)
                                cp("act" if j % 2 else "dve", Vp[:, j, :], ps, pk, ["Vp"])
                            for hh in range(2):
                                h = 2 * pr + hh
                                qk_, kk_ = ("QTm", hh), ("KTm", hh)
                                for t in range(NT if "noproj" not in DBG else 0):
                                    cols = slice(t * T, (t + 1) * T)
                                    psA = Z[0][:, 0:T]
                                    psB = Z[0][0:32, 512:512 + T]
                                    for c2 in range(2):
                                        mm(psA, wuq[:, c2, P * h:P * h + P], uT[:, c2, cols], c2 == 0, c2 == 1,
                                           ["wuq", "uT"], [("Zb", 0)])
                                    for c2 in range(2 if "noB" not in DBG else 0):
                                        mm(psB, wuqs[:, c2, 32 * h:32 * h + 32], uT[:, c2, cols], c2 == 0, c2 == 1,
                                           ["wuqs", "uT"], [("Zb", 1)])
                                    if "noQcopy" not in DBG:
                                        cp("act", QT[:, hh, cols], Z[0][:, 0:T], [("Zb", 0)], [qk_])
                                    if "norope" not in DBG:
                                        tt("dve", tmpA[R, :], Z[0][R, 0:T], rope[R, 0, cols], ALU.mult, [("Zb", 0), "rope"], ["tmpA"])
                                    if "noB" not in DBG:
                                        tt("dve", tmpB[R, :], psB, rope[R, 1, cols], ALU.mult, [("Zb", 1), "rope"], ["tmpB"])
                                    if "norope" not in DBG:
                                        tt("dve", QT[R, hh, cols], tmpA[R, :], tmpB[R, :], ALU.add, ["tmpA", "tmpB"], [qk_])
                                    psK = Z[1][:, 0:T]
                                    if "noK" not in DBG:
                                        mm(psK, wk[:, P * h:P * h + P], ukvT[:, cols], True, True, ["wk", "ukvT"], [("Zb", 2)])
                                        cp("act", KT[:, hh, cols], Z[1][:, 0:T], [("Zb", 2)], [kk_])
                                if "nokr" not in DBG:
                                    cp("dve", KT[R, hh, :], krT[R, :], ["krT"], [kk_])
                                sqb = wtsall
                                if "nonrm" not in DBG:
                                    act(sqb[:, 0:S], KT[:, hh, :], AF.Square, [kk_], ["sqb"])
                                for t in range(NT if "nonrm" not in DBG else 0):
                                    mm(M[0][0:64, 0:T], ones_b[:, 0:64], sqb[:, t * T:(t + 1) * T], True, True, ["sqb", "ones_b"], MK(0))
                                    sc.op("dve", lambda e, t=t: e.reduce_max(kst[0:64, t:t + 1], M[0][0:64, 0:T], AX.X),
                                          MK(0), ["kst"])
                                if "nonrm" not in DBG:
                                    sc.op("dve", lambda e: e.reduce_max(kst[0:64, 8:9], kst[0:64, 0:NT], AX.X), ["kst"], ["kst"])
                                    act(kst[0:64, 9:10], kst[0:64, 8:9], AF.Ln, ["kst"], ["kst"])
                                    act(kst[0:64, 9:10], kst[0:64, 9:10], AF.Exp, ["kst"], ["kst"], scale=0.5)
                                    ts("dve", kst[0:64, 10:11], kst[0:64, 9:10], -1.0, None, ALU.mult, None, ["kst"], ["kst"])
                                    act(sqb[:, 0:S], QT[:, hh, :], AF.Square, [qk_], ["sqb"])
                                A1 = slice(32, 33)
                                for t in range(NT if "nonrm" not in DBG else 0):
                                    cols = slice(t * T, (t + 1) * T)
                                    mm(M[1][0:64, 0:T], ones_b[:, 0:64], sqb[:, cols], True, True, ["sqb", "ones_b"], MK(1))
                                    if "noaug" in DBG:
                                        continue
                                    act(tmpC[A1, :], M[1][A1, 0:T], AF.Ln, MK(1), ["tmpC"], bias=1e-20)
                                    act(tmpC[A1, :], tmpC[A1, :], AF.Exp, ["tmpC"], ["tmpC"], scale=0.5)
                                    ts("dve", QT[A1, hh, cols], tmpC[A1, :], kst[A1, 10:11], None, ALU.mult, None,
                                       ["tmpC", "kst"], [qk_])
                                if "noaug" not in DBG:
                                    sc.op("dve", lambda e, hh=hh: e.memset(KT[32:33, hh, :], 1.0), [kk_], [kk_])

                            mt = []
                            cnt = 0
                            for hh in range(2):
                                qk_, kk_ = ("QTm", hh), ("KTm", hh)
                                rows = slice(0, 64) if hh == 0 else slice(64, 128)
                                Mv = P
                                for half in range(2 if "noattn" not in DBG else 0):
                                    qlo, qhi = half * HS, (half + 1) * HS
                                    nj = qhi // P
                                    for j in range(nj):
                                        stg = [[] for _ in range(4)]
                                        mt.append(stg)
                                        zi = cnt % 2
                                        cnt += 1
                                        c0 = max(qlo, j * P)
                                        F = qhi - c0
                                        diag = (j * P >= qlo)
                                        zt = Z[zi]
                                        pT, pk_ = wrall[:, zi * 1024:zi * 1024 + F], ("pT", zi)
                                        sc.defer = stg[0]
                                        for g0 in range(0, F, 512):
                                            g1 = min(F, g0 + 512)
                                            mm(zt[:, g0:g1], KT[:, hh, j * P:(j + 1) * P], QT[:, hh, c0 + g0:c0 + g1],
                                               True, True, [qk_, kk_], [("Zb", 2 * zi + g0 // 512)])
                                        sc.defer = stg[1]
                                        act(pT, zt[:, 0:F], AF.Exp, [("Zb", 2 * zi + k) for k in range((F + 511) // 512)],
                                            [pk_], scale=sm_scale)
                                        sc.defer = stg[2]
                                        if diag:
                                            tt("dve", pT[:, 0:P], pT[:, 0:P], maskT[:], ALU.mult, [pk_, "maskT"], [pk_])
                                        sc.defer = stg[3]
                                        a0 = c0 - qlo
                                        for ab in range((HS + 511) // 512):
                                            lo = max(a0, ab * 512)
                                            hi = min(HS, (ab + 1) * 512)
                                            if lo >= hi:
                                                continue
                                            jl = min(nj, (qlo + hi) // P) - 1
                                            st, sp_ = (j == 0), (j == jl)
                                            mm(WTp[0:Mv, lo:hi], Vp[:, j, 0:Mv], pT[:, lo - a0:hi - a0], st, sp_,
                                               [pk_, "Vp"], [("WT", ab)])
                                            mm(M[ab][0:Mv, lo - ab * 512:hi - ab * 512], ones_b[:, 0:Mv], pT[:, lo - a0:hi - a0],
                                               st, sp_, [pk_, "ones_b"], MK(ab))
                                        if j == nj - 1:
                                            for ab in range((HS + 511) // 512):
                                                wdt = min(512, HS - ab * 512)
                                                tb, tk = (tmpA, "tmpA") if ab == 0 else (tmpB, "tmpB")
                                                if "rcp2" in DBG:
                                                    cp("dve", tb[rows, 0:wdt], M[ab][rows, 0:wdt], MK(ab), [tk])
                                                    sc.op("dve", lambda e, tb=tb, wdt=wdt, rows=rows: e.reciprocal(
                                                        tb[rows, 0:wdt], tb[rows, 0:wdt]), [tk], [tk])
                                                else:
                                                    sc.op("dve", lambda e, tb=tb, ab=ab, wdt=wdt, rows=rows: e.reciprocal(
                                                        tb[rows, 0:wdt], M[ab][rows, 0:wdt]), MK(ab), [tk])
                                                tt("dve", mergedT[rows, 4 + pr, qlo + ab * 512:qlo + ab * 512 + wdt],
                                                   WTp[rows, ab * 512:ab * 512 + wdt], tb[rows, 0:wdt], ALU.mult,
                                                   [("WT", ab), tk], [("mg", 4 + pr)])
                            sc.defer = None
                            nt_ = len(mt)
                            for step in range(nt_ + 3):
                                for sg_ in range(3, -1, -1):
                                    k = step - sg_
                                    if 0 <= k < nt_:
                                        sc.flush(mt[k][sg_])
                        sc.barrier()

                    if DEBUG_MG and b == 0 and l == 0:
                        dma("sp", dbg_d, mergedT[:], [("mg", c) for c in range(KC)], ["dbg"], is_out=True)
                    with ExitStack() as esC:
                        wo = sb("wo", [P, KC, D], BF16, esC)
                        h2f = sb("h2f", [P, KC, T], F32, esC)
                        rt = sb("rt", [P, 4 * NS * NE + NS * 64 + 2 * NS + 8], F32, esC)
                        for half in range(2):
                            dma("pool", wo[:, :, half * 512:(half + 1) * 512],
                                w_o_d[l, :, half * 512:(half + 1) * 512].rearrange("(c p) n -> p c n", p=P), [], ["wo"])
                        xt2 = sb("xt2", [P, KC, T], F32, esC)
                        xbufs = [(xt, "xt"), (xt2, "xt2")]
                        ctasks = []
                        for t in range(NT):
                            cols = slice(t * T, (t + 1) * T)
                            xb, xk = xbufs[t % 2]
                            stg = [[] for _ in range(4)]
                            ctasks.append(stg)
                            sc.defer = stg[0]
                            reload(b, t, xb, xk)
                            for dc in range(KC):
                                bi = dc % 4
                                ps = Z[bi // 2][:, (bi % 2) * 512:(bi % 2) * 512 + T]
                                zk = ("Zb", bi)
                                for c in range(KC):
                                    mm(ps, wo[:, c, dc * P:(dc + 1) * P], mergedT[:, c, cols], c == 0, c == KC - 1,
                                       ["wo", ("mg", c)], [zk])
                                act(xb[:, dc, :], xb[:, dc, :], AF.Copy, [xk], [xk], scale=ALPHA)
                                stt("dve", xb[:, dc, :], ps, mod(l, b, 16 + dc), xb[:, dc, :], ALU.mult, ALU.add,
                                    [zk, "modT", xk], [xk])
                            sc.defer = stg[1]
                            stats([xk], xb)
                            for c in range(KC):
                                normalize(c, xb[:, c, :], lnpar(l, 0, c), lnpar(l, 1, c), [xk], [xk], None, xb)
                            spill(b, t, xb, xk)
                            sc.defer = stg[2]
                            adaln_to_hT(l, b, t, 1, second=h2f, xt=xb, xk=xk)
                            sc.defer = stg[3]
                            W = NS * NE
                            lg = M[0][:, 0:W]
                            for s_ in range(NS):
                                for c in range(KC):
                                    mm(lg[:, s_ * NE:(s_ + 1) * NE], h2f[:, c, s_ * P:(s_ + 1) * P], rw[:, c, :],
                                       c == 0, c == KC - 1, ["h2f", "rw"], MK(0))
                            scv = rt[:, 0:W]
                            bs = rt[:, W:2 * W]
                            act(scv, lg, AF.Exp, MK(0), ["rt"], scale=-1.0)
                            ts("dve", scv, scv, 1.0, None, ALU.add, None, ["rt"], ["rt"])
                            sc.op("dve", lambda e, scv=scv: e.reciprocal(scv, scv), ["rt"], ["rt"])
                            tt("dve", bs, scv, rbt[:], ALU.add, ["rt", "rbt"], ["rt"])
                            b4 = bs.rearrange("p (s g f) -> p s g f", s=NS, f=4)
                            G8 = lambda i: rt[:, 2 * W + NS * 8 * i:2 * W + NS * 8 * (i + 1)].rearrange("p (s g) -> p s g", s=NS)
                            hi1, lo1, hi2, lo2, top1, sec, gs, gm = [G8(i) for i in range(8)]
                            tt("dve", hi1, b4[:, :, :, 0], b4[:, :, :, 1], ALU.max, ["rt"], ["rt"])
                            tt("dve", lo1, b4[:, :, :, 0], b4[:, :, :, 1], ALU.min, ["rt"], ["rt"])
                            tt("dve", hi2, b4[:, :, :, 2], b4[:, :, :, 3], ALU.max, ["rt"], ["rt"])
                            tt("dve", lo2, b4[:, :, :, 2], b4[:, :, :, 3], ALU.min, ["rt"], ["rt"])
                            tt("dve", top1, hi1, hi2, ALU.max, ["rt"], ["rt"])
                            tt("dve", hi1, hi1, hi2, ALU.min, ["rt"], ["rt"])
                            tt("dve", lo1, lo1, lo2, ALU.max, ["rt"], ["rt"])
                            tt("dve", sec, hi1, lo1, ALU.max, ["rt"], ["rt"])
                            tt("dve", gs, top1, sec, ALU.add, ["rt"], ["rt"])
                            o2 = 2 * W + NS * 64
                            gmx = rt[:, o2:o2 + NS]
                            gsum = rt[:, o2 + NS:o2 + 2 * NS]
                            sc.op("dve", lambda e, gmx=gmx, gs=gs: e.reduce_max(gmx, gs, AX.X), ["rt"], ["rt"])
                            for s_ in range(NS):
                                ts("dve", gm[:, s_, :], gs[:, s_, :], gmx[:, s_:s_ + 1], None, ALU.is_ge, None, ["rt"], ["rt"])
                            sel = rt[:, o2 + 2 * NS:o2 + 2 * NS + W]
                            s4 = sel.rearrange("p (s g f) -> p s g f", s=NS, f=4)
                            for i in range(4):
                                tt("dve", s4[:, :, :, i], b4[:, :, :, i], sec, ALU.is_ge, ["rt"], ["rt"])
                                tt("dve", s4[:, :, :, i], s4[:, :, :, i], gm, ALU.mult, ["rt"], ["rt"])
                            tt("dve", sel, sel, scv, ALU.mult, ["rt"], ["rt"])
                            sc.op("dve", lambda e, gsum=gsum, sel=sel: e.reduce_sum(
                                gsum, sel.rearrange("p (s e) -> p s e", s=NS), AX.X), ["rt"], ["rt"])
                            sc.op("dve", lambda e, gsum=gsum: e.reciprocal(gsum, gsum), ["rt"], ["rt"])
                            cwv = rt[:, o2 + 2 * NS + W:o2 + 2 * NS + 2 * W]
                            for s_ in range(NS):
                                ts("dve", cwv[:, s_ * NE:(s_ + 1) * NE], sel[:, s_ * NE:(s_ + 1) * NE], gsum[:, s_:s_ + 1],
                                   None, ALU.mult, None, ["rt"], ["rt"])
                            for s_ in range(NS):
                                tr(M[1][0:NE, s_ * P:(s_ + 1) * P], cwv[:, s_ * NE:(s_ + 1) * NE], ident_f[:],
                                   ["rt", "ident_f"], MK(1))
                            cp("dve", cwT[:, 0, cols], M[1][0:NE, 0:T], MK(1), ["cwT"])
                            tt("dve", cwT[:, 1, cols], M[1][0:NE, 0:T], cwT[:, 0, cols], ALU.subtract,
                               MK(1) + ["cwT"], ["cwT"])
                        sc.defer = None
                        for step in range(NT + 3):
                            for sg_ in range(3, -1, -1):
                                k = step - sg_
                                if 0 <= k < NT:
                                    sc.flush(ctasks[k][sg_])
                        sc.barrier()

                with ExitStack() as esD:
                    TM = min(TMOE, T)
                    NTM = S // TM
                    yacc = sb("yacc", [P, KC, S], F32, esD)
                    wall = sb("wall", [P, 6 * KC * DE], BF16, esD)
                    WSZ = KC * DE
                    wgs = [wall[:, i * WSZ:(i + 1) * WSZ].rearrange("p (c n) -> p c n", c=KC) for i in range(2)]
                    wus = [wall[:, (2 + i) * WSZ:(3 + i) * WSZ].rearrange("p (c n) -> p c n", c=KC) for i in range(2)]
                    wds = [wall[:, (4 + i) * WSZ:(5 + i) * WSZ].rearrange("p (j n) -> p j n", j=2) for i in range(2)]
                    xt2e = wall[:, 0:2 * KC * T].bitcast(F32).rearrange("p (c t) -> p c t", c=KC)
                    sgs = [sb("sg%d" % i, [P, 2 * TM], F32, esD) for i in range(2)]
                    actTs = [sb("actT%d" % i, [P, 2 * TM], BF16, esD) for i in range(2)]

                    def load_gu(ex):
                        i = ex % 2
                        dma("pool", wgs[i], wg_d[l, ex].rearrange("(c p) n -> p c n", p=P), [], [("wg", i)])
                        dma("pool", wus[i], wu_d[l, ex].rearrange("(c p) n -> p c n", p=P), [], [("wu", i)])

                    def load_d(ex):
                        i = ex % 2
                        dma("pool", wds[i], wd_d[l, ex].rearrange("(c p) n -> p c n", p=P), [], [("wd", i)])

                    load_gu(0)
                    load_d(0)
                    mtasks = []
                    cnt = 0
                    for ex in range(NEXP):
                        i = ex % 2
                        for t in range(NTM):
                            stg = [[] for _ in range(4)]
                            mtasks.append(stg)
                            par = cnt % 2
                            cnt += 1
                            cols = slice(t * TM, (t + 1) * TM)
                            t5 = (t * TM) // T
                            sc.defer = stg[0]
                            if t == 0 and ex + 1 < NEXP:
                                load_gu(ex + 1)
                                if ex == 0:
                                    load_d(1)
                            Gb, gk = Z[par][:, 0:2 * TM], ("Zb", 2 * par)
                            Ub, uk = Z[par][:, 512:512 + 2 * TM], ("Zb", 2 * par + 1)
                            CW, ck = WTp[:, par * 512:par * 512 + TM], ("WT", par)
                            for j in range(2):
                                for c in range(KC):
                                    mm(Gb[:, j * TM:(j + 1) * TM], wgs[i][:, c, j * P:(j + 1) * P], hT[:, c, cols],
                                       c == 0, c == KC - 1, [("wg", i), ("hT", t5)], [gk])
                            for j in range(2):
                                for c in range(KC):
                                    mm(Ub[:, j * TM:(j + 1) * TM], wus[i][:, c, j * P:(j + 1) * P], hT[:, c, cols],
                                       c == 0, c == KC - 1, [("wu", i), ("hT", t5)], [uk])
                            mm(CW, selrows[:, ex * P:(ex + 1) * P], cwT[:, 0, cols], True, False, ["selrows", "cwT"], [ck])
                            mm(CW, selrows[:, ex * P:(ex + 1) * P], cwT[:, 1, cols], False, True, ["selrows", "cwT"], [ck])
                            sg, sk = sgs[par], ("sg", par)
                            aT, ak = actTs[par], ("actT", par)
                            sc.defer = stg[1]
                            act(sg[:], Gb, AF.Silu, [gk], [sk])
                            sc.defer = stg[2]
                            tt("dve", sg[:], sg[:], Ub, ALU.mult, [sk, uk], [sk])
                            for j in range(2):
                                tt("dve", aT[:, j * TM:(j + 1) * TM], sg[:, j * TM:(j + 1) * TM], CW, ALU.mult,
                                   [sk, ck], [ak])
                            sc.defer = stg[3]
                            for dp in range(KC // 2):
                                Yb = M[dp % 2][:, 0:2 * TM]
                                yk = MK(dp % 2)
                                for dd in range(2):
                                    dc = 2 * dp + dd
                                    for j in range(2):
                                        mm(Yb[:, dd * TM:(dd + 1) * TM], wds[i][:, j, dc * P:(dc + 1) * P],
                                           aT[:, j * TM:(j + 1) * TM], j == 0, j == 1, [("wd", i), ak], yk)
                                yv = yacc[:, 2 * dp:2 * dp + 2, cols]
                                Yv = Yb.rearrange("p (a t) -> p a t", a=2)
                                if ex == 0:
                                    cp("act" if dp % 2 else "dve", yv, Yv, yk, [("ya", t5)])
                                else:
                                    tt("dve", yv, yv, Yv, ALU.add, yk + [("ya", t5)], [("ya", t5)])
                            if t == NTM - 1 and ex + 2 < NEXP:
                                load_d(ex + 2)
                    sc.defer = None
                    nt = len(mtasks)
                    for step in range(nt + 3):
                        for sg_ in range(3, -1, -1):
                            k = step - sg_
                            if 0 <= k < nt:
                                sc.flush(mtasks[k][sg_])
                    sc.barrier()

                    xbufs = [(xt, "xt"), (xt2e, "xt2e")]
                    etasks = []
                    for t in range(NT):
                        cols = slice(t * T, (t + 1) * T)
                        xb, xk = xbufs[t % 2]
                        stg = [[] for _ in range(3)]
                        etasks.append(stg)
                        sc.defer = stg[0]
                        reload(b, t, xb, xk)
                        for dc in range(KC):
                            act(xb[:, dc, :], xb[:, dc, :], AF.Copy, [xk], [xk], scale=ALPHA)
                            stt("dve", xb[:, dc, :], yacc[:, dc, cols], mod(l, b, 40 + dc), xb[:, dc, :], ALU.mult, ALU.add,
                                [("ya", t), "modT", xk], [xk])
                        sc.defer = stg[1]
                        stats([xk], xb)
                        for c in range(KC):
                            normalize(c, xb[:, c, :], lnpar(l, 2, c), lnpar(l, 3, c), [xk], [xk], None, xb)
                        sc.defer = stg[2]
                        if l + 1 < L:
                            spill(b, t, xb, xk)
                            adaln_to_hT(l + 1, b, t, 0, xt=xb, xk=xk)
                        else:
                            for s_ in range(NS):
                                tok0 = t * T + s_ * P
                                xi, xik = xins[io_cnt[0] % 2]
                                io_cnt[0] += 1
                                for half in range(2):
                                    Zt = Z[half]
                                    for c4 in range(4):
                                        c = half * 4 + c4
                                        tr(Zt[:, c4 * P:(c4 + 1) * P], xb[:, c, s_ * P:(s_ + 1) * P], ident_f[:],
                                           [xk, "ident_f"], [("Zb", 2 * half)])
                                    cp("act" if half else "dve", xi[:, half * 512:(half + 1) * 512], Zt[:, 0:512],
                                       [("Zb", 2 * half)], [xik])
                                dma("sp", out_d[b, tok0:tok0 + P, :], xi[:], [xik], [("out", b, tok0)], is_out=True)
                    sc.defer = None
                    for step in range(NT + 2):
                        for sg_ in range(2, -1, -1):
                            k = step - sg_
                            if 0 <= k < NT:
                                sc.flush(etasks[k][sg_])
                    sc.barrier()

        sc.finish()
        block = es.enter_context(nc.Block())
        sc.replay(block)
    return nc


def make_in_maps(inputs, n_cores, NB, L=2):
    f = np.float32
    x = np.ascontiguousarray(inputs["x"], dtype=f)
    c = np.asarray(inputs["c"], dtype=f)
    pos = np.asarray(inputs["positions"]).astype(np.int32)
    w_in = np.ascontiguousarray(inputs["w_in"], dtype=f)
    S = x.shape[1]
    inv = (np.float32(10000.0) ** (-np.arange(0, 32, 2, dtype=np.float32) / np.float32(32))).astype(f)
    cst = np.zeros((P, 4), f)
    cst[:, 1] = 1.0
    for i in range(32):
        cst[i, 0] = inv[i % 16]
        cst[i, 1] = -1.0 if i < 16 else 1.0
    w_kr = np.ascontiguousarray(np.concatenate(
        [w_in[:, :, 1920:1952], w_in[:, :, 1936:1952], w_in[:, :, 1920:1936]], axis=2))
    w_uq0 = np.asarray(inputs["w_uq"], dtype=f)
    wq4 = w_uq0.reshape(L, 256, 8, 96)
    zq = np.zeros((L, 256, 8, 32), f)
    w_uq = np.ascontiguousarray(np.concatenate([wq4[..., 64:96], zq, wq4[..., 0:64]], axis=-1).reshape(L, 256, 1024))
    w_uqs = np.ascontiguousarray(np.concatenate([wq4[..., 80:96], wq4[..., 64:80]], axis=-1).reshape(L, 256, 256))
    wkv4 = np.asarray(inputs["w_ukv"], dtype=f).reshape(L, 128, 8, 128)
    zk = np.zeros((L, 128, 8, 64), f)
    wk_p = np.ascontiguousarray(np.concatenate([zk, wkv4[..., 0:64]], axis=-1).reshape(L, 128, 1024))
    wv_p = np.ascontiguousarray(wkv4[..., 64:128].reshape(L, 128, 512))
    ada_bT = np.ascontiguousarray(np.asarray(inputs["ada_b"], dtype=f).reshape(L, 48, P).transpose(2, 0, 1).reshape(P, L * 48))
    lnp = np.stack([np.asarray(inputs[k], dtype=f) for k in ("ln1_g", "ln1_b", "ln2_g", "ln2_b")], axis=1)
    lnp = np.ascontiguousarray(lnp.reshape(L, 4, KC, P).transpose(3, 0, 1, 2).reshape(P, L * 4 * KC))
    qn = np.ascontiguousarray(np.asarray(inputs["q_norm"], dtype=f).reshape(L, 2, P).transpose(2, 0, 1).reshape(P, L * 2))
    kvn = np.ascontiguousarray(np.asarray(inputs["kv_norm"], dtype=f).reshape(L, P).T)
    rbias = np.ascontiguousarray(np.broadcast_to(np.asarray(inputs["router_bias"], dtype=f)[None, :], (P, NE)))
    shared = {
        "cst": cst, "ada_w": np.ascontiguousarray(inputs["ada_w"], dtype=f), "ada_bT": ada_bT,
        "w_in": w_in, "w_kr": w_kr, "qn": qn, "kvn": kvn, "w_uq": w_uq, "w_uqs": w_uqs,
        "wk_p": wk_p, "wv_p": wv_p, "w_o": np.ascontiguousarray(inputs["w_o"], dtype=f),
        "lnp": lnp, "router_w": np.ascontiguousarray(inputs["router_w"], dtype=f), "rbias": rbias,
        "w_gate": np.ascontiguousarray(inputs["w_gate"], dtype=f), "w_up": np.ascontiguousarray(inputs["w_up"], dtype=f),
        "w_down": np.ascontiguousarray(inputs["w_down"], dtype=f),
    }
    maps = []
    for ci in range(n_cores):
        sl = slice(ci * NB, (ci + 1) * NB)
        cc = c[sl]
        cT = np.ascontiguousarray(cc.reshape(NB, KC, P).transpose(2, 1, 0).reshape(P, KC * NB))
        posr = np.ascontiguousarray(np.broadcast_to(pos[sl][:, None, :], (NB, 32, S)))
        m = dict(shared)
        m.update({"x": np.ascontiguousarray(x[sl]), "cT": cT, "posr": posr})
        maps.append(m)
    return maps


_NC_CACHE = {}


def kernel(**inputs):
    n_cores = 8
    B, S, _ = inputs["x"].shape
    NB = B // n_cores
    key = (S, NB)
    if key not in _NC_CACHE:
        _NC_CACHE[key] = build_nc(S=S, NB=NB, L=2)
    nc = _NC_CACHE[key]
    maps = make_in_maps(inputs, n_cores, NB)
    res = run_bass_kernel_spmd(nc, maps, core_ids=list(range(n_cores)))
    out = np.concatenate([np.asarray(r["out"]) for r in res.results], axis=0)
    return out.astype(np.float32)
```

```python
import numpy as np
from contextlib import ExitStack
import concourse.bass as bass
import concourse.mybir as mybir
from concourse.bass_utils import run_bass_kernel_spmd

F32 = mybir.dt.float32
BF16 = mybir.dt.bfloat16
I32 = mybir.dt.int32
AF = mybir.ActivationFunctionType
ALU = mybir.AluOpType
AX = mybir.AxisListType

P = 128
D = 1024
KC = 8
NE = 32
DE = 256
IN_W = 1952
ALPHA = float((2 * 2) ** 0.25)
LN_EPS = 1e-5
RMS_EPS = 1e-6
PI = float(np.pi)
NDMA = 24
DEBUG_MG = False
DBG = set()


class Sched:
    ENG = ("pe", "act", "dve", "pool", "sp")

    def __init__(self, nc, es):
        self.nc = nc
        self.sem = {e: es.enter_context(nc.semaphore("s_" + e)) for e in self.ENG}
        self.dsem = [es.enter_context(nc.semaphore("s_dma%d" % i)) for i in range(NDMA)]
        self.cnt = {e: 0 for e in self.ENG}
        self.dcnt = [0] * NDMA
        self.drr = {"sp": 0, "pool": 0}
        self.known = {e: {} for e in self.ENG}
        self.prog = {e: [] for e in self.ENG}
        self.last_w = {}
        self.readers = {}
        self.out_tokens = []
        self.defer = None

    def _deps(self, eng, r, w):
        deps = {}

        def add(tok):
            if tok is None:
                return
            k, v = tok
            if deps.get(k, 0) < v:
                deps[k] = v

        for k in r:
            add(self.last_w.get(k))
            if isinstance(k, tuple) and k[0] in ("Zb", "WT", "M"):
                for tok in self.readers.get(k, {}).items():
                    if tok[0] != eng:
                        add(tok)
        for k in w:
            add(self.last_w.get(k))
            for tok in self.readers.get(k, {}).items():
                add(tok)
        waits = []
        kn = self.known[eng]
        for k, v in deps.items():
            if k == eng and eng == "pe":
                continue
            if kn.get(k, 0) >= v:
                continue
            kn[k] = v
            waits.append((k, v))
        return waits

    def _commit(self, tok, r, w):
        for k in w:
            self.last_w[k] = tok
            self.readers[k] = {}
        for k in r:
            if k in w:
                continue
            d = self.readers.setdefault(k, {})
            if d.get(tok[0], 0) < tok[1]:
                d[tok[0]] = tok[1]

    def flush(self, lst):
        d, self.defer = self.defer, None
        for item in lst:
            if item[0] == "dma":
                self.dma(*item[1:])
            else:
                self.op(*item)
        self.defer = d

    def op(self, eng, fn, r=(), w=()):
        if self.defer is not None:
            self.defer.append((eng, fn, tuple(r), tuple(w)))
            return
        waits = self._deps(eng, r, w)
        self.cnt[eng] += 1
        tok = (eng, self.cnt[eng])
        self.prog[eng].append((waits, fn, (eng, 1)))
        self._commit(tok, r, w)

    def dma(self, q, fn, r=(), w=(), is_out=False):
        if self.defer is not None:
            self.defer.append(("dma", q, fn, tuple(r), tuple(w), is_out))
            return
        half = NDMA // 2
        i = self.drr[q] + (0 if q == "sp" else half)
        self.drr[q] = (self.drr[q] + 1) % half
        waits = self._deps(q, r, w)
        dk = ("d", i)
        prev = self.dcnt[i]
        if prev > 0 and self.known[q].get(dk, 0) < prev:
            self.known[q][dk] = prev
            waits.append((dk, prev))
        self.dcnt[i] += 16
        tok = (dk, self.dcnt[i])
        self.prog[q].append((waits, fn, (dk, 16)))
        self._commit(tok, r, w)
        if is_out:
            self.out_tokens.append(tok)

    def barrier(self):
        for e in self.ENG:
            waits = []
            for o in self.ENG:
                if o != e and self.cnt[o] > self.known[e].get(o, 0):
                    self.known[e][o] = self.cnt[o]
                    waits.append((o, self.cnt[o]))
            for i in range(NDMA):
                dk = ("d", i)
                if self.dcnt[i] > self.known[e].get(dk, 0):
                    self.known[e][dk] = self.dcnt[i]
                    waits.append((dk, self.dcnt[i]))
            if waits:
                self.prog[e].append((waits, None, None))

    def finish(self):
        best = {}
        for (dk, v) in self.out_tokens:
            if best.get(dk, 0) < v:
                best[dk] = v
        self.prog["sp"].append((list(best.items()), None, None))

    def _semof(self, k):
        if isinstance(k, tuple):
            return self.dsem[k[1]]
        return self.sem[k]

    def replay(self, block):
        sections = {"pe": block.tensor, "act": block.scalar, "dve": block.vector,
                    "pool": block.gpsimd, "sp": block.sync}
        for en in self.ENG:
            prog = self.prog[en]

            def body(e, prog=prog):
                for waits, fn, inc in prog:
                    for (k, v) in waits:
                        e.wait_ge(self._semof(k), v)
                    if fn is not None:
                        ins = fn(e)
                        ins.then_inc(self._semof(inc[0]), inc[1])

            sections[en](body)


def build_nc(S=2048, NB=2, L=2, TQ=512, KCH=1024, NEXP=NE, KSB=512, KML=1024, TMOE=256):
    T = min(TQ, S)
    NT = S // T
    NS = T // P
    TT = S // P
    nc = bass.Bass("TRN2", target_bir_lowering=False)

    def din(name, shape, dt=F32):
        return nc.dram_tensor(name, list(shape), dt, kind="ExternalInput").ap()

    x_d = din("x", [NB, S, D])
    cT_d = din("cT", [P, KC * NB])
    posr_d = din("posr", [NB, 32, S], I32)
    cst_d = din("cst", [P, 4])
    ada_w_d = din("ada_w", [L, D, 6 * D])
    ada_bT_d = din("ada_bT", [P, L * 48])
    w_in_d = din("w_in", [L, D, IN_W])
    w_kr_d = din("w_kr", [L, D, 64])
    qn_d = din("qn", [P, L * 2])
    kvn_d = din("kvn", [P, L])
    w_uq_d = din("w_uq", [L, 256, 1024])
    w_uqs_d = din("w_uqs", [L, 256, 256])
    wk_d = din("wk_p", [L, 128, 1024])
    wv_d = din("wv_p", [L, 128, 512])
    w_o_d = din("w_o", [L, D, D])
    lnp_d = din("lnp", [P, L * 4 * KC])
    rw_d = din("router_w", [D, NE])
    rb_d = din("rbias", [P, NE])
    wg_d = din("w_gate", [L, NE, D, DE])
    wu_d = din("w_up", [L, NE, D, DE])
    wd_d = din("w_down", [L, NE, DE, D])
    out_d = nc.dram_tensor("out", [NB, S, D], F32, kind="ExternalOutput").ap()
    scr_d = nc.dram_tensor("xscr", [NB, P, KC, S], F32, kind="Internal").ap()
    dbg_d = nc.dram_tensor("dbg_mg", [P, KC, S], BF16, kind="ExternalOutput").ap() if DEBUG_MG else None

    with ExitStack() as es:
        sc = Sched(nc, es)

        sb_cache = {}

        def sb(name, shape, dt, stack=es):
            if name not in sb_cache:
                sb_cache[name] = stack.enter_context(nc.sbuf_tensor("sb_" + name, list(shape), dt))
            return sb_cache[name]

        Z = [es.enter_context(nc.psum_tensor("Z%d" % i, [P, 1024], F32)) for i in range(2)]
        WTp = es.enter_context(nc.psum_tensor("WTp", [P, 1024], F32))
        M = [es.enter_context(nc.psum_tensor("M%d" % i, [P, 512], F32)) for i in range(2)]
        WTb = WTp[:, :].bitcast(BF16)

        def MK(i, a=0, b=512):
            return [("M", i)]

        def mm(out, lhsT, rhs, start, stop, r, w):
            sc.op("pe", lambda e: e.matmul(out, lhsT=lhsT, rhs=rhs, start=start, stop=stop), r, w)

        def tr(out, in_, ident, r, w):
            sc.op("pe", lambda e: e.transpose(out, in_, ident), r, w)

        def act(out, in_, func, r, w, bias=None, scale=None, accum_out=None):
            kw = {}
            if bias is not None:
                kw["bias"] = bias
            if scale is not None:
                kw["scale"] = scale
            if accum_out is not None:
                kw["accum_out"] = accum_out
            sc.op("act", lambda e: e.activation(out, in_, func, **kw), r, w)

        def tt(eng, out, in0, in1, op, r, w):
            sc.op(eng, lambda e: e.tensor_tensor(out, in0, in1, op), r, w)

        def ts(eng, out, in0, s1, s2, op0, op1, r, w):
            if s2 is None:
                sc.op(eng, lambda e: e.tensor_scalar(out, in0, s1, None, op0), r, w)
            else:
                sc.op(eng, lambda e: e.tensor_scalar(out, in0, s1, s2, op0, op1), r, w)

        def stt(eng, out, in0, scalar, in1, op0, op1, r, w):
            sc.op(eng, lambda e: e.scalar_tensor_tensor(out, in0, scalar, in1, op0, op1), r, w)

        def cp(eng, out, in_, r, w):
            if eng == "act":
                sc.op("act", lambda e: e.activation(out, in_, AF.Copy), r, w)
            else:
                sc.op(eng, lambda e: e.tensor_copy(out, in_), r, w)

        def dma(q, out, in_, r, w, is_out=False):
            sc.dma(q, lambda e: e.dma_start(out=out, in_=in_), r, w, is_out=is_out)

        ident_f = sb("ident_f", [P, P], F32)
        ident_b = sb("ident_b", [P, P], BF16)
        ones_f = sb("ones_f", [P, P], F32)
        mask_s = sb("mask_s", [P, P], F32)
        mask_sb = sb("mask_sb", [P, P], BF16)
        negm = sb("negm", [P, P], F32)
        maskT = sb("maskT", [P, P], BF16)
        ones_b = sb("ones_b", [P, P], BF16)
        scan1 = sb("scan1", [P, KCH], F32)
        selrows = sb("selrows", [32, NE * P], BF16)
        cst = sb("cst", [P, 4], F32)
        modT = sb("modT", [P, L * NB * 48], F32)
        ada_bT = sb("ada_bT", [P, L * 48], F32)
        lnp = sb("lnp", [P, L * 4 * KC], F32)
        qn = sb("qn", [P, L * 2], F32)
        kvn = sb("kvn", [P, L], F32)
        rw = sb("rw", [P, KC, NE], F32)
        rb = sb("rb", [P, NE], F32)
        rbt = sb("rbt", [P, NS * NE], F32)
        cact = sb("cact", [P, KC * NB], F32)
        hT = sb("hT", [P, KC, S], BF16)
        xt = sb("xt", [P, KC, T], F32)
        tmpA = sb("tmpA", [P, T], F32)
        tmpB = sb("tmpB", [P, T], F32)
        tmpC = sb("tmpC", [P, T], F32)
        tmpD = sb("tmpD", [P, T], F32)
        cb16 = [sb("cb16_%d" % i, [P, T], BF16) for i in range(2)]
        sq16 = [sb("sq16_%d" % i, [P, T], BF16) for i in range(2)]
        cwT = sb("cwT", [32, 2, S], BF16)
        rope = sb("rope", [P, 2, S], BF16)
        xin = sb("xin", [P, D], F32)
        xin2 = sb("xin2", [P, D], F32)
        xins = [(xin, "xin0"), (xin2, "xin1")]
        io_cnt = [0]

        def mod(l, b, j):
            o = (l * NB + b) * 48 + j
            return modT[:, o:o + 1]

        def lnpar(l, which, c):
            o = (l * 4 + which) * KC + c
            return lnp[:, o:o + 1]

        sc.op("pool", lambda e: e.memset(ident_f[:], 1.0), w=["ident_f"])
        sc.op("pool", lambda e: e.affine_select(out=ident_f[:], in_=ident_f[:], pattern=[[-1, P]],
                                                compare_op=ALU.is_equal, fill=0.0, base=0,
                                                channel_multiplier=1), r=["ident_f"], w=["ident_f"])
        cp("pool", ident_b[:], ident_f[:], ["ident_f"], ["ident_b"])
        sc.op("pool", lambda e: e.memset(ones_f[:], 1.0), w=["ones_f"])
        sc.op("pool", lambda e: e.memset(scan1[:], 1.0), w=["scan1"])
        sc.op("pool", lambda e: e.memset(mask_s[:], 1.0), w=["mask_s"])
        sc.op("pool", lambda e: e.affine_select(out=mask_s[:], in_=mask_s[:], pattern=[[-1, P]],
                                                compare_op=ALU.is_gt, fill=0.0, base=0,
                                                channel_multiplier=1), r=["mask_s"], w=["mask_s"])
        cp("pool", mask_sb[:], mask_s[:], ["mask_s"], ["mask_sb"])
        sc.op("pool", lambda e: e.memset(negm[:], 0.0), w=["negm"])
        sc.op("pool", lambda e: e.affine_select(out=negm[:], in_=negm[:], pattern=[[-1, P]],
                                                compare_op=ALU.is_ge, fill=-30000.0, base=0,
                                                channel_multiplier=1), r=["negm"], w=["negm"])
        ts("dve", maskT[:], mask_s[:], -1.0, 1.0, ALU.mult, ALU.add, ["mask_s"], ["maskT"])
        cp("dve", ones_b[:], ones_f[:], ["ones_f"], ["ones_b"])
        sc.op("pool", lambda e: e.memset(selrows[:], 1.0), w=["selrows"])
        sc.op("pool", lambda e: e.affine_select(
            out=selrows[:].rearrange("k (e m) -> k e m", m=P), in_=selrows[:].rearrange("k (e m) -> k e m", m=P),
            pattern=[[-1, NE], [0, P]], compare_op=ALU.is_equal, fill=0.0, base=0,
            channel_multiplier=1), r=["selrows"], w=["selrows"])

        dma("sp", cst[:], cst_d, [], ["cst"])
        dma("sp", ada_bT[:], ada_bT_d, [], ["ada_bT"])
        dma("sp", lnp[:], lnp_d, [], ["lnp"])
        dma("sp", qn[:], qn_d, [], ["qn"])
        dma("sp", kvn[:], kvn_d, [], ["kvn"])
        dma("sp", rw[:], rw_d.rearrange("(c p) n -> p c n", p=P), [], ["rw"])
        dma("sp", rb[:], rb_d, [], ["rb"])
        for s_ in range(NS):
            dma("sp", rbt[:, s_ * NE:(s_ + 1) * NE], rb_d, [], ["rbt"])
        dma("sp", cact[:], cT_d, [], ["cact"])
        act(tmpA[:, 0:KC * NB], cact[:], AF.Exp, ["cact"], ["tmpA"], scale=-1.0)
        ts("dve", tmpA[:, 0:KC * NB], tmpA[:, 0:KC * NB], 1.0, None, ALU.add, None, ["tmpA"], ["tmpA"])
        sc.op("dve", lambda e: e.reciprocal(tmpA[:, 0:KC * NB], tmpA[:, 0:KC * NB]), ["tmpA"], ["tmpA"])
        tt("dve", cact[:], cact[:], tmpA[:, 0:KC * NB], ALU.mult, ["cact", "tmpA"], ["cact"])

        with ExitStack() as es0:
            awst = [sb("awst%d" % i, [P, KC, 512], BF16, es0) for i in range(3)]
            cact16 = sb("cact16", [P, KC * NB], BF16, es0)
            cp("dve", cact16[:], cact[:], ["cact"], ["cact16"])
            gi = 0
            for l in range(L):
                for jg in range(12):
                    bufi = gi % 3
                    gi += 1
                    aw = awst[bufi]
                    dma("pool", aw[:], ada_w_d[l, :, jg * 512:(jg + 1) * 512].rearrange("(c p) n -> p c n", p=P),
                        [], [("awst", bufi)])
                    for jj in range(4):
                        j = jg * 4 + jj
                        for kc in range(KC):
                            mm(M[jj % 2][:, 0:NB], aw[:, kc, jj * P:(jj + 1) * P],
                               cact16[:, kc * NB:(kc + 1) * NB], kc == 0, kc == KC - 1,
                               [("awst", bufi), "cact16"], MK(jj % 2))
                        for b in range(NB):
                            ts("dve", mod(l, b, j), M[jj % 2][:, b:b + 1], ada_bT[:, l * 48 + j:l * 48 + j + 1], None,
                               ALU.add, None, MK(jj % 2) + ["ada_bT"], ["modT"])
                for b in range(NB):
                    for (lo, hi) in ((8, 24), (32, 48)):
                        o = (l * NB + b) * 48
                        ts("dve", modT[:, o + lo:o + hi], modT[:, o + lo:o + hi], 1.0, None, ALU.add, None,
                           ["modT"], ["modT"])
            sc.barrier()

        def stats(src_keys, xt=xt):
            for c in range(KC):
                i = c % 2
                cp("dve", cb16[i][:], xt[:, c, :], src_keys, [("cb16", i)])
                mm(M[0][:, 0:T], ones_b[:], cb16[i][:], c == 0, c == KC - 1, [("cb16", i), "ones_b"], MK(0))
                act(sq16[i][:], xt[:, c, :], AF.Square, src_keys, [("sq16", i)])
                mm(M[1][:, 0:T], ones_b[:], sq16[i][:], c == 0, c == KC - 1, [("sq16", i), "ones_b"], MK(1))
            ts("dve", tmpA[:], M[0][:, 0:T], 1.0 / D, None, ALU.mult, None, MK(0), ["tmpA"])
            tt("dve", tmpC[:], tmpA[:], tmpA[:], ALU.mult, ["tmpA"], ["tmpC"])
            stt("dve", tmpC[:], M[1][:, 0:T], 1.0 / D, tmpC[:], ALU.mult, ALU.subtract, MK(1) + ["tmpC"], ["tmpC"])
            ts("dve", tmpC[:], tmpC[:], LN_EPS, None, ALU.add, None, ["tmpC"], ["tmpC"])
            act(tmpC[:], tmpC[:], AF.Ln, ["tmpC"], ["tmpC"])
            act(tmpB[:], tmpC[:], AF.Exp, ["tmpC"], ["tmpB"], scale=-0.5)

        def normalize(c, out_ap, scale_ap, bias_ap, src_keys, out_keys, second_out=None, xt=xt):
            tbuf, tk = (tmpD, "tmpD") if c % 2 == 0 else (tmpC, "tmpC")
            tt("dve", tbuf[:], xt[:, c, :], tmpA[:], ALU.subtract, src_keys + ["tmpA"], [tk])
            tt("dve", tbuf[:], tbuf[:], tmpB[:], ALU.mult, [tk, "tmpB"], [tk])
            act(out_ap, tbuf[:], AF.Identity, [tk, "modT", "lnp"], out_keys, bias=bias_ap, scale=scale_ap)
            if second_out is not None:
                o2, k2 = second_out
                act(o2, tbuf[:], AF.Identity, [tk, "modT", "lnp"], k2, bias=bias_ap, scale=scale_ap)

        def adaln_to_hT(l, b, t, which, second=None, xt=xt, xk="xt"):
            stats([xk], xt)
            base = 0 if which == 0 else 24
            for c in range(KC):
                so = None
                if second is not None:
                    so = (second[:, c, :], ["h2f"])
                normalize(c, hT[:, c, t * T:(t + 1) * T], mod(l, b, base + 8 + c), mod(l, b, base + c),
                          [xk], [("hT", t)], so, xt)

        def spill(b, t, xt=xt, xk="xt"):
            dma("sp", scr_d[b, :, :, t * T:(t + 1) * T], xt[:], [xk], [("scr", b, t)])

        def reload(b, t, xt=xt, xk="xt"):
            dma("sp", xt[:], scr_d[b, :, :, t * T:(t + 1) * T], [("scr", b, t)], [xk])

        for b in range(NB):
            with ExitStack() as esr:
                posi = sb("posi", [P, S], I32, esr)
                ang = sb("ang", [P, S], F32, esr)
                ang2 = sb("ang2", [P, S], F32, esr)
                kf = sb("kf", [P, S], F32, esr)
                R = slice(0, 32)
                dma("sp", posi[R, :], posr_d[b], [], ["posi"])
                cp("dve", ang[R, :], posi[R, :], ["posi"], ["ang"])
                ts("dve", ang[R, :], ang[R, :], cst[R, 0:1], None, ALU.mult, None, ["ang", "cst"], ["ang"])
                C1 = 6.28125
                C2 = 2.0 * PI - C1
                for which, shift in ((0, 0.5 * PI), (1, 0.0)):
                    if shift != 0.0:
                        ts("dve", ang2[R, :], ang[R, :], shift, None, ALU.add, None, ["ang"], ["ang2"])
                    else:
                        cp("dve", ang2[R, :], ang[R, :], ["ang"], ["ang2"])
                    ts("dve", posi[R, :], ang2[R, :], 1.0 / (2.0 * PI), None, ALU.mult, None, ["ang2"], ["posi"])
                    cp("dve", kf[R, :], posi[R, :], ["posi"], ["kf"])
                    stt("dve", ang2[R, :], kf[R, :], -C1, ang2[R, :], ALU.mult, ALU.add, ["kf", "ang2"], ["ang2"])
                    stt("dve", ang2[R, :], kf[R, :], -C2, ang2[R, :], ALU.mult, ALU.add, ["kf", "ang2"], ["ang2"])
                    ts("dve", kf[R, :], ang2[R, :], PI, -2.0 * PI, ALU.is_gt, ALU.mult, ["ang2"], ["kf"])
                    tt("dve", ang2[R, :], ang2[R, :], kf[R, :], ALU.add, ["ang2", "kf"], ["ang2"])
                    ts("dve", ang2[R, :], ang2[R, :], -3.1415925, 3.1415925, ALU.max, ALU.min, ["ang2"], ["ang2"])
                    act(ang2[R, :], ang2[R, :], AF.Sin, ["ang2"], ["ang2"])
                    if which == 0:
                        cp("dve", rope[R, 0, :], ang2[R, :], ["ang2"], ["rope"])
                    else:
                        ts("dve", rope[R, 1, :], ang2[R, :], cst[R, 1:2], None, ALU.mult, None,
                           ["ang2", "cst"], ["rope"])
                sc.barrier()

            for l in range(L):
                if l == 0:
                    for t in range(NT):
                        for s_ in range(NS):
                            tok0 = t * T + s_ * P
                            xi, xik = xins[io_cnt[0] % 2]
                            io_cnt[0] += 1
                            dma("sp", xi[:], x_d[b, tok0:tok0 + P, :], [], [xik])
                            for half in range(2):
                                Zt = Z[half]
                                for c4 in range(4):
                                    c = half * 4 + c4
                                    tr(Zt[:, c4 * P:(c4 + 1) * P], xi[:, c * P:(c + 1) * P], ident_f[:],
                                       [xik, "ident_f"], [("Zb", 2 * half)])
                                for c4 in range(4):
                                    c = half * 4 + c4
                                    cp("act" if c4 % 2 else "dve", xt[:, c, s_ * P:(s_ + 1) * P],
                                       Zt[:, c4 * P:(c4 + 1) * P], [("Zb", 2 * half)], ["xt"])
                        spill(b, t)
                        adaln_to_hT(l, b, t, 0)
                    sc.barrier()

                with ExitStack() as esBC:
                    mergedT = sb("mergedT", [P, KC, S], BF16, esBC)
                    with ExitStack() as esB:
                        QT = sb("QT", [P, 2, S], BF16, esB)
                        KT = sb("KT", [P, 2, S], BF16, esB)
                        Vp = sb("Vp", [P, TT, P], BF16, esB)
                        lat = sb("lat", [P, 4096], F32, esB)
                        uT = lat[:, 0:S].bitcast(BF16).rearrange("p (c s) -> p c s", c=2)
                        ukvT = lat[:, 2048:2048 + S // 2].bitcast(BF16)
                        krT = lat[:, 3072:3072 + S // 2].bitcast(BF16)
                        wst = [sb("wst%d" % i, [P, KC, 384], BF16, esB) for i in range(2)]
                        wuq = sb("wuq", [P, 2, 1024], BF16, esB)
                        wuqs = sb("wuqs", [P, 2, 256], BF16, esB)
                        wk = sb("wk", [P, 1024], BF16, esB)
                        wv = sb("wv", [P, 512], BF16, esB)
                        kst = sb("kst", [P, 16], F32, esB)
                        nball = lat[:, 0:2048]
                        bball = lat[:, 2048:4096]
                        dbuf = [sb("dbuf%d" % i, [P, P], F32, esB) for i in range(4)]
                        wrall = sb("wrall", [P, 2048], BF16, esB)
                        wtsall = sb("wtsall", [P, 2048], BF16, esB)
                        opair = [sb("opair%d" % i, [P, P], BF16, esB) for i in range(2)]
                        rcs = [sb("rcs%d" % i, [P, 8], F32, esB) for i in range(8)]
                        rrs = [sb("rrs%d" % i, [P, 16], F32, esB) for i in range(4)]
                        state = {"row": 0, "wst": 0, "zc": 0, "oc": 0, "rc": 0}

                        def load_w(col_specs):
                            i = state["wst"] % 2
                            state["wst"] += 1
                            off = 0
                            for (src, ncols) in col_specs:
                                dma("pool", wst[i][:, :, off:off + ncols], src.rearrange("(c p) n -> p c n", p=P),
                                    [], [("wst", i)])
                                off += ncols
                            return wst[i], ("wst", i)

                        def proj_T(dst_fn, w_ap, wkey, ncolsM, scale=None, dst_keys=()):
                            for t in range(NT):
                                bi = t % 4
                                ps = Z[bi // 2][0:ncolsM, (bi % 2) * 512:(bi % 2) * 512 + T]
                                for c in range(KC):
                                    mm(ps, w_ap[:, c, :], hT[:, c, t * T:(t + 1) * T], c == 0, c == KC - 1,
                                       [wkey, ("hT", t)], [("Zb", bi)])
                                dst_fn(t, ps, ("Zb", bi))

                        NSTG = 7

                        def attn_rows(kind, hh, Kdim, pbase, qidx, vcol0, mcol, opi, tasks):
                            sm_scale = (64 + 32) ** -0.5
                            CH = KSB if kind == "sb" else KML
                            units = 1 if kind == "sb" else 2
                            for qb in range(TT):
                                F = (qb + 1) * P
                                q_ap = QT[pbase:pbase + Kdim, qidx, qb * P:(qb + 1) * P]
                                rr = rrs[state["row"] % 4]
                                rrk = ("rrs", state["row"] % 4)
                                state["row"] += 1
                                chunks = []
                                f1 = F
                                while f1 > 0:
                                    f0 = max(0, f1 - CH)
                                    chunks.append((f0, f1))
                                    f1 = f0
                                nch = len(chunks)
                                crec = []
                                if kind == "sb":
                                    osl = state["oc"] % 2
                                    state["oc"] += 1
                                for ci, (f0, f1) in enumerate(chunks):
                                    n = f1 - f0
                                    if kind == "sb":
                                        u = state["zc"] % 4
                                        state["zc"] += 1
                                        ukeys = [u]
                                    else:
                                        u = 2 * (state["zc"] % 2)
                                        state["zc"] += 1
                                        ukeys = [u, u + 1]
                                    c0 = u * 512
                                    zt = Z[u // 2][:, (u % 2) * 512:(u % 2) * 512 + units * 512]
                                    zk = [("Zb", k) for k in ukeys]
                                    nb, nk = nball[:, c0:c0 + units * 512], [("nb", k) for k in ukeys]
                                    bb, bk = bball[:, c0:c0 + units * 512], [("bb", k) for k in ukeys]
                                    db, dk = dbuf[u], [("dbuf", u)]
                                    wr, wrk = wrall[:, c0:c0 + units * 512], [("wr", k) for k in ukeys]
                                    wti = (u // units) % 2
                                    wt, wtk = WTb[:, wti * 1024:(wti + 1) * 1024], [("WT", wti)]
                                    wts, wtsk = wtsall[:, c0:c0 + units * 512], [("wts", k) for k in ukeys]
                                    rci = state["rc"] % 8
                                    state["rc"] += 1
                                    rc, rck = rcs[rci], [("rcs", rci)]
                                    stg = [[] for _ in range(NSTG)]
                                    tasks.append(stg)
                                    sc.defer = stg[0]
                                    for g0 in range(f0, f1, 512):
                                        g1 = min(f1, g0 + 512)
                                        mm(zt[:, g0 - f0:g1 - f0], q_ap, KT[pbase:pbase + Kdim, qidx, g0:g1],
                                           True, True, ["QT", "KT"], [("Zb", u + (g0 - f0) // 512)])
                                    lo_n = n
                                    if kind == "sb":
                                        sc.defer = stg[1]
                                        act(nb[:, 0:n], zt[:, 0:n], AF.Exp, zk, nk, scale=-1.0)
                                        act(nb[:, 0:n], nb[:, 0:n], AF.Ln, nk, nk, bias=1.0)
                                        sc.defer = stg[2]
                                        if ci == 0:
                                            d0 = n - P
                                            lo_n = d0
                                            tt("dve", db[:], zt[:, d0:n], nb[:, d0:n], ALU.add, zk + nk, dk)
                                            tt("dve", db[:], db[:], mask_s[:], ALU.mult, dk + ["mask_s"], dk)
                                            sc.op("dve", lambda e, bb=bb, db=db, d0=d0, n=n: e.tensor_tensor_scan(
                                                out=bb[:, d0:n][:, ::-1], data0=scan1[:, 0:P], data1=db[:, ::-1],
                                                initial=0.0, op0=ALU.mult, op1=ALU.add),
                                                dk + ["scan1"], bk)
                                            cp("dve", rr[:, 0:1], bb[:, d0:d0 + 1], bk, [rrk])
                                            tt("dve", bb[:, d0:n], bb[:, d0:n], zt[:, d0:n], ALU.subtract, bk + zk, bk)
                                        if lo_n > 0:
                                            tt("dve", bb[:, lo_n - 1:lo_n], rr[:, 0:1], nb[:, lo_n - 1:lo_n], ALU.add,
                                               [rrk] + nk, bk)
                                            if lo_n > 1:
                                                sc.op("dve", lambda e, zt=zt, nb=nb, bb=bb, lo_n=lo_n: e.tensor_tensor_scan(
                                                    out=bb[:, 0:lo_n - 1][:, ::-1], data0=zt[:, 1:lo_n][:, ::-1],
                                                    data1=nb[:, 0:lo_n - 1][:, ::-1], initial=bb[:, lo_n - 1:lo_n],
                                                    op0=ALU.add, op1=ALU.add),
                                                    zk + nk + bk, bk)
                                        if ci + 1 < nch:
                                            tt("dve", rr[:, 0:1], bb[:, 0:1], zt[:, 0:1], ALU.add, bk + zk, [rrk])
                                        sc.defer = stg[3]
                                        act(wr[:, 0:n], bb[:, 0:n], AF.Exp, bk, wrk, scale=-1.0)
                                        if ci == 0:
                                            tt("pool", wr[:, n - P:n], wr[:, n - P:n], mask_sb[:], ALU.mult,
                                               wrk + ["mask_sb"], wrk)
                                    else:
                                        sc.defer = stg[1]
                                        sc.op("dve", lambda e, rc=rc, zt=zt, n=n: e.reduce_max(rc[:, 0:1], zt[:, 0:n], AX.X),
                                              zk, rck)
                                        ts("dve", rc[:, 1:2], rc[:, 0:1], -sm_scale, None, ALU.mult, None, rck, rck)
                                        sc.op("dve", lambda e, rc=rc: e.memset(rc[:, 2:4], 0.0), [], rck)
                                        if ci == 0:
                                            d0 = n - P
                                            lo_n = d0
                                            tt("dve", db[:], zt[:, d0:n], negm[:], ALU.add, zk + ["negm"], dk)
                                        sc.defer = stg[2]
                                        if ci == 0:
                                            act(wr[:, d0:n], db[:], AF.Exp, dk + rck, wrk + rck,
                                                bias=rc[:, 1:2], scale=sm_scale, accum_out=rc[:, 2:3])
                                        if lo_n > 0:
                                            act(wr[:, 0:lo_n], zt[:, 0:lo_n], AF.Exp, zk + rck, wrk + rck,
                                                bias=rc[:, 1:2], scale=sm_scale, accum_out=rc[:, 3:4])
                                        tt("dve", rc[:, 4:5], rc[:, 2:3], rc[:, 3:4], ALU.add, rck, rck)
                                        osl = state["oc"] % 2
                                        state["oc"] += 1
                                    sc.defer = stg[4]
                                    nblk = n // P
                                    for jj in range(nblk):
                                        tr(wt[:, jj * P:(jj + 1) * P], wr[:, jj * P:(jj + 1) * P], ident_b[:],
                                           wrk + ["ident_b"], wtk)
                                    sc.defer = stg[5]
                                    cp("act" if (u % 4 == 3) else "dve", wts[:, 0:n], wt[:, 0:n], wtk, wtsk)
                                    sc.defer = stg[6]
                                    ops = M[osl][:, 0:64]
                                    opk = MK(osl)
                                    for jj in range(nblk):
                                        if kind == "sb":
                                            st = (ci == 0 and jj == 0)
                                            sp_ = (ci == nch - 1 and jj == nblk - 1)
                                        else:
                                            st = (jj == 0)
                                            sp_ = (jj == nblk - 1)
                                        mm(ops, wts[:, jj * P:(jj + 1) * P], Vp[:, f0 // P + jj, vcol0:vcol0 + 64], st, sp_,
                                           wtsk + ["Vp"], opk)
                                    crec.append((ops, opk, rc, rck))
                                op_ = opair[opi[0] % 2]
                                ok_ = ("opair", opi[0] % 2)
                                dst = op_[:, 64 * hh:64 * hh + 64]
                                if kind == "sb":
                                    ops, opk, _, _ = crec[-1]
                                    cp("dve", dst, ops, opk, [ok_])
                                elif nch == 1:
                                    ops, opk, rc, rck = crec[0]
                                    sc.op("dve", lambda e, rc=rc: e.reciprocal(rc[:, 5:6], rc[:, 4:5]), rck, rck)
                                    ts("dve", dst, ops, rc[:, 5:6], None, ALU.mult, None, opk + rck, [ok_])
                                else:
                                    assert nch == 2
                                    (o0, ok0, r0, rk0), (o1, ok1, r1, rk1) = crec
                                    tt("dve", rr[:, 2:3], r0[:, 0:1], r1[:, 0:1], ALU.max, rk0 + rk1, [rrk])
                                    ts("dve", rr[:, 3:4], rr[:, 2:3], -sm_scale, None, ALU.mult, None, [rrk], [rrk])
                                    act(rr[:, 4:5], r0[:, 0:1], AF.Exp, rk0 + [rrk], [rrk], bias=rr[:, 3:4], scale=sm_scale)
                                    act(rr[:, 5:6], r1[:, 0:1], AF.Exp, rk1 + [rrk], [rrk], bias=rr[:, 3:4], scale=sm_scale)
                                    tt("dve", rr[:, 6:7], rr[:, 4:5], r0[:, 4:5], ALU.mult, [rrk] + rk0, [rrk])
                                    stt("dve", rr[:, 7:8], rr[:, 5:6], r1[:, 4:5], rr[:, 6:7], ALU.mult, ALU.add,
                                        [rrk] + rk1, [rrk])
                                    sc.op("dve", lambda e, rr=rr: e.reciprocal(rr[:, 8:9], rr[:, 7:8]), [rrk], [rrk])
                                    ts("dve", rr[:, 4:6], rr[:, 4:6], rr[:, 8:9], None, ALU.mult, None, [rrk], [rrk])
                                    ts("dve", dst, o0, rr[:, 4:5], None, ALU.mult, None, ok0 + [rrk], [ok_])
                                    stt("dve", dst, o1, rr[:, 5:6], dst, ALU.mult, ALU.add, ok1 + [rrk, ok_], [ok_])
                                if hh == 1:
                                    finish_pair(qb, op_, ok_, mcol)
                                sc.defer = None
                                yield qb

                        def finish_pair(qb, op_, ok_, mcol):
                            k = state["oc"] % 2
                            state["oc"] += 1
                            tps = M[k][:, 0:64].bitcast(BF16)
                            tr(tps, op_[:], ident_b[:], [ok_, "ident_b"], MK(k))
                            cp("dve", mergedT[:, mcol, qb * P:(qb + 1) * P], tps, MK(k), [("mg", mcol)])

                        def run_pair(kind, Kdim, pbases, qidxs, vcols, mcol):
                            opi = [0]
                            tasks = []
                            g0 = attn_rows(kind, 0, Kdim, pbases[0], qidxs[0], vcols[0], mcol, opi, tasks)
                            g1 = attn_rows(kind, 1, Kdim, pbases[1], qidxs[1], vcols[1], mcol, opi, tasks)
                            for _ in range(TT):
                                next(g0)
                                next(g1)
                                opi[0] += 1
                            nt = len(tasks)
                            for step in range(nt + NSTG - 1):
                                for sg_ in range(NSTG - 1, -1, -1):
                                    k = step - sg_
                                    if 0 <= k < nt:
                                        sc.flush(tasks[k][sg_])

                        for pr in range(4):
                            wt_, wkey = load_w([(w_in_d[l, :, pr * P:(pr + 1) * P], P),
                                                (w_in_d[l, :, 512 + pr * P:512 + (pr + 1) * P], P),
                                                (w_in_d[l, :, 1024 + pr * P:1024 + (pr + 1) * P], P)])

                            def put_q(t, ps, zk):
                                act(QT[:, 0, t * T:(t + 1) * T], ps, AF.Copy, [zk], ["QT"], scale=0.125)

                            def put_k(t, ps, zk):
                                cp("dve", KT[:, 0, t * T:(t + 1) * T], ps, [zk], ["KT"])

                            proj_T(put_q, wt_[:, :, 0:P], wkey, P)
                            proj_T(put_k, wt_[:, :, P:2 * P], wkey, P)
                            for j in range(TT):
                                t = (j * P) // T
                                ps = M[j % 2][:, 0:P]
                                pk = MK(j % 2, 0, P)
                                for c in range(KC):
                                    mm(ps, hT[:, c, j * P:(j + 1) * P], wt_[:, c, 2 * P:3 * P], c == 0, c == KC - 1,
                                       [wkey, ("hT", t)], pk)
                                cp("act" if j % 2 else "dve", Vp[:, j, :], ps, pk, ["Vp"])
                            run_pair("sb", 64, (0, 64), (0, 0), (0, 64), pr)

                        sc.barrier()
                        wt_, wkey = load_w([(w_in_d[l, :, 1536:1792], 256)])
                        wt2_, wkey2 = load_w([(w_in_d[l, :, 1792:1920], 128), (w_kr_d[l, :, 0:64], 64)])
                        dma("pool", wuq[:], w_uq_d[l].rearrange("(c p) n -> p c n", p=P), [], ["wuq"])
                        dma("pool", wuqs[:], w_uqs_d[l].rearrange("(c p) n -> p c n", p=P), [], ["wuqs"])
                        dma("pool", wk[:], wk_d[l], [], ["wk"])
                        dma("pool", wv[:], wv_d[l], [], ["wv"])
                        R = slice(0, 32)
                        for t in range(NT if "nolat" not in DBG else 0):
                            cols = slice(t * T, (t + 1) * T)
                            for c2 in range(2):
                                ps = Z[0][:, c2 * 512:c2 * 512 + T]
                                for c in range(KC):
                                    mm(ps, wt_[:, c, c2 * P:(c2 + 1) * P], hT[:, c, cols], c == 0, c == KC - 1,
                                       [wkey, ("hT", t)], [("Zb", c2)])
                                act(tmpA[:] if c2 == 0 else tmpB[:], ps, AF.Square, [("Zb", c2)],
                                    ["tmpA" if c2 == 0 else "tmpB"])
                            mm(M[0][:, 0:T], ones_f[:], tmpA[:], True, False, ["tmpA", "ones_f"], MK(0))
                            mm(M[0][:, 0:T], ones_f[:], tmpB[:], False, True, ["tmpB", "ones_f"], MK(0))
                            act(tmpC[:], M[0][:, 0:T], AF.Ln, MK(0), ["tmpC"], bias=RMS_EPS, scale=1.0 / 256)
                            act(tmpC[:], tmpC[:], AF.Exp, ["tmpC"], ["tmpC"], scale=-0.5)
                            for c2 in range(2):
                                stt("dve", uT[:, c2, cols], Z[0][:, c2 * 512:c2 * 512 + T], qn[:, l * 2 + c2:l * 2 + c2 + 1],
                                    tmpC[:], ALU.mult, ALU.mult, [("Zb", c2), "qn", "tmpC"], ["uT"])
                            ps = Z[1][:, 0:T]
                            for c in range(KC):
                                mm(ps, wt2_[:, c, 0:P], hT[:, c, cols], c == 0, c == KC - 1, [wkey2, ("hT", t)], [("Zb", 2)])
                            act(tmpA[:], ps, AF.Square, [("Zb", 2)], ["tmpA"])
                            mm(M[1][:, 0:T], ones_f[:], tmpA[:], True, True, ["tmpA", "ones_f"], MK(1))
                            act(tmpD[:], M[1][:, 0:T], AF.Ln, MK(1), ["tmpD"], bias=RMS_EPS, scale=1.0 / 128)
                            act(tmpD[:], tmpD[:], AF.Exp, ["tmpD"], ["tmpD"], scale=-0.5)
                            stt("dve", ukvT[:, cols], ps, kvn[:, l:l + 1], tmpD[:], ALU.mult, ALU.mult,
                                [("Zb", 2), "kvn", "tmpD"], ["ukvT"])
                            psA = Z[1][0:32, 512:512 + T]
                            for c in range(KC):
                                mm(psA, wt2_[:, c, P:P + 32], hT[:, c, cols], c == 0, c == KC - 1, [wkey2, ("hT", t)], [("Zb", 3)])
                            psB = M[0][0:32, 0:T]
                            for c in range(KC):
                                mm(psB, wt2_[:, c, P + 32:P + 64], hT[:, c, cols], c == 0, c == KC - 1, [wkey2, ("hT", t)], MK(0))
                            tt("dve", tmpA[R, :], psA, rope[R, 0, cols], ALU.mult, [("Zb", 3), "rope"], ["tmpA"])
                            tt("dve", tmpB[R, :], psB, rope[R, 1, cols], ALU.mult, MK(0) + ["rope"], ["tmpB"])
                            tt("dve", krT[R, cols], tmpA[R, :], tmpB[R, :], ALU.add, ["tmpA", "tmpB"], ["krT"])

                        sm_scale = (64 + 32) ** -0.5
                        HS = S // 2
                        for pr in range(4 if "nomlaheads" not in DBG else 0):
                            for j in range(TT):
                                ps = M[j % 2][:, 0:P]
                                pk = MK(j % 2, 0, P)
                                mm(ps, ukvT[:, j * P:(j + 1) * P], wv[:, 128 * pr:128 * pr + 128], True, True, ["ukvT", "wv"], pk)
                                cp("act" if j % 2 else "dve", Vp[:, j, :], ps, pk, ["Vp"])
                            for hh in range(2):
                                h = 2 * pr + hh
                                qk_, kk_ = ("QTm", hh), ("KTm", hh)
                                for t in range(NT if "noproj" not in DBG else 0):
                                    cols = slice(t * T, (t + 1) * T)
                                    psA = Z[0][:, 0:T]
                                    psB = Z[0][0:32, 512:512 + T]
                                    for c2 in range(2):
                                        mm(psA, wuq[:, c2, P * h:P * h + P], uT[:, c2, cols], c2 == 0, c2 == 1,
                                           ["wuq", "uT"], [("Zb", 0)])
                                    for c2 in range(2 if "noB" not in DBG else 0):
                                        mm(psB, wuqs[:, c2, 32 * h:32 * h + 32], uT[:, c2, cols], c2 == 0, c2 == 1,
                                           ["wuqs", "uT"], [("Zb", 1)])
                                    if "noQcopy" not in DBG:
                                        cp("act", QT[:, hh, cols], Z[0][:, 0:T], [("Zb", 0)], [qk_])
                                    if "norope" not in DBG:
                                        tt("dve", tmpA[R, :], Z[0][R, 0:T], rope[R, 0, cols], ALU.mult, [("Zb", 0), "rope"], ["tmpA"])
                                    if "noB" not in DBG:
                                        tt("dve", tmpB[R, :], psB, rope[R, 1, cols], ALU.mult, [("Zb", 1), "rope"], ["tmpB"])
                                    if "norope" not in DBG:
                                        tt("dve", QT[R, hh, cols], tmpA[R, :], tmpB[R, :], ALU.add, ["tmpA", "tmpB"], [qk_])
                                    psK = Z[1][:, 0:T]
                                    if "noK" not in DBG:
                                        mm(psK, wk[:, P * h:P * h + P], ukvT[:, cols], True, True, ["wk", "ukvT"], [("Zb", 2)])
                                        cp("act", KT[:, hh, cols], Z[1][:, 0:T], [("Zb", 2)], [kk_])
                                if "nokr" not in DBG:
                                    cp("dve", KT[R, hh, :], krT[R, :], ["krT"], [kk_])
                                sqb = wtsall
                                if "nonrm" not in DBG:
                                    act(sqb[:, 0:S], KT[:, hh, :], AF.Square, [kk_], ["sqb"])
                                for t in range(NT if "nonrm" not in DBG else 0):
                                    mm(M[0][0:64, 0:T], ones_b[:, 0:64], sqb[:, t * T:(t + 1) * T], True, True, ["sqb", "ones_b"], MK(0))
                                    sc.op("dve", lambda e, t=t: e.reduce_max(kst[0:64, t:t + 1], M[0][0:64, 0:T], AX.X),
                                          MK(0), ["kst"])
                                if "nonrm" not in DBG:
                                    sc.op("dve", lambda e: e.reduce_max(kst[0:64, 8:9], kst[0:64, 0:NT], AX.X), ["kst"], ["kst"])
                                    act(kst[0:64, 9:10], kst[0:64, 8:9], AF.Ln, ["kst"], ["kst"])
                                    act(kst[0:64, 9:10], kst[0:64, 9:10], AF.Exp, ["kst"], ["kst"], scale=0.5)
                                    ts("dve", kst[0:64, 10:11], kst[0:64, 9:10], -1.0, None, ALU.mult, None, ["kst"], ["kst"])
                                    act(sqb[:, 0:S], QT[:, hh, :], AF.Square, [qk_], ["sqb"])
                                A1 = slice(32, 33)
                                for t in range(NT if "nonrm" not in DBG else 0):
                                    cols = slice(t * T, (t + 1) * T)
                                    mm(M[1][0:64, 0:T], ones_b[:, 0:64], sqb[:, cols], True, True, ["sqb", "ones_b"], MK(1))
                                    if "noaug" in DBG:
                                        continue
                                    act(tmpC[A1, :], M[1][A1, 0:T], AF.Ln, MK(1), ["tmpC"], bias=1e-20)
                                    act(tmpC[A1, :], tmpC[A1, :], AF.Exp, ["tmpC"], ["tmpC"], scale=0.5)
                                    ts("dve", QT[A1, hh, cols], tmpC[A1, :], kst[A1, 10:11], None, ALU.mult, None,
                                       ["tmpC", "kst"], [qk_])
                                if "noaug" not in DBG:
                                    sc.op("dve", lambda e, hh=hh: e.memset(KT[32:33, hh, :], 1.0), [kk_], [kk_])

                            mt = []
                            cnt = 0
                            for hh in range(2):
                                qk_, kk_ = ("QTm", hh), ("KTm", hh)
                                rows = slice(0, 64) if hh == 0 else slice(64, 128)
                                Mv = P
                                for half in range(2 if "noattn" not in DBG else 0):
                                    qlo, qhi = half * HS, (half + 1) * HS
                                    nj = qhi // P
                                    for j in range(nj):
                                        stg = [[] for _ in range(4)]
                                        mt.append(stg)
                                        zi = cnt % 2
                                        cnt += 1
                                        c0 = max(qlo, j * P)
                                        F = qhi - c0
                                        diag = (j * P >= qlo)
                                        zt = Z[zi]
                                        pT, pk_ = wrall[:, zi * 1024:zi * 1024 + F], ("pT", zi)
                                        sc.defer = stg[0]
                                        for g0 in range(0, F, 512):
                                            g1 = min(F, g0 + 512)
                                            mm(zt[:, g0:g1], KT[:, hh, j * P:(j + 1) * P], QT[:, hh, c0 + g0:c0 + g1],
                                               True, True, [qk_, kk_], [("Zb", 2 * zi + g0 // 512)])
                                        sc.defer = stg[1]
                                        act(pT, zt[:, 0:F], AF.Exp, [("Zb", 2 * zi + k) for k in range((F + 511) // 512)],
                                            [pk_], scale=sm_scale)
                                        sc.defer = stg[2]
                                        if diag:
                                            tt("dve", pT[:, 0:P], pT[:, 0:P], maskT[:], ALU.mult, [pk_, "maskT"], [pk_])
                                        sc.defer = stg[3]
                                        a0 = c0 - qlo
                                        for ab in range((HS + 511) // 512):
                                            lo = max(a0, ab * 512)
                                            hi = min(HS, (ab + 1) * 512)
                                            if lo >= hi:
                                                continue
                                            jl = min(nj, (qlo + hi) // P) - 1
                                            st, sp_ = (j == 0), (j == jl)
                                            mm(WTp[0:Mv, lo:hi], Vp[:, j, 0:Mv], pT[:, lo - a0:hi - a0], st, sp_,
                                               [pk_, "Vp"], [("WT", ab)])
                                            mm(M[ab][0:Mv, lo - ab * 512:hi - ab * 512], ones_b[:, 0:Mv], pT[:, lo - a0:hi - a0],
                                               st, sp_, [pk_, "ones_b"], MK(ab))
                                        if j == nj - 1:
                                            for ab in range((HS + 511) // 512):
                                                wdt = min(512, HS - ab * 512)
                                                tb, tk = (tmpA, "tmpA") if ab == 0 else (tmpB, "tmpB")
                                                if "rcp2" in DBG:
                                                    cp("dve", tb[rows, 0:wdt], M[ab][rows, 0:wdt], MK(ab), [tk])
                                                    sc.op("dve", lambda e, tb=tb, wdt=wdt, rows=rows: e.reciprocal(
                                                        tb[rows, 0:wdt], tb[rows, 0:wdt]), [tk], [tk])
                                                else:
                                                    sc.op("dve", lambda e, tb=tb, ab=ab, wdt=wdt, rows=rows: e.reciprocal(
                                                        tb[rows, 0:wdt], M[ab][rows, 0:wdt]), MK(ab), [tk])
                                                tt("dve", mergedT[rows, 4 + pr, qlo + ab * 512:qlo + ab * 512 + wdt],
                                                   WTp[rows, ab * 512:ab * 512 + wdt], tb[rows, 0:wdt], ALU.mult,
                                                   [("WT", ab), tk], [("mg", 4 + pr)])
                            sc.defer = None
                            nt_ = len(mt)
                            for step in range(nt_ + 3):
                                for sg_ in range(3, -1, -1):
                                    k = step - sg_
                                    if 0 <= k < nt_:
                                        sc.flush(mt[k][sg_])
                        sc.barrier()

                    if DEBUG_MG and b == 0 and l == 0:
                        dma("sp", dbg_d, mergedT[:], [("mg", c) for c in range(KC)], ["dbg"], is_out=True)
                    with ExitStack() as esC:
                        wo = sb("wo", [P, KC, D], BF16, esC)
                        h2f = sb("h2f", [P, KC, T], F32, esC)
                        rt = sb("rt", [P, 4 * NS * NE + NS * 64 + 2 * NS + 8], F32, esC)
                        for half in range(2):
                            dma("pool", wo[:, :, half * 512:(half + 1) * 512],
                                w_o_d[l, :, half * 512:(half + 1) * 512].rearrange("(c p) n -> p c n", p=P), [], ["wo"])
                        xt2 = sb("xt2", [P, KC, T], F32, esC)
                        xbufs = [(xt, "xt"), (xt2, "xt2")]
                        ctasks = []
                        for t in range(NT):
                            cols = slice(t * T, (t + 1) * T)
                            xb, xk = xbufs[t % 2]
                            stg = [[] for _ in range(4)]
                            ctasks.append(stg)
                            sc.defer = stg[0]
                            reload(b, t, xb, xk)
                            for dc in range(KC):
                                bi = dc % 4
                                ps = Z[bi // 2][:, (bi % 2) * 512:(bi % 2) * 512 + T]
                                zk = ("Zb", bi)
                                for c in range(KC):
                                    mm(ps, wo[:, c, dc * P:(dc + 1) * P], mergedT[:, c, cols], c == 0, c == KC - 1,
                                       ["wo", ("mg", c)], [zk])
                                act(xb[:, dc, :], xb[:, dc, :], AF.Copy, [xk], [xk], scale=ALPHA)
                                stt("dve", xb[:, dc, :], ps, mod(l, b, 16 + dc), xb[:, dc, :], ALU.mult, ALU.add,
                                    [zk, "modT", xk], [xk])
                            sc.defer = stg[1]
                            stats([xk], xb)
                            for c in range(KC):
                                normalize(c, xb[:, c, :], lnpar(l, 0, c), lnpar(l, 1, c), [xk], [xk], None, xb)
                            spill(b, t, xb, xk)
                            sc.defer = stg[2]
                            adaln_to_hT(l, b, t, 1, second=h2f, xt=xb, xk=xk)
                            sc.defer = stg[3]
                            W = NS * NE
                            lg = M[0][:, 0:W]
                            for s_ in range(NS):
                                for c in range(KC):
                                    mm(lg[:, s_ * NE:(s_ + 1) * NE], h2f[:, c, s_ * P:(s_ + 1) * P], rw[:, c, :],
                                       c == 0, c == KC - 1, ["h2f", "rw"], MK(0))
                            scv = rt[:, 0:W]
                            bs = rt[:, W:2 * W]
                            act(scv, lg, AF.Exp, MK(0), ["rt"], scale=-1.0)
                            ts("dve", scv, scv, 1.0, None, ALU.add, None, ["rt"], ["rt"])
                            sc.op("dve", lambda e, scv=scv: e.reciprocal(scv, scv), ["rt"], ["rt"])
                            tt("dve", bs, scv, rbt[:], ALU.add, ["rt", "rbt"], ["rt"])
                            b4 = bs.rearrange("p (s g f) -> p s g f", s=NS, f=4)
                            G8 = lambda i: rt[:, 2 * W + NS * 8 * i:2 * W + NS * 8 * (i + 1)].rearrange("p (s g) -> p s g", s=NS)
                            hi1, lo1, hi2, lo2, top1, sec, gs, gm = [G8(i) for i in range(8)]
                            tt("dve", hi1, b4[:, :, :, 0], b4[:, :, :, 1], ALU.max, ["rt"], ["rt"])
                            tt("dve", lo1, b4[:, :, :, 0], b4[:, :, :, 1], ALU.min, ["rt"], ["rt"])
                            tt("dve", hi2, b4[:, :, :, 2], b4[:, :, :, 3], ALU.max, ["rt"], ["rt"])
                            tt("dve", lo2, b4[:, :, :, 2], b4[:, :, :, 3], ALU.min, ["rt"], ["rt"])
                            tt("dve", top1, hi1, hi2, ALU.max, ["rt"], ["rt"])
                            tt("dve", hi1, hi1, hi2, ALU.min, ["rt"], ["rt"])
                            tt("dve", lo1, lo1, lo2, ALU.max, ["rt"], ["rt"])
                            tt("dve", sec, hi1, lo1, ALU.max, ["rt"], ["rt"])
                            tt("dve", gs, top1, sec, ALU.add, ["rt"], ["rt"])
                            o2 = 2 * W + NS * 64
                            gmx = rt[:, o2:o2 + NS]
                            gsum = rt[:, o2 + NS:o2 + 2 * NS]
                            sc.op("dve", lambda e, gmx=gmx, gs=gs: e.reduce_max(gmx, gs, AX.X), ["rt"], ["rt"])
                            for s_ in range(NS):
                                ts("dve", gm[:, s_, :], gs[:, s_, :], gmx[:, s_:s_ + 1], None, ALU.is_ge, None, ["rt"], ["rt"])
                            sel = rt[:, o2 + 2 * NS:o2 + 2 * NS + W]
                            s4 = sel.rearrange("p (s g f) -> p s g f", s=NS, f=4)
                            for i in range(4):
                                tt("dve", s4[:, :, :, i], b4[:, :, :, i], sec, ALU.is_ge, ["rt"], ["rt"])
                                tt("dve", s4[:, :, :, i], s4[:, :, :, i], gm, ALU.mult, ["rt"], ["rt"])
                            tt("dve", sel, sel, scv, ALU.mult, ["rt"], ["rt"])
                            sc.op("dve", lambda e, gsum=gsum, sel=sel: e.reduce_sum(
                                gsum, sel.rearrange("p (s e) -> p s e", s=NS), AX.X), ["rt"], ["rt"])
                            sc.op("dve", lambda e, gsum=gsum: e.reciprocal(gsum, gsum), ["rt"], ["rt"])
                            cwv = rt[:, o2 + 2 * NS + W:o2 + 2 * NS + 2 * W]
                            for s_ in range(NS):
                                ts("dve", cwv[:, s_ * NE:(s_ + 1) * NE], sel[:, s_ * NE:(s_ + 1) * NE], gsum[:, s_:s_ + 1],
                                   None, ALU.mult, None, ["rt"], ["rt"])
                            for s_ in range(NS):
                                tr(M[1][0:NE, s_ * P:(s_ + 1) * P], cwv[:, s_ * NE:(s_ + 1) * NE], ident_f[:],
                                   ["rt", "ident_f"], MK(1))
                            cp("dve", cwT[:, 0, cols], M[1][0:NE, 0:T], MK(1), ["cwT"])
                            tt("dve", cwT[:, 1, cols], M[1][0:NE, 0:T], cwT[:, 0, cols], ALU.subtract,
                               MK(1) + ["cwT"], ["cwT"])
                        sc.defer = None
                        for step in range(NT + 3):
                            for sg_ in range(3, -1, -1):
                                k = step - sg_
                                if 0 <= k < NT:
                                    sc.flush(ctasks[k][sg_])
                        sc.barrier()

                with ExitStack() as esD:
                    TM = min(TMOE, T)
                    NTM = S // TM
                    yacc = sb("yacc", [P, KC, S], F32, esD)
                    wall = sb("wall", [P, 6 * KC * DE], BF16, esD)
                    WSZ = KC * DE
                    wgs = [wall[:, i * WSZ:(i + 1) * WSZ].rearrange("p (c n) -> p c n", c=KC) for i in range(2)]
                    wus = [wall[:, (2 + i) * WSZ:(3 + i) * WSZ].rearrange("p (c n) -> p c n", c=KC) for i in range(2)]
                    wds = [wall[:, (4 + i) * WSZ:(5 + i) * WSZ].rearrange("p (j n) -> p j n", j=2) for i in range(2)]
                    xt2e = wall[:, 0:2 * KC * T].bitcast(F32).rearrange("p (c t) -> p c t", c=KC)
                    sgs = [sb("sg%d" % i, [P, 2 * TM], F32, esD) for i in range(2)]
                    actTs = [sb("actT%d" % i, [P, 2 * TM], BF16, esD) for i in range(2)]

                    def load_gu(ex):
                        i = ex % 2
                        dma("pool", wgs[i], wg_d[l, ex].rearrange("(c p) n -> p c n", p=P), [], [("wg", i)])
                        dma("pool", wus[i], wu_d[l, ex].rearrange("(c p) n -> p c n", p=P), [], [("wu", i)])

                    def load_d(ex):
                        i = ex % 2
                        dma("pool", wds[i], wd_d[l, ex].rearrange("(c p) n -> p c n", p=P), [], [("wd", i)])

                    load_gu(0)
                    load_d(0)
                    mtasks = []
                    cnt = 0
                    for ex in range(NEXP):
                        i = ex % 2
                        for t in range(NTM):
                            stg = [[] for _ in range(4)]
                            mtasks.append(stg)
                            par = cnt % 2
                            cnt += 1
                            cols = slice(t * TM, (t + 1) * TM)
                            t5 = (t * TM) // T
                            sc.defer = stg[0]
                            if t == 0 and ex + 1 < NEXP:
                                load_gu(ex + 1)
                                if ex == 0:
                                    load_d(1)
                            Gb, gk = Z[par][:, 0:2 * TM], ("Zb", 2 * par)
                            Ub, uk = Z[par][:, 512:512 + 2 * TM], ("Zb", 2 * par + 1)
                            CW, ck = WTp[:, par * 512:par * 512 + TM], ("WT", par)
                            for j in range(2):
                                for c in range(KC):
                                    mm(Gb[:, j * TM:(j + 1) * TM], wgs[i][:, c, j * P:(j + 1) * P], hT[:, c, cols],
                                       c == 0, c == KC - 1, [("wg", i), ("hT", t5)], [gk])
                            for j in range(2):
                                for c in range(KC):
                                    mm(Ub[:, j * TM:(j + 1) * TM], wus[i][:, c, j * P:(j + 1) * P], hT[:, c, cols],
                                       c == 0, c == KC - 1, [("wu", i), ("hT", t5)], [uk])
                            mm(CW, selrows[:, ex * P:(ex + 1) * P], cwT[:, 0, cols], True, False, ["selrows", "cwT"], [ck])
                            mm(CW, selrows[:, ex * P:(ex + 1) * P], cwT[:, 1, cols], False, True, ["selrows", "cwT"], [ck])
                            sg, sk = sgs[par], ("sg", par)
                            aT, ak = actTs[par], ("actT", par)
                            sc.defer = stg[1]
                            act(sg[:], Gb, AF.Silu, [gk], [sk])
                            sc.defer = stg[2]
                            tt("dve", sg[:], sg[:], Ub, ALU.mult, [sk, uk], [sk])
                            for j in range(2):
                                tt("dve", aT[:, j * TM:(j + 1) * TM], sg[:, j * TM:(j + 1) * TM], CW, ALU.mult,
                                   [sk, ck], [ak])
                            sc.defer = stg[3]
                            for dp in range(KC // 2):
                                Yb = M[dp % 2][:, 0:2 * TM]
                                yk = MK(dp % 2)
                                for dd in range(2):
                                    dc = 2 * dp + dd
                                    for j in range(2):
                                        mm(Yb[:, dd * TM:(dd + 1) * TM], wds[i][:, j, dc * P:(dc + 1) * P],
                                           aT[:, j * TM:(j + 1) * TM], j == 0, j == 1, [("wd", i), ak], yk)
                                yv = yacc[:, 2 * dp:2 * dp + 2, cols]
                                Yv = Yb.rearrange("p (a t) -> p a t", a=2)
                                if ex == 0:
                                    cp("act" if dp % 2 else "dve", yv, Yv, yk, [("ya", t5)])
                                else:
                                    tt("dve", yv, yv, Yv, ALU.add, yk + [("ya", t5)], [("ya", t5)])
                            if t == NTM - 1 and ex + 2 < NEXP:
                                load_d(ex + 2)
                    sc.defer = None
                    nt = len(mtasks)
                    for step in range(nt + 3):
                        for sg_ in range(3, -1, -1):
                            k = step - sg_
                            if 0 <= k < nt:
                                sc.flush(mtasks[k][sg_])
                    sc.barrier()

                    xbufs = [(xt, "xt"), (xt2e, "xt2e")]
                    etasks = []
                    for t in range(NT):
                        cols = slice(t * T, (t + 1) * T)
                        xb, xk = xbufs[t % 2]
                        stg = [[] for _ in range(3)]
                        etasks.append(stg)
                        sc.defer = stg[0]
                        reload(b, t, xb, xk)
                        for dc in range(KC):
                            act(xb[:, dc, :], xb[:, dc, :], AF.Copy, [xk], [xk], scale=ALPHA)
                            stt("dve", xb[:, dc, :], yacc[:, dc, cols], mod(l, b, 40 + dc), xb[:, dc, :], ALU.mult, ALU.add,
                                [("ya", t), "modT", xk], [xk])
                        sc.defer = stg[1]
                        stats([xk], xb)
                        for c in range(KC):
                            normalize(c, xb[:, c, :], lnpar(l, 2, c), lnpar(l, 3, c), [xk], [xk], None, xb)
                        sc.defer = stg[2]
                        if l + 1 < L:
                            spill(b, t, xb, xk)
                            adaln_to_hT(l + 1, b, t, 0, xt=xb, xk=xk)
                        else:
                            for s_ in range(NS):
                                tok0 = t * T + s_ * P
                                xi, xik = xins[io_cnt[0] % 2]
                                io_cnt[0] += 1
                                for half in range(2):
                                    Zt = Z[half]
                                    for c4 in range(4):
                                        c = half * 4 + c4
                                        tr(Zt[:, c4 * P:(c4 + 1) * P], xb[:, c, s_ * P:(s_ + 1) * P], ident_f[:],
                                           [xk, "ident_f"], [("Zb", 2 * half)])
                                    cp("act" if half else "dve", xi[:, half * 512:(half + 1) * 512], Zt[:, 0:512],
                                       [("Zb", 2 * half)], [xik])
                                dma("sp", out_d[b, tok0:tok0 + P, :], xi[:], [xik], [("out", b, tok0)], is_out=True)
                    sc.defer = None
                    for step in range(NT + 2):
                        for sg_ in range(2, -1, -1):
                            k = step - sg_
                            if 0 <= k < NT:
                                sc.flush(etasks[k][sg_])
                    sc.barrier()

        sc.finish()
        block = es.enter_context(nc.Block())
        sc.replay(block)
    return nc


def make_in_maps(inputs, n_cores, NB, L=2):
    f = np.float32
    x = np.ascontiguousarray(inputs["x"], dtype=f)
    c = np.asarray(inputs["c"], dtype=f)
    pos = np.asarray(inputs["positions"]).astype(np.int32)
    w_in = np.ascontiguousarray(inputs["w_in"], dtype=f)
    S = x.shape[1]
    inv = (np.float32(10000.0) ** (-np.arange(0, 32, 2, dtype=np.float32) / np.float32(32))).astype(f)
    cst = np.zeros((P, 4), f)
    cst[:, 1] = 1.0
    for i in range(32):
        cst[i, 0] = inv[i % 16]
        cst[i, 1] = -1.0 if i < 16 else 1.0
    w_kr = np.ascontiguousarray(np.concatenate(
        [w_in[:, :, 1920:1952], w_in[:, :, 1936:1952], w_in[:, :, 1920:1936]], axis=2))
    w_uq0 = np.asarray(inputs["w_uq"], dtype=f)
    wq4 = w_uq0.reshape(L, 256, 8, 96)
    zq = np.zeros((L, 256, 8, 32), f)
    w_uq = np.ascontiguousarray(np.concatenate([wq4[..., 64:96], zq, wq4[..., 0:64]], axis=-1).reshape(L, 256, 1024))
    w_uqs = np.ascontiguousarray(np.concatenate([wq4[..., 80:96], wq4[..., 64:80]], axis=-1).reshape(L, 256, 256))
    wkv4 = np.asarray(inputs["w_ukv"], dtype=f).reshape(L, 128, 8, 128)
    zk = np.zeros((L, 128, 8, 64), f)
    wk_p = np.ascontiguousarray(np.concatenate([zk, wkv4[..., 0:64]], axis=-1).reshape(L, 128, 1024))
    wv_p = np.ascontiguousarray(wkv4[..., 64:128].reshape(L, 128, 512))
    ada_bT = np.ascontiguousarray(np.asarray(inputs["ada_b"], dtype=f).reshape(L, 48, P).transpose(2, 0, 1).reshape(P, L * 48))
    lnp = np.stack([np.asarray(inputs[k], dtype=f) for k in ("ln1_g", "ln1_b", "ln2_g", "ln2_b")], axis=1)
    lnp = np.ascontiguousarray(lnp.reshape(L, 4, KC, P).transpose(3, 0, 1, 2).reshape(P, L * 4 * KC))
    qn = np.ascontiguousarray(np.asarray(inputs["q_norm"], dtype=f).reshape(L, 2, P).transpose(2, 0, 1).reshape(P, L * 2))
    kvn = np.ascontiguousarray(np.asarray(inputs["kv_norm"], dtype=f).reshape(L, P).T)
    rbias = np.ascontiguousarray(np.broadcast_to(np.asarray(inputs["router_bias"], dtype=f)[None, :], (P, NE)))
    shared = {
        "cst": cst, "ada_w": np.ascontiguousarray(inputs["ada_w"], dtype=f), "ada_bT": ada_bT,
        "w_in": w_in, "w_kr": w_kr, "qn": qn, "kvn": kvn, "w_uq": w_uq, "w_uqs": w_uqs,
        "wk_p": wk_p, "wv_p": wv_p, "w_o": np.ascontiguousarray(inputs["w_o"], dtype=f),
        "lnp": lnp, "router_w": np.ascontiguousarray(inputs["router_w"], dtype=f), "rbias": rbias,
        "w_gate": np.ascontiguousarray(inputs["w_gate"], dtype=f), "w_up": np.ascontiguousarray(inputs["w_up"], dtype=f),
        "w_down": np.ascontiguousarray(inputs["w_down"], dtype=f),
    }
    maps = []
    for ci in range(n_cores):
        sl = slice(ci * NB, (ci + 1) * NB)
        cc = c[sl]
        cT = np.ascontiguousarray(cc.reshape(NB, KC, P).transpose(2, 1, 0).reshape(P, KC * NB))
        posr = np.ascontiguousarray(np.broadcast_to(pos[sl][:, None, :], (NB, 32, S)))
        m = dict(shared)
        m.update({"x": np.ascontiguousarray(x[sl]), "cT": cT, "posr": posr})
        maps.append(m)
    return maps


_NC_CACHE = {}


def kernel(**inputs):
    n_cores = 8
    B, S, _ = inputs["x"].shape
    NB = B // n_cores
    key = (S, NB)
    if key not in _NC_CACHE:
        _NC_CACHE[key] = build_nc(S=S, NB=NB, L=2)
    nc = _NC_CACHE[key]
    maps = make_in_maps(inputs, n_cores, NB)
    res = run_bass_kernel_spmd(nc, maps, core_ids=list(range(n_cores)))
    out = np.concatenate([np.asarray(r["out"]) for r in res.results], axis=0)
    return out.astype(np.float32)
```
